# Optimizing a Trainium2 kernel written in Bass

```python
import math
import jax, jax.numpy as jnp
from jax import lax
import numpy as np

D_MODEL = 1024
BATCH = 4
SEQ = 4096
DEPTH = 2

GRID_W = 64
CTX_LEN = 256
EPS = 1e-6
ROPE_BASE = 10000.0
QBLK = 128

MLA_HEADS = 8
MLA_NOPE = 64
MLA_ROPE = 32
MLA_V = 64
MLA_Q_RANK = 256
MLA_KV_RANK = 128
MLA_SCALE = 1.0 / math.sqrt(MLA_NOPE + MLA_ROPE)

DIFF_HEADS = 4
DIFF_HD = 64
DIFF_SCALE = 1.0 / math.sqrt(DIFF_HD)

ATT_SPLITS = (MLA_Q_RANK,
              MLA_Q_RANK + MLA_KV_RANK,
              MLA_Q_RANK + MLA_KV_RANK + MLA_ROPE,
              MLA_Q_RANK + MLA_KV_RANK + MLA_ROPE + DIFF_HEADS * 2 * DIFF_HD,
              MLA_Q_RANK + MLA_KV_RANK + MLA_ROPE + 2 * DIFF_HEADS * 2 * DIFF_HD)
ATT_IN = MLA_Q_RANK + MLA_KV_RANK + MLA_ROPE + 3 * DIFF_HEADS * 2 * DIFF_HD
ATT_MIX = MLA_HEADS * MLA_V + DIFF_HEADS * 2 * DIFF_HD

D_INNER = 2 * D_MODEL
SSD_HEADDIM = 64
SSD_HEADS = D_INNER // SSD_HEADDIM
SSD_GROUPS = 4
SSD_HPG = SSD_HEADS // SSD_GROUPS
SSD_STATE = 128
SSD_CONV = 5
SSD_CHUNK = 128
SSD_CONV_DIM = D_INNER + 2 * SSD_GROUPS * SSD_STATE
SSD_IN = D_INNER + SSD_CONV_DIM + 2 * SSD_HEADS
SSD_SPLITS = (D_INNER, D_INNER + SSD_CONV_DIM)

MOE_GROUPS = 4
MOE_EXPERTS = 8
MOE_TOPK = 2
MOE_HIDDEN = 512

kernel_name = "hybrid_mla_diffattn_ssd_hmoe_dit"


def rmsnorm(x, g):
    xf = x.astype(jnp.float32)
    y = xf * lax.rsqrt(jnp.mean(xf * xf, axis=-1, keepdims=True) + EPS)
    return (y * g.astype(jnp.float32)).astype(x.dtype)


def axial_tables(rows, rot_dim):
    row = jnp.repeat(jnp.arange(rows, dtype=jnp.float32), GRID_W)
    col = jnp.tile(jnp.arange(GRID_W, dtype=jnp.float32), rows)
    nf = rot_dim // 4
    inv = ROPE_BASE ** (-jnp.arange(nf, dtype=jnp.float32) / nf)
    ang = jnp.concatenate([row[:, None] * inv, col[:, None] * inv], axis=-1)
    return jnp.cos(ang), jnp.sin(ang)


def apply_rope(x, rope):
    cos, sin = rope
    c = cos[:, None, :].astype(x.dtype)
    s = sin[:, None, :].astype(x.dtype)
    x1, x2 = jnp.split(x, 2, axis=-1)
    return jnp.concatenate([x1 * c - x2 * s, x2 * c + x1 * s], axis=-1)


def softmax_f32(s):
    return jax.nn.softmax(s.astype(jnp.float32), axis=-1)


def mla_core(q, k, v):
    p = softmax_f32(jnp.einsum('bqhd,bkhd->bhqk', q, k) * MLA_SCALE).astype(v.dtype)
    return jnp.einsum('bhqk,bkhd->bqhd', p, v)


def diff_core(q1, q2, k1, k2, v, lam):
    p1 = softmax_f32(jnp.einsum('bqhd,bkhd->bhqk', q1, k1) * DIFF_SCALE)
    p2 = softmax_f32(jnp.einsum('bqhd,bkhd->bhqk', q2, k2) * DIFF_SCALE)
    return jnp.einsum('bhqk,bkhd->bqhd', (p1 - lam * p2).astype(v.dtype), v)


def query_blocks(fn, *qs):
    b, n = qs[0].shape[:2]
    nb = n // QBLK
    blocks = tuple(jnp.swapaxes(q.reshape(b, nb, QBLK, *q.shape[2:]), 0, 1) for q in qs)
    out = lax.map(lambda blk: fn(*blk), blocks)
    return jnp.swapaxes(out, 0, 1).reshape(b, n, out.shape[-1])


def attn_groups(h, w_in, q_norm, w_uq, kv_norm, w_ukv, rope_mla, rope_diff):
    b, n, _ = h.shape
    c_q, c_kv, k_pe, dq, dk, dv = jnp.split(h @ w_in, ATT_SPLITS, axis=-1)
    q = (rmsnorm(c_q, q_norm) @ w_uq).reshape(b, n, MLA_HEADS, MLA_NOPE + MLA_ROPE)
    kv = (rmsnorm(c_kv, kv_norm) @ w_ukv).reshape(b, n, MLA_HEADS, MLA_NOPE + MLA_V)
    q_nope, q_pe = q[..., :MLA_NOPE], q[..., MLA_NOPE:]
    k_nope, v_mla = kv[..., :MLA_NOPE], kv[..., MLA_NOPE:]
    k_pe = k_pe[:, :, None, :]
    dq = dq.reshape(b, n, DIFF_HEADS, 2, DIFF_HD)
    dk = dk.reshape(b, n, DIFF_HEADS, 2, DIFF_HD)
    q1, q2, k1, k2 = dq[..., 0, :], dq[..., 1, :], dk[..., 0, :], dk[..., 1, :]
    if rope_mla is not None:
        q_pe = apply_rope(q_pe, rope_mla)
        k_pe = apply_rope(k_pe, rope_mla)
        q1, q2, k1, k2 = (apply_rope(t, rope_diff) for t in (q1, q2, k1, k2))
    k_pe = jnp.broadcast_to(k_pe, (b, n, MLA_HEADS, MLA_ROPE))
    q_mla = jnp.concatenate([q_nope, q_pe], axis=-1)
    k_mla = jnp.concatenate([k_nope, k_pe], axis=-1)
    v_d = dv.reshape(b, n, DIFF_HEADS, 2 * DIFF_HD)
    return q_mla, k_mla, v_mla, q1, q2, k1, k2, v_d


def attention_mixer(h, hc, w_in, q_norm, w_uq, kv_norm, w_ukv, lq1, lk1, lq2, lk2, subln, w_out,
                    lambda_init, rope_mla, rope_diff, need_ctx_out):
    lat = attn_groups(h, w_in, q_norm, w_uq, kv_norm, w_ukv, rope_mla, rope_diff)
    cxt = attn_groups(hc, w_in, q_norm, w_uq, kv_norm, w_ukv, None, None)
    f32 = jnp.float32
    lam = (jnp.exp(jnp.sum(lq1.astype(f32) * lk1.astype(f32)))
           - jnp.exp(jnp.sum(lq2.astype(f32) * lk2.astype(f32))) + lambda_init)

    def heads(q_mla, q1, q2, k_mla, v_mla, k1, k2, v_d):
        bq, nq = q_mla.shape[:2]
        o_mla = mla_core(q_mla, k_mla, v_mla).reshape(bq, nq, MLA_HEADS * MLA_V)
        o_d = rmsnorm(diff_core(q1, q2, k1, k2, v_d, lam), subln) * (1.0 - lambda_init)
        return jnp.concatenate([o_mla, o_d.reshape(bq, nq, DIFF_HEADS * 2 * DIFF_HD)], axis=-1)

    kv_all = [jnp.concatenate([cxt[i], lat[i]], axis=1) for i in (1, 2, 5, 6, 7)]
    y_lat = query_blocks(lambda qm, a1, a2: heads(qm, a1, a2, *kv_all), lat[0], lat[3], lat[4]) @ w_out
    y_ctx = None
    if need_ctx_out:
        y_ctx = heads(cxt[0], cxt[3], cxt[4], cxt[1], cxt[2], cxt[5], cxt[6], cxt[7]) @ w_out
    return y_lat, y_ctx


def depthwise_conv_centred(u, w, b):
    k = w.shape[0]
    out = lax.conv_general_dilated(u, w[:, None, :], window_strides=(1,), padding=[(k // 2, k // 2)],
                                   dimension_numbers=('NWC', 'WIO', 'NWC'),
                                   feature_group_count=u.shape[-1])
    return out + b


def ssd_scan(x, dt, a, bm, cm, d_skip, init_state, with_output):
    f32 = jnp.float32
    b, n = x.shape[:2]
    nc = n // SSD_CHUNK
    xr = x.astype(f32).reshape(b, nc, SSD_CHUNK, SSD_GROUPS, SSD_HPG, SSD_HEADDIM)
    dtr = dt.reshape(b, nc, SSD_CHUNK, SSD_GROUPS, SSD_HPG)
    br = bm.astype(f32).reshape(b, nc, SSD_CHUNK, SSD_GROUPS, SSD_STATE)
    cr = cm.astype(f32).reshape(b, nc, SSD_CHUNK, SSD_GROUPS, SSD_STATE)
    a_cs = jnp.cumsum(dtr * a.reshape(SSD_GROUPS, SSD_HPG), axis=2)
    xdt = xr * dtr[..., None]
    decay_to_end = jnp.exp(a_cs[:, :, -1:] - a_cs)
    chunk_states = jnp.einsum('bcjgn,bcjgr,bcjgrp->bcgrpn', br, decay_to_end, xdt)
    chunk_decay = jnp.exp(a_cs[:, :, -1])

    def step(state, inp):
        st, dec = inp
        return dec[..., None, None] * state + st, state

    final, prev = lax.scan(step, init_state,
                           (jnp.moveaxis(chunk_states, 1, 0), jnp.moveaxis(chunk_decay, 1, 0)))
    if not with_output:
        return None, final
    prev = jnp.moveaxis(prev, 0, 1)
    lower = jnp.tril(jnp.ones((SSD_CHUNK, SSD_CHUNK), dtype=bool))[:, :, None, None]
    seg = a_cs[:, :, :, None] - a_cs[:, :, None]
    lmat = jnp.exp(jnp.where(lower, seg, -jnp.inf))
    cb = jnp.einsum('bcign,bcjgn->bcijg', cr, br)
    y = (jnp.einsum('bcijg,bcijgr,bcjgrp->bcigrp', cb, lmat, xdt)
         + jnp.einsum('bcign,bcigr,bcgrpn->bcigrp', cr, jnp.exp(a_cs), prev)
         + d_skip.astype(f32).reshape(SSD_GROUPS, SSD_HPG)[:, :, None] * xr)
    return y.reshape(b, n, SSD_HEADS, SSD_HEADDIM).astype(x.dtype), final


def gated_rmsnorm(y, z, g):
    b, n, _ = y.shape
    u = (y * jax.nn.silu(z)).astype(jnp.float32).reshape(b, n, SSD_GROUPS, D_INNER // SSD_GROUPS)
    u = u * lax.rsqrt(jnp.mean(u * u, axis=-1, keepdims=True) + EPS)
    return (u.reshape(b, n, D_INNER) * g.astype(jnp.float32)).astype(z.dtype)


def ssd_mixer(h, hc, w_in, conv_w, conv_b, dt_bias, a_log, d_skip, norm_g, w_out, need_ctx_out):
    f32 = jnp.float32
    a = -jnp.exp(a_log.astype(f32))

    def prep(u):
        b, n, _ = u.shape
        z, xbc, dt = jnp.split(u @ w_in, SSD_SPLITS, axis=-1)
        xbc = jax.nn.silu(depthwise_conv_centred(xbc, conv_w, conv_b))
        xs, bm, cm = jnp.split(xbc, (D_INNER, D_INNER + SSD_GROUPS * SSD_STATE), axis=-1)
        dt = jax.nn.softplus(dt.astype(f32).reshape(b, n, 2, SSD_HEADS) + dt_bias.astype(f32))
        return (z, xs.reshape(b, n, SSD_HEADS, SSD_HEADDIM),
                bm.reshape(b, n, SSD_GROUPS, SSD_STATE), cm.reshape(b, n, SSD_GROUPS, SSD_STATE), dt)

    z, xs, bm, cm, dt = prep(h)
    zc, xc, bc, cc, dtc = prep(hc)
    b, n = h.shape[:2]
    zero = jnp.zeros((b, SSD_GROUPS, SSD_HPG, SSD_HEADDIM, SSD_STATE), f32)
    rev = lambda t: jnp.flip(t, axis=1)
    yc_f, s_f = ssd_scan(xc, dtc[:, :, 0], a[0], bc, cc, d_skip[0], zero, need_ctx_out)
    yc_b, s_b = ssd_scan(rev(xc), rev(dtc[:, :, 1]), a[1], rev(bc), rev(cc), d_skip[1], zero, need_ctx_out)
    y_f, _ = ssd_scan(xs, dt[:, :, 0], a[0], bm, cm, d_skip[0], s_f, True)
    y_b, _ = ssd_scan(rev(xs), rev(dt[:, :, 1]), a[1], rev(bm), rev(cm), d_skip[1], s_b, True)
    y_lat = gated_rmsnorm((y_f + rev(y_b)).reshape(b, n, D_INNER), z, norm_g) @ w_out
    y_ctx = None
    if need_ctx_out:
        lc = hc.shape[1]
        y_ctx = gated_rmsnorm((yc_f + rev(yc_b)).reshape(b, lc, D_INNER), zc, norm_g) @ w_out
    return y_lat, y_ctx


def hier_moe(h, w_rg, b_rg, w_rf, b_rf, w_gate, w_up, w_down):
    n = h.shape[0]
    f32 = jnp.float32
    pg = softmax_f32(h @ w_rg + b_rg)
    p_top, g_top = lax.top_k(pg, 1)
    group_gate = jax.nn.one_hot(g_top[:, 0], MOE_GROUPS, dtype=f32) * p_top
    lf = (h @ w_rf + b_rf).astype(f32).reshape(n, MOE_GROUPS, MOE_EXPERTS)
    v_top, e_top = lax.top_k(lf, MOE_TOPK)
    fine = jnp.sum(jax.nn.one_hot(e_top, MOE_EXPERTS, dtype=f32)
                   * jax.nn.softmax(v_top, axis=-1)[..., None], axis=-2)
    combine = (group_gate[..., None] * fine).astype(h.dtype)
    out = jnp.zeros_like(h)
    for g in range(MOE_GROUPS):
        act = jax.nn.silu(jnp.einsum('nd,edf->enf', h, w_gate[g])) * jnp.einsum('nd,edf->enf', h, w_up[g])
        out = out + jnp.einsum('enf,efd,ne->nd', act, w_down[g], combine[:, g])
    return out


def setup_inputs(seed: int = 0) -> dict:
    key = jax.random.key(seed)
    ks = iter(jax.random.split(key, 48))
    f32 = jnp.float32
    D = D_MODEL
    n_att = (DEPTH + 1) // 2
    n_ssd = DEPTH // 2

    def nrm(shape, scale):
        return jax.random.normal(next(ks), shape, f32) * scale

    def gain(shape):
        return 1.0 + nrm(shape, 0.02)

    inp = {}
    inp["x"] = nrm((BATCH, SEQ, D), 1.0)
    inp["c"] = nrm((BATCH, D), 1.0)
    inp["ctx"] = nrm((BATCH, CTX_LEN, D), 1.0)
    inp["c_ctx"] = nrm((D,), 1.0)
    inp["w_mod"] = nrm((DEPTH, D, 6 * D), 0.5 * D ** -0.5)
    inp["b_mod"] = nrm((DEPTH, 6 * D), 0.02)
    inp["norm_g"] = gain((DEPTH, 2, D))
    inp["final_g"] = gain((D,))
    inp["att_w_in"] = nrm((n_att, D, ATT_IN), D ** -0.5)
    inp["att_q_norm"] = gain((n_att, MLA_Q_RANK))
    inp["att_w_uq"] = nrm((n_att, MLA_Q_RANK, MLA_HEADS * (MLA_NOPE + MLA_ROPE)), MLA_Q_RANK ** -0.5)
    inp["att_kv_norm"] = gain((n_att, MLA_KV_RANK))
    inp["att_w_ukv"] = nrm((n_att, MLA_KV_RANK, MLA_HEADS * (MLA_NOPE + MLA_V)), MLA_KV_RANK ** -0.5)
    inp["att_lq1"] = nrm((n_att, DIFF_HD), 0.1)
    inp["att_lk1"] = nrm((n_att, DIFF_HD), 0.1)
    inp["att_lq2"] = nrm((n_att, DIFF_HD), 0.1)
    inp["att_lk2"] = nrm((n_att, DIFF_HD), 0.1)
    inp["att_subln"] = gain((n_att, 2 * DIFF_HD))
    inp["att_w_out"] = nrm((n_att, ATT_MIX, D), ATT_MIX ** -0.5)
    inp["ssd_w_in"] = nrm((n_ssd, D, SSD_IN), D ** -0.5)
    inp["ssd_conv_w"] = nrm((n_ssd, SSD_CONV, SSD_CONV_DIM), SSD_CONV ** -0.5)
    inp["ssd_conv_b"] = nrm((n_ssd, SSD_CONV_DIM), 0.02)
    dt0 = jnp.exp(jax.random.uniform(next(ks), (n_ssd, 2, SSD_HEADS), f32, math.log(1e-3), math.log(1e-1)))
    inp["ssd_dt_bias"] = dt0 + jnp.log(-jnp.expm1(-dt0))
    inp["ssd_a_log"] = jnp.log(jax.random.uniform(next(ks), (n_ssd, 2, SSD_HEADS), f32, 1.0, 16.0))
    inp["ssd_d"] = gain((n_ssd, 2, SSD_HEADS))
    inp["ssd_norm_g"] = gain((n_ssd, D_INNER))
    inp["ssd_w_out"] = nrm((n_ssd, D_INNER, D), D_INNER ** -0.5)
    inp["moe_w_rg"] = nrm((DEPTH, D, MOE_GROUPS), D ** -0.5)
    inp["moe_b_rg"] = nrm((DEPTH, MOE_GROUPS), 0.01)
    inp["moe_w_rf"] = nrm((DEPTH, D, MOE_GROUPS * MOE_EXPERTS), D ** -0.5)
    inp["moe_b_rf"] = nrm((DEPTH, MOE_GROUPS * MOE_EXPERTS), 0.01)
    inp["moe_w_gate"] = nrm((DEPTH, MOE_GROUPS, MOE_EXPERTS, D, MOE_HIDDEN), D ** -0.5)
    inp["moe_w_up"] = nrm((DEPTH, MOE_GROUPS, MOE_EXPERTS, D, MOE_HIDDEN), D ** -0.5)
    inp["moe_w_down"] = nrm((DEPTH, MOE_GROUPS, MOE_EXPERTS, MOE_HIDDEN, D), MOE_HIDDEN ** -0.5)
    return inp


def reference(x, c, ctx, c_ctx, w_mod, b_mod, norm_g, final_g,
              att_w_in, att_q_norm, att_w_uq, att_kv_norm, att_w_ukv,
              att_lq1, att_lk1, att_lq2, att_lk2, att_subln, att_w_out,
              ssd_w_in, ssd_conv_w, ssd_conv_b, ssd_dt_bias, ssd_a_log, ssd_d, ssd_norm_g, ssd_w_out,
              moe_w_rg, moe_b_rg, moe_w_rf, moe_b_rf, moe_w_gate, moe_w_up, moe_w_down):
    b, n, d = x.shape
    lc = ctx.shape[1]
    rows = n // GRID_W
    rope_mla = axial_tables(rows, MLA_ROPE)
    rope_diff = axial_tables(rows, DIFF_HD)
    xc = ctx
    for l in range(DEPTH):
        last = l == DEPTH - 1
        i = l // 2
        mod = (jax.nn.silu(c) @ w_mod[l] + b_mod[l])[:, None, :]
        modc = jax.nn.silu(c_ctx) @ w_mod[l] + b_mod[l]
        sh1, sc1, g1, sh2, sc2, g2 = jnp.split(mod, 6, axis=-1)
        sh1c, sc1c, g1c, sh2c, sc2c, g2c = jnp.split(modc, 6, axis=-1)
        hx = rmsnorm(x, norm_g[l, 0]) * (1 + sc1) + sh1
        hc = rmsnorm(xc, norm_g[l, 0]) * (1 + sc1c) + sh1c
        if l % 2 == 0:
            y, yc = attention_mixer(hx, hc, att_w_in[i], att_q_norm[i], att_w_uq[i], att_kv_norm[i],
                                    att_w_ukv[i], att_lq1[i], att_lk1[i], att_lq2[i], att_lk2[i],
                                    att_subln[i], att_w_out[i], 0.8 - 0.6 * math.exp(-0.3 * l),
                                    rope_mla, rope_diff, not last)
        else:
            y, yc = ssd_mixer(hx, hc, ssd_w_in[i], ssd_conv_w[i], ssd_conv_b[i], ssd_dt_bias[i],
                              ssd_a_log[i], ssd_d[i], ssd_norm_g[i], ssd_w_out[i], not last)
        x = x + g1 * y
        hx = rmsnorm(x, norm_g[l, 1]) * (1 + sc2) + sh2
        moe_args = (moe_w_rg[l], moe_b_rg[l], moe_w_rf[l], moe_b_rf[l], moe_w_gate[l], moe_w_up[l], moe_w_down[l])
        if last:
            x = x + g2 * hier_moe(hx.reshape(b * n, d), *moe_args).reshape(b, n, d)
        else:
            xc = xc + g1c * yc
            hc = rmsnorm(xc, norm_g[l, 1]) * (1 + sc2c) + sh2c
            out = hier_moe(jnp.concatenate([hx.reshape(b * n, d), hc.reshape(b * lc, d)], axis=0), *moe_args)
            x = x + g2 * out[:b * n].reshape(b, n, d)
            xc = xc + g2c * out[b * n:].reshape(b, lc, d)
    return rmsnorm(x, final_g)
```

```python
import math
from contextlib import ExitStack
import numpy as np
import concourse.bass as bass
import concourse.mybir as mybir
from concourse.bass_utils import run_bass_kernel_spmd

F32 = mybir.dt.float32
BF16 = mybir.dt.bfloat16
AF = mybir.ActivationFunctionType
ALU = mybir.AluOpType
AX = mybir.AxisListType

EPS = 1e-6
D = 1024
MLA_SCALE = 1.0 / math.sqrt(96.0)
DIFF_SCALE = 1.0 / math.sqrt(64.0)
LAMBDA_INIT0 = 0.8 - 0.6 * math.exp(-0.3 * 0)


class Res:
    __slots__ = ("w", "r")

    def __init__(self):
        self.w = None
        self.r = []


class Prog:
    ENGS = ("pe", "act", "dve", "pool", "sp")
    NDMASEM = 12

    def __init__(self, nc, stack):
        self.nc = nc
        self.ops = {e: [] for e in self.ENGS}
        self.cnt = {e: 0 for e in self.ENGS}
        self.sem = {e: stack.enter_context(nc.semaphore("s_" + e)) for e in self.ENGS}
        self.seen = {e: {} for e in self.ENGS}
        self.dsem, self.dcnt, self.dnext = {}, {}, {}
        for q in ("sp", "act", "pool"):
            self.dsem[q] = [stack.enter_context(nc.semaphore("d_%s%d" % (q, i)))
                            for i in range(self.NDMASEM)]
            self.dcnt[q] = [0] * self.NDMASEM
            self.dnext[q] = 0

    def _need(self, e, tok, waits):
        if tok is None:
            return
        if tok[0] == "e":
            _, pe_, v = tok
            if pe_ == e and e == "pe":
                return
            key = ("e", pe_)
        else:
            _, q, i, v = tok
            key = ("d", q, i)
        if self.seen[e].get(key, 0) >= v:
            return
        if v > waits.get(key, 0):
            waits[key] = v

    def _deps(self, e, reads, writes):
        waits = {}
        for r in reads:
            self._need(e, r.w, waits)
        for w in writes:
            self._need(e, w.w, waits)
            for t in w.r:
                self._need(e, t, waits)
        out = []
        for key, v in waits.items():
            self.seen[e][key] = v
            if key[0] == "e":
                out.append((self.sem[key[1]], v))
            else:
                out.append((self.dsem[key[1]][key[2]], v))
        return out

    def _mark(self, tok, reads, writes):
        for r in reads:
            r.r.append(tok)
            if len(r.r) > 64:
                r.r = r.r[-48:]
        for w in writes:
            w.w = tok
            w.r = []

    def op(self, e, fn, reads=(), writes=()):
        waits = self._deps(e, reads, writes)
        self.cnt[e] += 1
        v = self.cnt[e]
        sem = self.sem[e]

        def run(eng, fn=fn, waits=waits, sem=sem):
            for s, val in waits:
                eng.wait_ge(s, val)
            fn(eng).then_inc(sem, 1)
        self.ops[e].append(run)
        tok = ("e", e, v)
        self._mark(tok, reads, writes)
        return tok

    def dma(self, q, out, in_, reads=(), writes=(), **kw):
        i = self.dnext[q]
        self.dnext[q] = (i + 1) % self.NDMASEM
        waits = self._deps(q, reads, writes)
        prev = self.dcnt[q][i]
        key = ("d", q, i)
        if prev > 0 and self.seen[q].get(key, 0) < prev:
            waits.append((self.dsem[q][i], prev))
            self.seen[q][key] = prev
        self.dcnt[q][i] = prev + 16
        v = prev + 16
        sem = self.dsem[q][i]

        def run(eng, waits=waits, sem=sem, out=out, in_=in_, kw=kw):
            for s, val in waits:
                eng.wait_ge(s, val)
            eng.dma_start(out=out, in_=in_, **kw).then_inc(sem, 16)
        self.ops[q].append(run)
        tok = ("d", q, i, v)
        self._mark(tok, reads, writes)
        return tok

    def flush(self, final=False):
        for q in ("sp", "act", "pool"):
            fin = []
            for i in range(self.NDMASEM):
                if self.dcnt[q][i] > 0 and self.seen[q].get(("d", q, i), 0) < self.dcnt[q][i]:
                    fin.append((self.dsem[q][i], self.dcnt[q][i]))
                    self.seen[q][("d", q, i)] = self.dcnt[q][i]

            def run(eng, fin=fin):
                for s, val in fin:
                    eng.wait_ge(s, val)
            self.ops[q].append(run)
        ops = self.ops
        with self.nc.Block() as block:
            @block.tensor
            def _(eng):
                for f in ops["pe"]:
                    f(eng)

            @block.scalar
            def _(eng):
                for f in ops["act"]:
                    f(eng)

            @block.vector
            def _(eng):
                for f in ops["dve"]:
                    f(eng)

            @block.gpsimd
            def _(eng):
                for f in ops["pool"]:
                    f(eng)

            @block.sync
            def _(eng):
                for f in ops["sp"]:
                    f(eng)
        self.ops = {e: [] for e in self.ENGS}


class TL:
    __slots__ = ("t", "r")

    def __init__(self, t):
        self.t = t
        self.r = Res()


class Ctx:
    def __init__(self, nc, st):
        self.nc = nc
        self.p = Prog(nc, st)
        self.n = 0

    def sb(self, st, shape, dt, name=None):
        self.n += 1
        return TL(st.enter_context(self.nc.sbuf_tensor("%s_%d" % (name or "sb", self.n), list(shape), dt)))

    def ps(self, st, shape, dt, name=None):
        self.n += 1
        return TL(st.enter_context(self.nc.psum_tensor("%s_%d" % (name or "ps", self.n), list(shape), dt)))

    def dram(self, shape, dt, name):
        self.n += 1
        return TL(self.nc.dram_tensor("%s_%d" % (name, self.n), list(shape), dt).ap())


def make_ident(cx, st, dt):
    idn = cx.sb(st, [128, 128], dt, "ident")
    if dt == F32:
        cx.p.op("pool", lambda e: e.memset(idn.t[:], 0.0), writes=[idn.r])
        cx.p.op("pool", lambda e: e.affine_select(out=idn.t[:], in_=idn.t[:], pattern=[[-1, 128]],
                                                  compare_op=ALU.not_equal, fill=1.0, base=0,
                                                  channel_multiplier=1), reads=[idn.r], writes=[idn.r])
    return idn


def emit_rstd(cx, ssq, rstd, n, eps=EPS):
    p = cx.p
    (s_tl, s_ap), (r_tl, r_ap) = ssq, rstd
    p.op("dve", lambda e: e.tensor_scalar(out=r_ap, in0=s_ap, scalar1=1.0 / n, scalar2=eps,
                                          op0=ALU.mult, op1=ALU.add), reads=[s_tl.r], writes=[r_tl.r])
    p.op("act", lambda e: e.activation(out=r_ap, in_=r_ap, func=AF.Sqrt), reads=[r_tl.r], writes=[r_tl.r])
    p.op("dve", lambda e: e.reciprocal(out=r_ap, in_=r_ap), reads=[r_tl.r], writes=[r_tl.r])


def phase_mod(cx, cvec, w_mod, b_mod, mod_d):
    p, nc = cx.p, cx.nc
    with ExitStack() as st:
        crow = cx.sb(st, [1, 2048], F32, "crow")
        ones = cx.sb(st, [1, 128], F32, "ones")
        brow = cx.sb(st, [1, 6144], F32, "brow")
        cbc = cx.sb(st, [128, 2, 8, 128], F32, "cbc")
        wm = [cx.sb(st, [128, 8, 512], F32, "wm") for _ in range(2)]
        mrow = [cx.sb(st, [1, 512], F32, "mrow") for _ in range(2)]
        pst = [cx.ps(st, [128, 128], F32, "pst") for _ in range(2)]
        psm = [cx.ps(st, [128, 512], F32, "psm") for _ in range(2)]
        p.dma("sp", crow.t[0:1, :], cvec.rearrange("(o s) d -> o (s d)", o=1), writes=[crow.r])
        p.dma("sp", brow.t[0:1, :], b_mod.rearrange("(o n) -> o n", o=1), writes=[brow.r])
        p.op("dve", lambda e: e.memset(ones.t[:], 1.0), writes=[ones.r])
        k = 0
        for s in range(2):
            for kc in range(8):
                pt = pst[k % 2]
                k += 1
                p.op("pe", lambda e, pt=pt, s=s, kc=kc: e.matmul(
                    pt.t[:], lhsT=crow.t[0:1, s * 1024 + kc * 128: s * 1024 + (kc + 1) * 128],
                    rhs=ones.t[0:1, :], start=True, stop=True), reads=[crow.r, ones.r], writes=[pt.r])
                p.op("act", lambda e, pt=pt, s=s, kc=kc: e.activation(
                    out=cbc.t[:, s, kc, :], in_=pt.t[:], func=AF.Silu), reads=[pt.r], writes=[cbc.r])
        wv = w_mod.rearrange("(kc p) n -> p kc n", p=128)
        k = 0
        for nb in range(12):
            w = wm[nb % 2]
            p.dma("sp" if nb % 2 == 0 else "act", w.t[:], wv[:, :, nb * 512:(nb + 1) * 512], writes=[w.r])
            for s in range(2):
                pm = psm[k % 2]
                mr = mrow[k % 2]
                k += 1
                for kc in range(8):
                    p.op("pe", lambda e, pm=pm, w=w, s=s, kc=kc: e.matmul(
                        pm.t[:], lhsT=cbc.t[:, s, kc, :], rhs=w.t[:, kc, :], start=(kc == 0), stop=(kc == 7)),
                        reads=[cbc.r, w.r], writes=[pm.r])
                p.op("dve", lambda e, pm=pm, mr=mr, nb=nb: e.tensor_tensor(
                    out=mr.t[0:1, :], in0=pm.t[0:1, :], in1=brow.t[0:1, nb * 512:(nb + 1) * 512], op=ALU.add),
                    reads=[pm.r, brow.r], writes=[mr.r])
                p.dma("sp", mod_d.t[s:s + 1, nb * 512:(nb + 1) * 512], mr.t[0:1, :], reads=[mr.r], writes=[mod_d.r])
        p.flush()


def load_bc(cx, q, dst, src_ap, n, src=None):
    tl, ap = dst
    return cx.p.dma(q, ap, src_ap.partition_broadcast(128), reads=([src.r] if src is not None else []), writes=[tl.r])


def emit_norm_mod(cx, x_t, h_t, A, Bm, junk, ssq, rstd):
    p = cx.p
    p.op("act", lambda e: e.activation(out=junk.t[:], in_=x_t.t[:], func=AF.Square, accum_out=ssq.t[:, 0:1]),
         reads=[x_t.r], writes=[junk.r, ssq.r])
    emit_rstd(cx, (ssq, ssq.t[:, 0:1]), (rstd, rstd.t[:, 0:1]), 1024.0)
    p.op("dve", lambda e: e.scalar_tensor_tensor(out=h_t.t[:], in0=x_t.t[:], scalar=rstd.t[:, 0:1], in1=A[1],
                                                 op0=ALU.mult, op1=ALU.mult),
         reads=[x_t.r, rstd.r, A[0].r], writes=[h_t.r])
    p.op("pool", lambda e: e.tensor_tensor(out=h_t.t[:], in0=h_t.t[:], in1=Bm[1], op=ALU.add),
         reads=[h_t.r, Bm[0].r], writes=[h_t.r])


def emit_transpose8(cx, h_t, hT, pt, idn, dt_in=F32):
    p = cx.p
    for kc in range(8):
        p.op("pe", lambda e, kc=kc: e.transpose(pt.t[:, kc * 128:(kc + 1) * 128], h_t.t[:, kc * 128:(kc + 1) * 128],
                                                idn.t[:]), reads=[h_t.r, idn.r], writes=[pt.r])
    p.op("act", lambda e: e.activation(out=hT.t[:, 0:4, :].rearrange("p a b -> p (a b)"), in_=pt.t[:, 0:512], func=AF.Copy),
         reads=[pt.r], writes=[hT.r])
    p.op("dve", lambda e: e.tensor_copy(out=hT.t[:, 4:8, :].rearrange("p a b -> p (a b)"), in_=pt.t[:, 512:1024]),
         reads=[pt.r], writes=[hT.r])


def emit_rope(cx, eng, src, dst, tmp, cos, sin, H, half, reads, writes):
    p = cx.p
    cb = cos.unsqueeze(1).to_broadcast([128, H, half])
    sbc = sin.unsqueeze(1).to_broadcast([128, H, half])
    x1, x2 = src[:, :, 0:half], src[:, :, half:2 * half]
    o1, o2 = dst[:, :, 0:half], dst[:, :, half:2 * half]
    t1, t2 = tmp[:, 0, :, :], tmp[:, 1, :, :]
    rd, wr = list(reads), list(writes)
    p.op(eng, lambda e: e.tensor_tensor(out=t1, in0=x1, in1=cb, op=ALU.mult), reads=rd, writes=wr)
    p.op(eng, lambda e: e.tensor_tensor(out=t2, in0=x2, in1=sbc, op=ALU.mult), reads=rd, writes=wr)
    p.op(eng, lambda e: e.tensor_tensor(out=o1, in0=t1, in1=t2, op=ALU.subtract), reads=rd + wr, writes=wr)
    p.op(eng, lambda e: e.tensor_tensor(out=t1, in0=x2, in1=cb, op=ALU.mult), reads=rd, writes=wr)
    p.op(eng, lambda e: e.tensor_tensor(out=t2, in0=x1, in1=sbc, op=ALU.mult), reads=rd, writes=wr)
    p.op(eng, lambda e: e.tensor_tensor(out=o2, in0=t1, in1=t2, op=ALU.add), reads=rd + wr, writes=wr)


DBG_CUT = None
NT_ALL = 34
NQ_T = 18
NK = NT_ALL * 128
NQ = NQ_T * 128


def phase_attn_prep(cx, xin, rope, mod_d, norm_g0, W, S):
    p, nc = cx.p, cx.nc
    with ExitStack() as st:
        idn = make_ident(cx, st, F32)
        gbc = cx.sb(st, [128, 1024], F32, "gbc")
        Abc = [cx.sb(st, [128, 1024], F32, "Abc") for _ in range(2)]
        Bbc = [cx.sb(st, [128, 1024], F32, "Bbc") for _ in range(2)]
        load_bc(cx, "sp", (gbc, gbc.t[:]), norm_g0, 1024)
        for s in range(2):
            load_bc(cx, "sp", (Bbc[s], Bbc[s].t[:]), mod_d.t[s, 0:1024], 1024, src=mod_d)
            load_bc(cx, "act", (Abc[s], Abc[s].t[:]), mod_d.t[s, 1024:2048], 1024, src=mod_d)
            p.op("dve", lambda e, s=s: e.scalar_tensor_tensor(out=Abc[s].t[:], in0=Abc[s].t[:], scalar=1.0, in1=gbc.t[:],
                                                              op0=ALU.add, op1=ALU.mult),
                 reads=[Abc[s].r, gbc.r], writes=[Abc[s].r])
        gq = cx.sb(st, [128, 256], F32, "gq")
        gkv = cx.sb(st, [128, 128], F32, "gkv")
        load_bc(cx, "sp", (gq, gq.t[:]), W["q_norm"], 256)
        load_bc(cx, "sp", (gkv, gkv.t[:]), W["kv_norm"], 128)
        w_in = cx.sb(st, [128, 8, 1952], BF16, "w_in")
        w_uq = cx.sb(st, [128, 2, 768], BF16, "w_uq")
        w_ukv = cx.sb(st, [128, 1024], BF16, "w_ukv")
        wiv = W["w_in"].rearrange("(kc p) n -> p kc n", p=128)
        for kc in range(8):
            p.dma("pool", w_in.t[:, kc, :], wiv[:, kc, :], writes=[w_in.r])
        p.dma("pool", w_uq.t[:], W["w_uq"].rearrange("(kc p) n -> p kc n", p=128), writes=[w_uq.r])
        p.dma("pool", w_ukv.t[:], W["w_ukv"], writes=[w_ukv.r])

        NB = 2
        x_t = [cx.sb(st, [128, 1024], F32, "x_t") for _ in range(NB)]
        rp_t = [cx.sb(st, [128, 96], F32, "rp_t") for _ in range(NB)]
        h_t = [cx.sb(st, [128, 1024], F32, "h_t") for _ in range(NB)]
        junk = cx.sb(st, [128, 1024], F32, "junk")
        small = [cx.sb(st, [128, 8], F32, "small") for _ in range(NB)]
        hT = [cx.sb(st, [128, 8, 128], BF16, "hT") for _ in range(NB)]
        pj = [cx.sb(st, [128, 1952], F32, "pj") for _ in range(NB)]
        cn = [cx.sb(st, [128, 384], F32, "cn") for _ in range(NB)]
        cnT = [cx.sb(st, [128, 3, 128], BF16, "cnT") for _ in range(NB)]
        Kt = [cx.sb(st, [128, 8, 96], F32, "Kt") for _ in range(NB)]
        Qt = [cx.sb(st, [128, 8, 96], F32, "Qt") for _ in range(NB)]
        qraw = [cx.sb(st, [128, 8, 96], F32, "qraw") for _ in range(NB)]
        Vm = [cx.sb(st, [128, 8, 66], BF16, "Vm") for _ in range(NB)]
        Vd = [cx.sb(st, [128, 4, 130], BF16, "Vd") for _ in range(NB)]
        dkr = [cx.sb(st, [128, 8, 64], F32, "dkr") for _ in range(NB)]
        dqr = [cx.sb(st, [128, 8, 64], F32, "dqr") for _ in range(NB)]
        tmp = [cx.sb(st, [128, 2, 8, 32], F32, "tmp") for _ in range(NB)]
        kper = [cx.sb(st, [128, 1, 32], F32, "kper") for _ in range(NB)]
        KTm_s = [cx.sb(st, [96, 8, 128], BF16, "KTm_s") for _ in range(NB)]
        QTm_s = [cx.sb(st, [96, 8, 128], BF16, "QTm_s") for _ in range(NB)]
        KTd_s = [cx.sb(st, [128, 4, 128], BF16, "KTd_s") for _ in range(NB)]
        QTd_s = [cx.sb(st, [128, 4, 128], BF16, "QTd_s") for _ in range(NB)]
        ptA = cx.ps(st, [128, 1024], F32, "ptA")
        ppj = cx.ps(st, [128, 2048], F32, "ppj")
        pkv = cx.ps(st, [128, 1024], F32, "pkv")
        for b in range(NB):
            p.op("pool", lambda e, b=b: e.memset(Vm[b].t[:], 1.0), writes=[Vm[b].r])
            p.op("pool", lambda e, b=b: e.memset(Vd[b].t[:], 1.0), writes=[Vd[b].r])

        for i in range(NT_ALL):
            b = i % NB
            is_ctx = i < 2
            is_q = i < NQ_T
            s = 1 if is_ctx else 0
            X, RP, H, SM, HT, PJ, CN, CNT = x_t[b], rp_t[b], h_t[b], small[b], hT[b], pj[b], cn[b], cnT[b]
            p.dma("sp", X.t[:], xin[i * 128:(i + 1) * 128, :], writes=[X.r])
            p.dma("act", RP.t[:], rope[i * 128:(i + 1) * 128, :], writes=[RP.r])
            p.op("act", lambda e, X=X, SM=SM: e.activation(out=junk.t[:], in_=X.t[:], func=AF.Square, accum_out=SM.t[:, 0:1]),
                 reads=[X.r], writes=[junk.r, SM.r])
            emit_rstd(cx, (SM, SM.t[:, 0:1]), (SM, SM.t[:, 1:2]), 1024.0)
            p.op("dve", lambda e, X=X, H=H, SM=SM, s=s: e.scalar_tensor_tensor(
                out=H.t[:], in0=X.t[:], scalar=SM.t[:, 1:2], in1=Abc[s].t[:], op0=ALU.mult, op1=ALU.mult),
                reads=[X.r, SM.r, Abc[s].r], writes=[H.r])
            p.op("pool", lambda e, H=H, s=s: e.tensor_tensor(out=H.t[:], in0=H.t[:], in1=Bbc[s].t[:], op=ALU.add),
                 reads=[H.r, Bbc[s].r], writes=[H.r])
            if DBG_CUT == 1:
                p.flush()
                return
            emit_transpose8(cx, H, HT, ptA, idn)
            for nb in range(4):
                c0, c1 = nb * 512, min(1952, (nb + 1) * 512)
                for kc in range(8):
                    p.op("pe", lambda e, HT=HT, kc=kc, c0=c0, c1=c1: e.matmul(
                        ppj.t[:, c0:c1], lhsT=HT.t[:, kc, :], rhs=w_in.t[:, kc, c0:c1], start=(kc == 0), stop=(kc == 7)),
                        reads=[HT.r, w_in.r], writes=[ppj.r])
            p.op("act", lambda e, PJ=PJ: e.activation(out=PJ.t[:, 0:1024], in_=ppj.t[:, 0:1024], func=AF.Copy),
                 reads=[ppj.r], writes=[PJ.r])
            p.op("dve", lambda e, PJ=PJ: e.tensor_copy(out=PJ.t[:, 1024:1952], in_=ppj.t[:, 1024:1952]),
                 reads=[ppj.r], writes=[PJ.r])
            if DBG_CUT == 2:
                p.flush()
                return
            p.op("act", lambda e, PJ=PJ, SM=SM: e.activation(out=junk.t[:, 0:256], in_=PJ.t[:, 0:256], func=AF.Square,
                                                             accum_out=SM.t[:, 2:3]), reads=[PJ.r], writes=[junk.r, SM.r])
            p.op("act", lambda e, PJ=PJ, SM=SM: e.activation(out=junk.t[:, 0:128], in_=PJ.t[:, 256:384], func=AF.Square,
                                                             accum_out=SM.t[:, 3:4]), reads=[PJ.r], writes=[junk.r, SM.r])
            emit_rstd(cx, (SM, SM.t[:, 2:3]), (SM, SM.t[:, 4:5]), 256.0)
            emit_rstd(cx, (SM, SM.t[:, 3:4]), (SM, SM.t[:, 5:6]), 128.0)
            p.op("dve", lambda e, PJ=PJ, CN=CN, SM=SM: e.scalar_tensor_tensor(
                out=CN.t[:, 0:256], in0=PJ.t[:, 0:256], scalar=SM.t[:, 4:5], in1=gq.t[:], op0=ALU.mult, op1=ALU.mult),
                reads=[PJ.r, SM.r, gq.r], writes=[CN.r])
            p.op("dve", lambda e, PJ=PJ, CN=CN, SM=SM: e.scalar_tensor_tensor(
                out=CN.t[:, 256:384], in0=PJ.t[:, 256:384], scalar=SM.t[:, 5:6], in1=gkv.t[:], op0=ALU.mult, op1=ALU.mult),
                reads=[PJ.r, SM.r, gkv.r], writes=[CN.r])
            for j in range(3):
                p.op("pe", lambda e, CN=CN, j=j: e.transpose(ptA.t[:, j * 128:(j + 1) * 128], CN.t[:, j * 128:(j + 1) * 128],
                                                             idn.t[:]), reads=[CN.r, idn.r], writes=[ptA.r])
            p.op("act", lambda e, CNT=CNT: e.activation(out=CNT.t[:].rearrange("p a b -> p (a b)"), in_=ptA.t[:, 0:384],
                                                        func=AF.Copy), reads=[ptA.r], writes=[CNT.r])
            if DBG_CUT == 3:
                p.flush()
                return
            for hb in range(2):
                p.op("pe", lambda e, CNT=CNT, hb=hb: e.matmul(pkv.t[:, hb * 512:(hb + 1) * 512], lhsT=CNT.t[:, 2, :],
                                                              rhs=w_ukv.t[:, hb * 512:(hb + 1) * 512], start=True, stop=True),
                     reads=[CNT.r, w_ukv.r], writes=[pkv.r])
            if DBG_CUT == 31:
                p.flush()
                return
            KT_, VM, VD, KP, TMP, DKR = Kt[b], Vm[b], Vd[b], kper[b], tmp[b], dkr[b]
            kvv = pkv.t[:].rearrange("p (h d) -> p h d", h=8)
            p.op("act", lambda e, KT_=KT_, kvv=kvv: e.activation(out=KT_.t[:, :, 0:64], in_=kvv[:, :, 0:64], func=AF.Copy),
                 reads=[pkv.r], writes=[KT_.r])
            if DBG_CUT == 32:
                p.flush()
                return
            p.op("act", lambda e, VM=VM, kvv=kvv: e.activation(out=VM.t[:, :, 0:64], in_=kvv[:, :, 64:128], func=AF.Copy),
                 reads=[pkv.r], writes=[VM.r])
            if DBG_CUT == 4:
                p.flush()
                return
            emit_rope(cx, "pool", PJ.t[:, 384:416].rearrange("p (h d) -> p h d", h=1), KP.t[:], TMP.t[:, :, 0:1, 0:16],
                      RP.t[:, 0:16], RP.t[:, 16:32], 1, 16, [PJ.r, RP.r], [KP.r, TMP.r])
            p.op("pool", lambda e, KT_=KT_, KP=KP: e.tensor_copy(out=KT_.t[:, :, 64:96], in_=KP.t[:, 0:1, :].to_broadcast([128, 8, 32])),
                 reads=[KP.r], writes=[KT_.r])
            if DBG_CUT == 5:
                p.flush()
                return
            emit_rope(cx, "dve", PJ.t[:, 928:1440].rearrange("p (h d) -> p h d", h=8), DKR.t[:], TMP.t[:],
                      RP.t[:, 32:64], RP.t[:, 64:96], 8, 32, [PJ.r, RP.r], [DKR.r, TMP.r])
            if DBG_CUT == 6:
                p.flush()
                return
            p.op("pool", lambda e, VD=VD, PJ=PJ: e.tensor_copy(out=VD.t[:, :, 0:128],
                                                               in_=PJ.t[:, 1440:1952].rearrange("p (h d) -> p h d", h=4)),
                 reads=[PJ.r], writes=[VD.r])
            if DBG_CUT == 7:
                p.flush()
                return
            for h in range(8):
                p.op("pe", lambda e, KT_=KT_, h=h: e.transpose(pkv.t[0:96, h * 128:(h + 1) * 128], KT_.t[:, h, :], idn.t[:]),
                     reads=[KT_.r, idn.r], writes=[pkv.r])
            KS, KDS = KTm_s[b], KTd_s[b]
            p.op("act", lambda e, KS=KS: e.activation(out=KS.t[:].rearrange("p a b -> p (a b)"), in_=pkv.t[0:96, :], func=AF.Copy),
                 reads=[pkv.r], writes=[KS.r])
            p.dma("sp", S["KTm"].t[:, :, i * 128:(i + 1) * 128], KS.t[:], reads=[KS.r], writes=[S["KTm"].r])
            for h in range(4):
                p.op("pe", lambda e, DKR=DKR, h=h: e.transpose(ptA.t[:, h * 128:(h + 1) * 128],
                                                               DKR.t[:, 2 * h:2 * h + 2, :].rearrange("p a b -> p (a b)"), idn.t[:]),
                     reads=[DKR.r, idn.r], writes=[ptA.r])
            p.op("dve", lambda e, KDS=KDS: e.tensor_copy(out=KDS.t[:].rearrange("p a b -> p (a b)"), in_=ptA.t[:, 0:512]),
                 reads=[ptA.r], writes=[KDS.r])
            p.dma("act", S["KTd"].t[:, :, i * 128:(i + 1) * 128], KDS.t[:], reads=[KDS.r], writes=[S["KTd"].r])
            p.dma("sp", S["Vm"].t[i * 128:(i + 1) * 128, :], VM.t[:].rearrange("p a b -> p (a b)"), reads=[VM.r], writes=[S["Vm"].r])
            p.dma("act", S["Vd"].t[i * 128:(i + 1) * 128, :], VD.t[:].rearrange("p a b -> p (a b)"), reads=[VD.r], writes=[S["Vd"].r])
            if DBG_CUT == 8:
                p.flush()
                return
            if not is_q:
                continue
            QR, QT_, DQR, QS, QDS = qraw[b], Qt[b], dqr[b], QTm_s[b], QTd_s[b]
            for (c0, c1) in ((0, 512), (512, 768)):
                for kc in range(2):
                    p.op("pe", lambda e, CNT=CNT, kc=kc, c0=c0, c1=c1: e.matmul(
                        pkv.t[:, c0:c1], lhsT=CNT.t[:, kc, :], rhs=w_uq.t[:, kc, c0:c1], start=(kc == 0), stop=(kc == 1)),
                        reads=[CNT.r, w_uq.r], writes=[pkv.r])
            p.op("act", lambda e, QR=QR: e.activation(out=QR.t[:].rearrange("p a b -> p (a b)"), in_=pkv.t[:, 0:768], func=AF.Copy),
                 reads=[pkv.r], writes=[QR.r])
            p.op("pool", lambda e, QR=QR, QT_=QT_: e.tensor_copy(out=QT_.t[:, :, 0:64], in_=QR.t[:, :, 0:64]),
                 reads=[QR.r], writes=[QT_.r])
            emit_rope(cx, "dve", QR.t[:, :, 64:96], QT_.t[:, :, 64:96], TMP.t[:, :, :, 0:16],
                      RP.t[:, 0:16], RP.t[:, 16:32], 8, 16, [QR.r, RP.r], [QT_.r, TMP.r])
            emit_rope(cx, "pool", PJ.t[:, 416:928].rearrange("p (h d) -> p h d", h=8), DQR.t[:], TMP.t[:],
                      RP.t[:, 32:64], RP.t[:, 64:96], 8, 32, [PJ.r, RP.r], [DQR.r, TMP.r])
            for h in range(8):
                p.op("pe", lambda e, QT_=QT_, h=h: e.transpose(pkv.t[0:96, h * 128:(h + 1) * 128], QT_.t[:, h, :], idn.t[:]),
                     reads=[QT_.r, idn.r], writes=[pkv.r])
            p.op("act", lambda e, QS=QS: e.activation(out=QS.t[:].rearrange("p a b -> p (a b)"), in_=pkv.t[0:96, :], func=AF.Copy),
                 reads=[pkv.r], writes=[QS.r])
            p.dma("sp", S["QTm"].t[:, :, i * 128:(i + 1) * 128], QS.t[:], reads=[QS.r], writes=[S["QTm"].r])
            for h in range(4):
                p.op("pe", lambda e, DQR=DQR, h=h: e.transpose(ptA.t[:, h * 128:(h + 1) * 128],
                                                               DQR.t[:, 2 * h:2 * h + 2, :].rearrange("p a b -> p (a b)"), idn.t[:]),
                     reads=[DQR.r, idn.r], writes=[ptA.r])
            p.op("dve", lambda e, QDS=QDS: e.tensor_copy(out=QDS.t[:].rearrange("p a b -> p (a b)"), in_=ptA.t[:, 0:512]),
                 reads=[ptA.r], writes=[QDS.r])
            p.dma("act", S["QTd"].t[:, :, i * 128:(i + 1) * 128], QDS.t[:], reads=[QDS.r], writes=[S["QTd"].r])
        p.flush()


def phase_attn_core(cx, xin, mod_d, S, W, x1_d):
    p, nc = cx.p, cx.nc
    with ExitStack() as st0:
        o_sb = cx.sb(st0, [128, NQ_T, 1024], BF16, "o_sb")
        rec = [cx.sb(st0, [128, 8], F32, "rec") for _ in range(4)]
        with ExitStack() as st:
            KTm = cx.sb(st, [96, 8, NK], BF16, "KTm")
            Vm = cx.sb(st, [128, NT_ALL, 528], BF16, "Vm")
            QTm = cx.sb(st, [96, 8, NQ], BF16, "QTm")
            for h in range(8):
                p.dma("sp" if h % 2 == 0 else "act", KTm.t[:, h, :], S["KTm"].t[:, h, :], reads=[S["KTm"].r], writes=[KTm.r])
            p.dma("sp", QTm.t[:], S["QTm"].t[:], reads=[S["QTm"].r], writes=[QTm.r])
            vv = S["Vm"].t.rearrange("(t p) f -> p t f", p=128)
            for t4 in range(0, NT_ALL, 6):
                t5 = min(NT_ALL, t4 + 6)
                p.dma("act", Vm.t[:, t4:t5, :], vv[:, t4:t5, :], reads=[S["Vm"].r], writes=[Vm.r])
            sc = [cx.ps(st, [128, 512], F32, "sc") for _ in range(2)]
            ops_ = [cx.ps(st, [128, 512], F32, "ops") for _ in range(4)]
            pT = [cx.sb(st, [128, 512], BF16, "pT") for _ in range(3)]
            k = 0
            for h in range(8):
                for g in range(5):
                    if g == 0:
                        q0, nq, kts = 0, 256, range(0, 2)
                    else:
                        q0, nq, kts = (2 + 4 * (g - 1)) * 128, 512, range(0, NT_ALL)
                    nj = nq // 128
                    kl = list(kts)
                    for kt in kl:
                        s_, pt_ = sc[k % 2], pT[k % 3]
                        k += 1
                        p.op("pe", lambda e, s_=s_, h=h, kt=kt, q0=q0, nq=nq: e.matmul(
                            s_.t[:, 0:nq], lhsT=KTm.t[:, h, kt * 128:(kt + 1) * 128], rhs=QTm.t[:, h, q0:q0 + nq],
                            start=True, stop=True), reads=[KTm.r, QTm.r], writes=[s_.r])
                        p.op("act", lambda e, s_=s_, pt_=pt_, nq=nq: e.activation(
                            out=pt_.t[:, 0:nq], in_=s_.t[:, 0:nq], func=AF.Exp, scale=MLA_SCALE),
                            reads=[s_.r], writes=[pt_.r])
                        for j in range(nj):
                            p.op("pe", lambda e, pt_=pt_, j=j, kt=kt, h=h, first=(kt == kl[0]), last=(kt == kl[-1]): e.matmul(
                                ops_[j].t[:, 0:66], lhsT=pt_.t[:, j * 128:(j + 1) * 128], rhs=Vm.t[:, kt, h * 66:(h + 1) * 66],
                                start=first, stop=last), reads=[pt_.r, Vm.r], writes=[ops_[j].r])
                    for j in range(nj):
                        ti = q0 // 128 + j
                        rc = rec[j]
                        p.op("dve", lambda e, j=j, rc=rc: e.reciprocal(out=rc.t[:, 0:1], in_=ops_[j].t[:, 64:65]),
                             reads=[ops_[j].r], writes=[rc.r])
                        p.op("dve", lambda e, j=j, rc=rc, ti=ti, h=h: e.tensor_scalar(
                            out=o_sb.t[:, ti, h * 64:(h + 1) * 64], in0=ops_[j].t[:, 0:64], scalar1=rc.t[:, 0:1], scalar2=None,
                            op0=ALU.mult), reads=[ops_[j].r, rc.r], writes=[o_sb.r])
            p.flush()
        if DBG_CUT == 101:
            return
        with ExitStack() as st:
            KTd = cx.sb(st, [128, 4, NK], BF16, "KTd")
            Vd = cx.sb(st, [128, NT_ALL, 520], BF16, "Vd")
            QTd = cx.sb(st, [128, 4, NQ], BF16, "QTd")
            for h in range(4):
                p.dma("sp" if h % 2 == 0 else "act", KTd.t[:, h, :], S["KTd"].t[:, h, :], reads=[S["KTd"].r], writes=[KTd.r])
            p.dma("sp", QTd.t[:], S["QTd"].t[:], reads=[S["QTd"].r], writes=[QTd.r])
            vv = S["Vd"].t.rearrange("(t p) f -> p t f", p=128)
            for t4 in range(0, NT_ALL, 6):
                t5 = min(NT_ALL, t4 + 6)
                p.dma("act", Vd.t[:, t4:t5, :], vv[:, t4:t5, :], reads=[S["Vd"].r], writes=[Vd.r])
            lrow = cx.sb(st, [1, 4, 64], F32, "lrow")
            lsm = cx.sb(st, [1, 8], F32, "lsm")
            ones = cx.sb(st, [1, 128], F32, "ones")
            lamn = cx.sb(st, [128, 1], F32, "lamn")
            subbc = cx.sb(st, [128, 128], F32, "subbc")
            for a, nm in enumerate(("lq1", "lk1", "lq2", "lk2")):
                p.dma("sp", lrow.t[0:1, a, :], W[nm].rearrange("(o n) -> o n", o=1), writes=[lrow.r])
            load_bc(cx, "sp", (subbc, subbc.t[:]), W["subln"], 128)
            p.op("dve", lambda e: e.tensor_scalar(out=subbc.t[:], in0=subbc.t[:], scalar1=(1.0 - LAMBDA_INIT0), scalar2=None,
                                                  op0=ALU.mult), reads=[subbc.r], writes=[subbc.r])
            p.op("dve", lambda e: e.memset(ones.t[:], 1.0), writes=[ones.r])
            p.op("dve", lambda e: e.tensor_tensor(out=lrow.t[0:1, 0, :], in0=lrow.t[0:1, 0, :], in1=lrow.t[0:1, 1, :], op=ALU.mult),
                 reads=[lrow.r], writes=[lrow.r])
            p.op("dve", lambda e: e.tensor_tensor(out=lrow.t[0:1, 2, :], in0=lrow.t[0:1, 2, :], in1=lrow.t[0:1, 3, :], op=ALU.mult),
                 reads=[lrow.r], writes=[lrow.r])
            p.op("dve", lambda e: e.tensor_reduce(out=lsm.t[0:1, 0:1], in_=lrow.t[0:1, 0, :], axis=AX.X, op=ALU.add),
                 reads=[lrow.r], writes=[lsm.r])
            p.op("dve", lambda e: e.tensor_reduce(out=lsm.t[0:1, 1:2], in_=lrow.t[0:1, 2, :], axis=AX.X, op=ALU.add),
                 reads=[lrow.r], writes=[lsm.r])
            p.op("act", lambda e: e.activation(out=lsm.t[0:1, 2:4], in_=lsm.t[0:1, 0:2], func=AF.Exp), reads=[lsm.r], writes=[lsm.r])
            p.op("dve", lambda e: e.tensor_tensor(out=lsm.t[0:1, 4:5], in0=lsm.t[0:1, 3:4], in1=lsm.t[0:1, 2:3], op=ALU.subtract),
                 reads=[lsm.r], writes=[lsm.r])
            p.op("dve", lambda e: e.tensor_scalar(out=lsm.t[0:1, 4:5], in0=lsm.t[0:1, 4:5], scalar1=-LAMBDA_INIT0, scalar2=None,
                                                  op0=ALU.add), reads=[lsm.r], writes=[lsm.r])
            sc = [cx.ps(st, [128, 512], F32, "scd") for _ in range(2)]
            ops_ = [[cx.ps(st, [128, 512], F32, "opd") for _ in range(2)] for _ in range(2)]
            pT = [cx.sb(st, [128, 256], BF16, "pTd") for _ in range(4)]
            dd = [cx.sb(st, [128, 128], F32, "dd") for _ in range(2)]
            tt = [cx.sb(st, [128, 128], F32, "tt") for _ in range(2)]
            junk = cx.sb(st, [128, 128], F32, "junkd")
            p.op("pe", lambda e: e.matmul(sc[0].t[:, 0:1], lhsT=ones.t[0:1, :], rhs=lsm.t[0:1, 4:5], start=True, stop=True),
                 reads=[ones.r, lsm.r], writes=[sc[0].r])
            p.op("dve", lambda e: e.tensor_copy(out=lamn.t[:], in_=sc[0].t[:, 0:1]), reads=[sc[0].r], writes=[lamn.r])
            k = 0
            for h in range(4):
                for g in range(9):
                    if g == 0:
                        q0, kts = 0, range(0, 2)
                    else:
                        q0, kts = (2 + 2 * (g - 1)) * 128, range(0, NT_ALL)
                    kl = list(kts)
                    for kt in kl:
                        for m in range(2):
                            s_, pt_ = sc[k % 2], pT[k % 4]
                            k += 1
                            p.op("pe", lambda e, s_=s_, h=h, kt=kt, q0=q0, m=m: e.matmul(
                                s_.t[:, 0:256], lhsT=KTd.t[m * 64:(m + 1) * 64, h, kt * 128:(kt + 1) * 128],
                                rhs=QTd.t[m * 64:(m + 1) * 64, h, q0:q0 + 256], start=True, stop=True),
                                reads=[KTd.r, QTd.r], writes=[s_.r])
                            p.op("act", lambda e, s_=s_, pt_=pt_: e.activation(
                                out=pt_.t[:, :], in_=s_.t[:, 0:256], func=AF.Exp, scale=DIFF_SCALE),
                                reads=[s_.r], writes=[pt_.r])
                            for j in range(2):
                                p.op("pe", lambda e, pt_=pt_, j=j, kt=kt, h=h, m=m, first=(kt == kl[0]), last=(kt == kl[-1]): e.matmul(
                                    ops_[m][j].t[:, 0:130], lhsT=pt_.t[:, j * 128:(j + 1) * 128], rhs=Vd.t[:, kt, h * 130:(h + 1) * 130],
                                    start=first, stop=last), reads=[pt_.r, Vd.r], writes=[ops_[m][j].r])
                    for j in range(2):
                        ti = q0 // 128 + j
                        rc, d_, t_ = rec[j], dd[j], tt[j]
                        o1, o2 = ops_[0][j], ops_[1][j]
                        p.op("dve", lambda e, rc=rc, o1=o1: e.reciprocal(out=rc.t[:, 0:1], in_=o1.t[:, 128:129]),
                             reads=[o1.r], writes=[rc.r])
                        p.op("dve", lambda e, rc=rc, o2=o2: e.reciprocal(out=rc.t[:, 1:2], in_=o2.t[:, 128:129]),
                             reads=[o2.r], writes=[rc.r])
                        p.op("dve", lambda e, rc=rc: e.tensor_tensor(out=rc.t[:, 1:2], in0=rc.t[:, 1:2], in1=lamn.t[:, 0:1], op=ALU.mult),
                             reads=[rc.r, lamn.r], writes=[rc.r])
                        p.op("dve", lambda e, rc=rc, o2=o2, t_=t_: e.tensor_scalar(out=t_.t[:], in0=o2.t[:, 0:128], scalar1=rc.t[:, 1:2],
                                                                              scalar2=None, op0=ALU.mult),
                             reads=[o2.r, rc.r], writes=[t_.r])
                        p.op("dve", lambda e, rc=rc, o1=o1, t_=t_, d_=d_: e.scalar_tensor_tensor(
                            out=d_.t[:], in0=o1.t[:, 0:128], scalar=rc.t[:, 0:1], in1=t_.t[:], op0=ALU.mult, op1=ALU.add),
                            reads=[o1.r, rc.r, t_.r], writes=[d_.r])
                        p.op("act", lambda e, d_=d_, rc=rc: e.activation(out=junk.t[:], in_=d_.t[:], func=AF.Square, accum_out=rc.t[:, 2:3]),
                             reads=[d_.r], writes=[junk.r, rc.r])
                        emit_rstd(cx, (rc, rc.t[:, 2:3]), (rc, rc.t[:, 3:4]), 128.0)
                        p.op("dve", lambda e, d_=d_, rc=rc, ti=ti, h=h: e.scalar_tensor_tensor(
                            out=o_sb.t[:, ti, 512 + h * 128:512 + (h + 1) * 128], in0=d_.t[:], scalar=rc.t[:, 3:4], in1=subbc.t[:],
                            op0=ALU.mult, op1=ALU.mult), reads=[d_.r, rc.r, subbc.r], writes=[o_sb.r])
            p.flush()
        if DBG_CUT == 102:
            return
        with ExitStack() as st:
            idf = make_ident(cx, st, F32)
            idb = cx.sb(st, [128, 128], BF16, "idb")
            p.op("dve", lambda e: e.tensor_copy(out=idb.t[:], in_=idf.t[:]), reads=[idf.r], writes=[idb.r])
            w_out = cx.sb(st, [128, 8, 1024], BF16, "w_out")
            wov = W["w_out"].rearrange("(kc p) n -> p kc n", p=128)
            for kc in range(8):
                p.dma("pool", w_out.t[:, kc, :], wov[:, kc, :], writes=[w_out.r])
            g1 = [cx.sb(st, [128, 1024], F32, "g1") for _ in range(2)]
            for s in range(2):
                load_bc(cx, "sp", (g1[s], g1[s].t[:]), mod_d.t[s, 2048:3072], 1024, src=mod_d)
            ptr = [cx.ps(st, [128, 1024], BF16, "ptr") for _ in range(2)]
            py = [cx.ps(st, [128, 1024], F32, "py") for _ in range(2)]
            oT = [cx.sb(st, [128, 8, 128], BF16, "oT") for _ in range(2)]
            x_t = [cx.sb(st, [128, 1024], F32, "x_t2") for _ in range(2)]
            y_t = [cx.sb(st, [128, 1024], F32, "y_t") for _ in range(2)]
            for i in range(NQ_T):
                b = i % 2
                s = 1 if i < 2 else 0
                X, Y, OT, PT, PY = x_t[b], y_t[b], oT[b], ptr[b], py[b]
                p.dma("sp", X.t[:], xin[i * 128:(i + 1) * 128, :], writes=[X.r])
                for kc in range(8):
                    p.op("pe", lambda e, PT=PT, i=i, kc=kc: e.transpose(PT.t[:, kc * 128:(kc + 1) * 128],
                                                                      o_sb.t[:, i, kc * 128:(kc + 1) * 128], idb.t[:]),
                         reads=[o_sb.r, idb.r], writes=[PT.r])
                p.op("act", lambda e, PT=PT, OT=OT: e.activation(out=OT.t[:].rearrange("p a b -> p (a b)"), in_=PT.t[:], func=AF.Copy),
                     reads=[PT.r], writes=[OT.r])
                for hb in range(2):
                    for kc in range(8):
                        p.op("pe", lambda e, PY=PY, OT=OT, hb=hb, kc=kc: e.matmul(
                            PY.t[:, hb * 512:(hb + 1) * 512], lhsT=OT.t[:, kc, :], rhs=w_out.t[:, kc, hb * 512:(hb + 1) * 512],
                            start=(kc == 0), stop=(kc == 7)), reads=[OT.r, w_out.r], writes=[PY.r])
                p.op("dve", lambda e, PY=PY, Y=Y, s=s: e.tensor_tensor(out=Y.t[:], in0=PY.t[:], in1=g1[s].t[:], op=ALU.mult),
                     reads=[PY.r, g1[s].r], writes=[Y.r])
                p.op("pool", lambda e, X=X, Y=Y: e.tensor_tensor(out=Y.t[:], in0=Y.t[:], in1=X.t[:], op=ALU.add),
                     reads=[X.r, Y.r], writes=[Y.r])
                p.dma("act", x1_d.t[i * 128:(i + 1) * 128, :], Y.t[:], reads=[Y.r], writes=[x1_d.r])
            p.flush()


ATT_W = (("w_in", [1024, 1952]), ("q_norm", [256]), ("w_uq", [256, 768]), ("kv_norm", [128]), ("w_ukv", [128, 1024]),
         ("lq1", [64]), ("lk1", [64]), ("lq2", [64]), ("lk2", [64]), ("subln", [128]), ("w_out", [1024, 1024]))
MOE_W = (("w_rg", [1024, 4]), ("b_rg", [4]), ("w_rf", [1024, 32]), ("b_rf", [32]),
         ("w_gate", [32, 1024, 512]), ("w_up", [32, 1024, 512]), ("w_down", [32, 512, 1024]))


def build_A(stop_after=None):
    nc = bass.Bass("TRN2", target_bir_lowering=False)
    inp = lambda name, shape: nc.dram_tensor(name, list(shape), F32, kind="ExternalInput").ap()
    xin = inp("xin", [NK, 1024])
    cvec = inp("cvec", [2, 1024])
    rope = inp("rope", [NK, 96])
    w_mod = inp("w_mod", [1024, 6144])
    b_mod = inp("b_mod", [6144])
    norm_g = inp("norm_g", [2, 1024])
    W = {k: inp("att_" + k, s) for k, s in ATT_W}
    M = {k: inp("moe_" + k, s) for k, s in MOE_W}
    xout = nc.dram_tensor("xout", [NQ, 1024], F32, kind="ExternalOutput").ap()
    with ExitStack() as st:
        cx = Ctx(nc, st)
        mod_d = cx.dram([2, 6144], F32, "mod_d")
        S = {"KTm": cx.dram([96, 8, NK], BF16, "KTm_d"), "QTm": cx.dram([96, 8, NQ], BF16, "QTm_d"),
             "KTd": cx.dram([128, 4, NK], BF16, "KTd_d"), "QTd": cx.dram([128, 4, NQ], BF16, "QTd_d"),
             "Vm": cx.dram([NK, 528], BF16, "Vm_d"), "Vd": cx.dram([NK, 520], BF16, "Vd_d")}
        phase_mod(cx, cvec, w_mod, b_mod, mod_d)
        if stop_after == "mod":
            cx.p.dma("sp", xout[0:12, :].rearrange("(s a) d -> s (a d)", s=2), mod_d.t[:, :], reads=[mod_d.r])
            cx.p.flush()
            return nc
        phase_attn_prep(cx, xin, rope, mod_d, norm_g[0, :], W, S)
        if stop_after == "prep":
            cx.p.dma("sp", xout[0:12, :].rearrange("(s a) d -> s (a d)", s=2), mod_d.t[:, :], reads=[mod_d.r, S["Vd"].r, S["QTd"].r])
            cx.p.flush()
            return nc
        if stop_after == "attn":
            x1_d = TL(xout)
            phase_attn_core(cx, xin, mod_d, S, W, x1_d)
            return nc
        x1_d = cx.dram([NQ, 1024], F32, "x1_d")
        phase_attn_core(cx, xin, mod_d, S, W, x1_d)
        phase_moe(cx, x1_d, NQ_T, mod_d, norm_g[1, :], M, TL(xout), n_ctx_tiles=2)
    return nc


def rope_tables():
    rows = 64
    row = np.repeat(np.arange(rows, dtype=np.float32), 64)
    col = np.tile(np.arange(64, dtype=np.float32), rows)
    out = []
    for rot in (32, 64):
        nf = rot // 4
        inv = (np.float32(10000.0) ** (-np.arange(nf, dtype=np.float32) / np.float32(nf))).astype(np.float32)
        ang = np.concatenate([row[:, None] * inv, col[:, None] * inv], axis=-1).astype(np.float32)
        out += [np.cos(ang), np.sin(ang)]
    return np.concatenate(out, axis=-1).astype(np.float32)


def inputs_A(core, x, c, ctx, c_ctx, w_mod, b_mod, norm_g, att, moe):
    b, hf = core // 2, core % 2
    own = x[b, hf * 2048:(hf + 1) * 2048]
    oth = x[b, (1 - hf) * 2048:(2 - hf) * 2048]
    rt = rope_tables()
    rc = np.zeros((256, 96), np.float32)
    rc[:, 0:16] = 1.0
    rc[:, 32:64] = 1.0
    d = {"xin": np.ascontiguousarray(np.concatenate([ctx[b], own, oth], axis=0)),
         "cvec": np.ascontiguousarray(np.stack([c[b], c_ctx], axis=0)),
         "rope": np.ascontiguousarray(np.concatenate([rc, rt[hf * 2048:(hf + 1) * 2048], rt[(1 - hf) * 2048:(2 - hf) * 2048]], axis=0)),
         "w_mod": w_mod[0], "b_mod": b_mod[0], "norm_g": norm_g[0]}
    for k, s in ATT_W:
        d["att_" + k] = att[k][0]
    for k, s in MOE_W:
        d["moe_" + k] = np.ascontiguousarray(moe[k][0].reshape(s))
    return d


def emit_router_fine(cx, RT, R_, comb, i):
    p = cx.p
    lf = RT.t[:, 0, 4:36].rearrange("p (g e) -> p g e", g=4)
    mk1 = RT.t[:, 3, 0:32].rearrange("p (g e) -> p g e", g=4)
    lf2 = RT.t[:, 4, 0:32].rearrange("p (g e) -> p g e", g=4)
    mk2 = RT.t[:, 5, 0:32].rearrange("p (g e) -> p g e", g=4)
    fine = RT.t[:, 6, 0:32].rearrange("p (g e) -> p g e", g=4)
    m1, m2, w1, w2 = RT.t[:, 7, 0:4], RT.t[:, 7, 4:8], RT.t[:, 7, 8:12], RT.t[:, 7, 12:16]
    bc = lambda a: a.unsqueeze(2).to_broadcast([128, 4, 8])
    p.op("dve", lambda e: e.tensor_reduce(out=m1, in_=lf, axis=AX.X, op=ALU.max), reads=R_, writes=R_)
    p.op("dve", lambda e: e.tensor_tensor(out=mk1, in0=lf, in1=bc(m1), op=ALU.is_equal), reads=R_, writes=R_)
    p.op("dve", lambda e: e.scalar_tensor_tensor(out=lf2, in0=mk1, scalar=-1e30, in1=lf, op0=ALU.mult, op1=ALU.add),
         reads=R_, writes=R_)
    p.op("dve", lambda e: e.tensor_reduce(out=m2, in_=lf2, axis=AX.X, op=ALU.max), reads=R_, writes=R_)
    p.op("dve", lambda e: e.tensor_tensor(out=mk2, in0=lf2, in1=bc(m2), op=ALU.is_equal), reads=R_, writes=R_)
    p.op("dve", lambda e: e.tensor_tensor(out=w2, in0=m2, in1=m1, op=ALU.subtract), reads=R_, writes=R_)
    p.op("act", lambda e: e.activation(out=w2, in_=w2, func=AF.Exp), reads=R_, writes=R_)
    p.op("dve", lambda e: e.tensor_scalar(out=w1, in0=w2, scalar1=1.0, scalar2=None, op0=ALU.add), reads=R_, writes=R_)
    p.op("dve", lambda e: e.reciprocal(out=w1, in_=w1), reads=R_, writes=R_)
    p.op("dve", lambda e: e.tensor_tensor(out=w2, in0=w2, in1=w1, op=ALU.mult), reads=R_, writes=R_)
    p.op("dve", lambda e, RT=RT: e.tensor_tensor(out=w1, in0=w1, in1=RT.t[:, 2, 0:4], op=ALU.mult), reads=R_, writes=R_)
    p.op("dve", lambda e, RT=RT: e.tensor_tensor(out=w2, in0=w2, in1=RT.t[:, 2, 0:4], op=ALU.mult), reads=R_, writes=R_)
    p.op("dve", lambda e: e.tensor_tensor(out=mk1, in0=mk1, in1=bc(w1), op=ALU.mult), reads=R_, writes=R_)
    p.op("dve", lambda e: e.tensor_tensor(out=mk2, in0=mk2, in1=bc(w2), op=ALU.mult), reads=R_, writes=R_)
    p.op("dve", lambda e, i=i, RT=RT: e.tensor_tensor(out=comb.t[:, i, :], in0=RT.t[:, 3, 0:32], in1=RT.t[:, 5, 0:32], op=ALU.add),
         reads=R_, writes=[comb.r])


def phase_moe(cx, x_d, ntiles, mod_d, g_norm, M, out_d, n_ctx_tiles):
    p, nc = cx.p, cx.nc
    T = ntiles * 128
    with ExitStack() as st0:
        hT = cx.sb(st0, [128, 8, T], BF16, "hT_all")
        comb = cx.sb(st0, [128, ntiles, 32], F32, "comb")
        acc = cx.sb(st0, [128, ntiles, 1024], F32, "acc")
        with ExitStack() as st:
            idn = make_ident(cx, st, F32)
            gbc = cx.sb(st, [128, 1024], F32, "gbc2")
            nsrc = 2 if n_ctx_tiles > 0 else 1
            Abc = [cx.sb(st, [128, 1024], F32, "Abc2") for _ in range(nsrc)]
            Bbc = [cx.sb(st, [128, 1024], F32, "Bbc2") for _ in range(nsrc)]
            load_bc(cx, "sp", (gbc, gbc.t[:]), g_norm, 1024)
            for s in range(nsrc):
                load_bc(cx, "sp", (Bbc[s], Bbc[s].t[:]), mod_d.t[s, 3072:4096], 1024, src=mod_d)
                load_bc(cx, "act", (Abc[s], Abc[s].t[:]), mod_d.t[s, 4096:5120], 1024, src=mod_d)
                p.op("dve", lambda e, s=s: e.scalar_tensor_tensor(out=Abc[s].t[:], in0=Abc[s].t[:], scalar=1.0, in1=gbc.t[:],
                                                                  op0=ALU.add, op1=ALU.mult),
                     reads=[Abc[s].r, gbc.r], writes=[Abc[s].r])
            wr = cx.sb(st, [128, 8, 36], F32, "wr")
            brbc = cx.sb(st, [128, 36], F32, "brbc")
            p.dma("sp", wr.t[:, :, 0:4], M["w_rg"].rearrange("(kc p) n -> p kc n", p=128), writes=[wr.r])
            p.dma("sp", wr.t[:, :, 4:36], M["w_rf"].rearrange("(kc p) n -> p kc n", p=128), writes=[wr.r])
            load_bc(cx, "act", (brbc, brbc.t[:, 0:4]), M["b_rg"], 4)
            load_bc(cx, "act", (brbc, brbc.t[:, 4:36]), M["b_rf"], 32)
            x_t = [cx.sb(st, [128, 1024], F32, "x_t3") for _ in range(2)]
            h_t = [cx.sb(st, [128, 1024], F32, "h_t3") for _ in range(2)]
            junk = cx.sb(st, [128, 1024], F32, "junk3")
            hT32 = [cx.sb(st, [128, 8, 128], F32, "hT32") for _ in range(2)]
            sm = [cx.sb(st, [128, 8], F32, "sm3") for _ in range(2)]
            rt = [cx.sb(st, [128, 8, 36], F32, "rt3") for _ in range(2)]
            ptA = cx.ps(st, [128, 1024], F32, "ptA3")
            plg = cx.ps(st, [128, 512], F32, "plg")
            for i in range(ntiles):
                b = i % 2
                s = 1 if i < n_ctx_tiles else 0
                X, H, SM, H32, RT = x_t[b], h_t[b], sm[b], hT32[b], rt[b]
                if DBG_CUT == 210:
                    p.flush()
                    return
                p.dma("sp", X.t[:], x_d.t[i * 128:(i + 1) * 128, :], reads=[x_d.r], writes=[X.r])
                p.op("act", lambda e, X=X, SM=SM: e.activation(out=junk.t[:], in_=X.t[:], func=AF.Square, accum_out=SM.t[:, 0:1]),
                     reads=[X.r], writes=[junk.r, SM.r])
                emit_rstd(cx, (SM, SM.t[:, 0:1]), (SM, SM.t[:, 1:2]), 1024.0)
                p.op("dve", lambda e, X=X, H=H, SM=SM, s=s: e.scalar_tensor_tensor(
                    out=H.t[:], in0=X.t[:], scalar=SM.t[:, 1:2], in1=Abc[s].t[:], op0=ALU.mult, op1=ALU.mult),
                    reads=[X.r, SM.r, Abc[s].r], writes=[H.r])
                p.op("pool", lambda e, H=H, s=s: e.tensor_tensor(out=H.t[:], in0=H.t[:], in1=Bbc[s].t[:], op=ALU.add),
                     reads=[H.r, Bbc[s].r], writes=[H.r])
                for kc in range(8):
                    p.op("pe", lambda e, H=H, kc=kc: e.transpose(ptA.t[:, kc * 128:(kc + 1) * 128], H.t[:, kc * 128:(kc + 1) * 128],
                                                                 idn.t[:]), reads=[H.r, idn.r], writes=[ptA.r])
                p.op("act", lambda e, i=i: e.activation(out=hT.t[:, :, i * 128:(i + 1) * 128],
                                                        in_=ptA.t[:].rearrange("p (a b) -> p a b", a=8), func=AF.Copy),
                     reads=[ptA.r], writes=[hT.r])
                if DBG_CUT == 2105:
                    p.flush()
                    return
                p.op("dve", lambda e, H32=H32: e.tensor_copy(out=H32.t[:].rearrange("p a b -> p (a b)"), in_=ptA.t[:]),
                     reads=[ptA.r, hT.r], writes=[H32.r])
                if DBG_CUT == 211:
                    p.flush()
                    return
                for kc in range(8):
                    p.op("pe", lambda e, H32=H32, kc=kc: e.matmul(plg.t[:, 0:36], lhsT=H32.t[:, kc, :], rhs=wr.t[:, kc, :],
                                                                 start=(kc == 0), stop=(kc == 7)), reads=[H32.r, wr.r], writes=[plg.r])
                R_ = [RT.r]
                p.op("dve", lambda e, RT=RT: e.tensor_tensor(out=RT.t[:, 0, :], in0=plg.t[:, 0:36], in1=brbc.t[:], op=ALU.add),
                     reads=[plg.r, brbc.r], writes=R_)
                if DBG_CUT == 212:
                    p.flush()
                    return
                p.op("dve", lambda e, RT=RT, SM=SM: e.tensor_reduce(out=SM.t[:, 2:3], in_=RT.t[:, 0, 0:4], axis=AX.X, op=ALU.max),
                     reads=R_, writes=[SM.r])
                p.op("dve", lambda e, RT=RT, SM=SM: e.tensor_scalar(out=SM.t[:, 3:4], in0=SM.t[:, 2:3], scalar1=-1.0, scalar2=None,
                                                                    op0=ALU.mult), reads=[SM.r], writes=[SM.r])
                p.op("act", lambda e, RT=RT, SM=SM: e.activation(out=RT.t[:, 1, 0:4], in_=RT.t[:, 0, 0:4], func=AF.Exp,
                                                                 bias=SM.t[:, 3:4], scale=1.0, accum_out=SM.t[:, 4:5]),
                     reads=R_ + [SM.r], writes=R_ + [SM.r])
                p.op("dve", lambda e, SM=SM: e.reciprocal(out=SM.t[:, 5:6], in_=SM.t[:, 4:5]), reads=[SM.r], writes=[SM.r])
                p.op("dve", lambda e, RT=RT, SM=SM: e.tensor_scalar(out=RT.t[:, 2, 0:4], in0=RT.t[:, 0, 0:4], scalar1=SM.t[:, 2:3],
                                                                    scalar2=SM.t[:, 5:6], op0=ALU.is_equal, op1=ALU.mult),
                     reads=R_ + [SM.r], writes=R_)
                if DBG_CUT == 213:
                    p.flush()
                    return
                emit_router_fine(cx, RT, R_, comb, i)
            p.flush()
        if DBG_CUT == 201:
            return
        with ExitStack() as st:
            wg = [cx.sb(st, [128, 8, 512], BF16, "wg") for _ in range(2)]
            wu = [cx.sb(st, [128, 8, 512], BF16, "wu") for _ in range(2)]
            wd = [cx.sb(st, [128, 4, 1024], BF16, "wd") for _ in range(2)]
            actT = cx.sb(st, [128, 4, T], BF16, "actT")
            sg = [cx.sb(st, [128, 512], F32, "sg") for _ in range(2)]
            pg = [cx.ps(st, [128, 512], F32, "pg") for _ in range(2)]
            pu = [cx.ps(st, [128, 512], F32, "pu") for _ in range(2)]
            pd = [cx.ps(st, [128, 512], F32, "pd") for _ in range(2)]
            groups = [(t0, min(512, T - t0)) for t0 in range(0, T, 512)]
            k = 0
            kd = 0
            for ex in range(32):
                b = ex % 2
                WG, WU, WD = wg[b], wu[b], wd[b]
                p.dma("pool", WG.t[:], M["w_gate"][ex].rearrange("(kc p) n -> p kc n", p=128), writes=[WG.r])
                p.dma("pool", WU.t[:], M["w_up"][ex].rearrange("(kc p) n -> p kc n", p=128), writes=[WU.r])
                p.dma("pool", WD.t[:], M["w_down"][ex].rearrange("(fc p) n -> p fc n", p=128), writes=[WD.r])
                for fc in range(4):
                    for (t0, n) in groups:
                        PG, PU, SG = pg[k % 2], pu[k % 2], sg[k % 2]
                        k += 1
                        for kc in range(8):
                            p.op("pe", lambda e, PG=PG, WG=WG, kc=kc, fc=fc, t0=t0, n=n: e.matmul(
                                PG.t[:, 0:n], lhsT=WG.t[:, kc, fc * 128:(fc + 1) * 128], rhs=hT.t[:, kc, t0:t0 + n],
                                start=(kc == 0), stop=(kc == 7)), reads=[WG.r, hT.r], writes=[PG.r])
                        for kc in range(8):
                            p.op("pe", lambda e, PU=PU, WU=WU, kc=kc, fc=fc, t0=t0, n=n: e.matmul(
                                PU.t[:, 0:n], lhsT=WU.t[:, kc, fc * 128:(fc + 1) * 128], rhs=hT.t[:, kc, t0:t0 + n],
                                start=(kc == 0), stop=(kc == 7)), reads=[WU.r, hT.r], writes=[PU.r])
                        p.op("act", lambda e, PG=PG, SG=SG, n=n: e.activation(out=SG.t[:, 0:n], in_=PG.t[:, 0:n], func=AF.Silu),
                             reads=[PG.r], writes=[SG.r])
                        p.op("dve", lambda e, PU=PU, SG=SG, fc=fc, t0=t0, n=n: e.tensor_tensor(
                            out=actT.t[:, fc, t0:t0 + n], in0=SG.t[:, 0:n], in1=PU.t[:, 0:n], op=ALU.mult),
                            reads=[SG.r, PU.r], writes=[actT.r])
                for t in range(ntiles):
                    for hb in range(2):
                        PD = pd[kd % 2]
                        kd += 1
                        for fc in range(4):
                            p.op("pe", lambda e, PD=PD, WD=WD, fc=fc, t=t, hb=hb: e.matmul(
                                PD.t[:], lhsT=actT.t[:, fc, t * 128:(t + 1) * 128], rhs=WD.t[:, fc, hb * 512:(hb + 1) * 512],
                                start=(fc == 0), stop=(fc == 3)), reads=[actT.r, WD.r], writes=[PD.r])
                        if ex == 0:
                            p.op("dve", lambda e, PD=PD, t=t, hb=hb, ex=ex: e.tensor_scalar(
                                out=acc.t[:, t, hb * 512:(hb + 1) * 512], in0=PD.t[:], scalar1=comb.t[:, t, ex:ex + 1], scalar2=None,
                                op0=ALU.mult), reads=[PD.r, comb.r], writes=[acc.r])
                        else:
                            p.op("dve", lambda e, PD=PD, t=t, hb=hb, ex=ex: e.scalar_tensor_tensor(
                                out=acc.t[:, t, hb * 512:(hb + 1) * 512], in0=PD.t[:], scalar=comb.t[:, t, ex:ex + 1],
                                in1=acc.t[:, t, hb * 512:(hb + 1) * 512], op0=ALU.mult, op1=ALU.add),
                                reads=[PD.r, comb.r, acc.r], writes=[acc.r])
            p.flush()
        if DBG_CUT == 202:
            return
        with ExitStack() as st:
            nsrc = 2 if n_ctx_tiles > 0 else 1
            g2 = [cx.sb(st, [128, 1024], F32, "g2") for _ in range(nsrc)]
            for s in range(nsrc):
                load_bc(cx, "sp", (g2[s], g2[s].t[:]), mod_d.t[s, 5120:6144], 1024, src=mod_d)
            x_t = [cx.sb(st, [128, 1024], F32, "x_t4") for _ in range(2)]
            for i in range(ntiles):
                X = x_t[i % 2]
                s = 1 if i < n_ctx_tiles else 0
                p.dma("sp", X.t[:], x_d.t[i * 128:(i + 1) * 128, :], reads=[x_d.r], writes=[X.r])
                p.op("dve", lambda e, i=i, s=s: e.tensor_tensor(out=acc.t[:, i, :], in0=acc.t[:, i, :], in1=g2[s].t[:], op=ALU.mult),
                     reads=[acc.r, g2[s].r], writes=[acc.r])
                p.op("pool", lambda e, i=i, X=X: e.tensor_tensor(out=X.t[:], in0=X.t[:], in1=acc.t[:, i, :], op=ALU.add),
                     reads=[acc.r, X.r], writes=[X.r])
                p.dma("act", out_d.t[i * 128:(i + 1) * 128, :], X.t[:], reads=[X.r], writes=[out_d.r])
            p.flush()


NCH = 34


def tri_const(cx, st, kind):
    t = cx.sb(st, [128, 128], F32, "tri")
    pat, cm, op = {"p_le_j": ([[1, 128]], -1, ALU.is_ge), "p_ge_j": ([[-1, 128]], 1, ALU.is_ge),
                   "p_gt_j": ([[-1, 128]], 1, ALU.is_gt), "p_lt_j": ([[1, 128]], -1, ALU.is_gt)}[kind]
    cx.p.op("pool", lambda e: e.memset(t.t[:], 1.0), writes=[t.r])
    cx.p.op("pool", lambda e: e.affine_select(out=t.t[:], in_=t.t[:], pattern=pat, compare_op=op, fill=0.0, base=0,
                                              channel_multiplier=cm), reads=[t.r], writes=[t.r])
    return t


def phase_ssd_prep(cx, xin, mod_d, g_norm, Wd, S):
    p, nc = cx.p, cx.nc
    w_in = Wd["w_in"]
    with ExitStack() as st0:
        hT = cx.sb(st0, [128, 8, NK], BF16, "hT_ssd")
        idn = make_ident(cx, st0, F32)
        idb = cx.sb(st0, [128, 128], BF16, "idb")
        p.op("dve", lambda e: e.tensor_copy(out=idb.t[:], in_=idn.t[:]), reads=[idn.r], writes=[idb.r])
        with ExitStack() as st:
            gbc = cx.sb(st, [128, 1024], F32, "gbc5")
            Abc = [cx.sb(st, [128, 1024], F32, "Abc5") for _ in range(2)]
            Bbc = [cx.sb(st, [128, 1024], F32, "Bbc5") for _ in range(2)]
            load_bc(cx, "sp", (gbc, gbc.t[:]), g_norm, 1024)
            for s in range(2):
                load_bc(cx, "sp", (Bbc[s], Bbc[s].t[:]), mod_d.t[s, 0:1024], 1024, src=mod_d)
                load_bc(cx, "act", (Abc[s], Abc[s].t[:]), mod_d.t[s, 1024:2048], 1024, src=mod_d)
                p.op("dve", lambda e, s=s: e.scalar_tensor_tensor(out=Abc[s].t[:], in0=Abc[s].t[:], scalar=1.0, in1=gbc.t[:],
                                                                  op0=ALU.add, op1=ALU.mult),
                     reads=[Abc[s].r, gbc.r], writes=[Abc[s].r])
            w_dt = cx.sb(st, [128, 8, 64], BF16, "w_dt")
            p.dma("pool", w_dt.t[:], w_in.rearrange("(kc p) n -> p kc n", p=128)[:, :, 5120:5184], writes=[w_dt.r])
            dtb = cx.sb(st, [128, 64], F32, "dtb")
            load_bc(cx, "sp", (dtb, dtb.t[:]), Wd["dt_bias"], 64)
            x_t = [cx.sb(st, [128, 1024], F32, "x_t5") for _ in range(2)]
            h_t = [cx.sb(st, [128, 1024], F32, "h_t5") for _ in range(2)]
            junk = cx.sb(st, [128, 1024], F32, "junk5")
            sm = [cx.sb(st, [128, 8], F32, "sm5") for _ in range(2)]
            dts = [cx.sb(st, [128, 64], F32, "dts") for _ in range(2)]
            ptA = cx.ps(st, [128, 1024], F32, "ptA5")
            pdt = cx.ps(st, [128, 512], F32, "pdt")
            for i in range(NCH):
                b = i % 2
                s = 1 if i < 2 else 0
                X, H, SM, DT = x_t[b], h_t[b], sm[b], dts[b]
                p.dma("sp", X.t[:], xin[i * 128:(i + 1) * 128, :], writes=[X.r])
                p.op("act", lambda e, X=X, SM=SM: e.activation(out=junk.t[:], in_=X.t[:], func=AF.Square, accum_out=SM.t[:, 0:1]),
                     reads=[X.r], writes=[junk.r, SM.r])
                emit_rstd(cx, (SM, SM.t[:, 0:1]), (SM, SM.t[:, 1:2]), 1024.0)
                p.op("dve", lambda e, X=X, H=H, SM=SM, s=s: e.scalar_tensor_tensor(
                    out=H.t[:], in0=X.t[:], scalar=SM.t[:, 1:2], in1=Abc[s].t[:], op0=ALU.mult, op1=ALU.mult),
                    reads=[X.r, SM.r, Abc[s].r], writes=[H.r])
                p.op("pool", lambda e, H=H, s=s: e.tensor_tensor(out=H.t[:], in0=H.t[:], in1=Bbc[s].t[:], op=ALU.add),
                     reads=[H.r, Bbc[s].r], writes=[H.r])
                for kc in range(8):
                    p.op("pe", lambda e, H=H, kc=kc: e.transpose(ptA.t[:, kc * 128:(kc + 1) * 128], H.t[:, kc * 128:(kc + 1) * 128],
                                                                 idn.t[:]), reads=[H.r, idn.r], writes=[ptA.r])
                p.op("act", lambda e, i=i: e.activation(out=hT.t[:, :, i * 128:(i + 1) * 128],
                                                        in_=ptA.t[:].rearrange("p (a b) -> p a b", a=8), func=AF.Copy),
                     reads=[ptA.r], writes=[hT.r])
                for kc in range(8):
                    p.op("pe", lambda e, i=i, kc=kc: e.matmul(pdt.t[:, 0:64], lhsT=hT.t[:, kc, i * 128:(i + 1) * 128], rhs=w_dt.t[:, kc, :],
                                                              start=(kc == 0), stop=(kc == 7)), reads=[hT.r, w_dt.r], writes=[pdt.r])
                p.op("dve", lambda e, DT=DT: e.tensor_tensor(out=DT.t[:], in0=pdt.t[:, 0:64], in1=dtb.t[:], op=ALU.add),
                     reads=[pdt.r, dtb.r], writes=[DT.r])
                p.op("act", lambda e, DT=DT: e.activation(out=DT.t[:], in_=DT.t[:], func=AF.Exp), reads=[DT.r], writes=[DT.r])
                p.op("act", lambda e, DT=DT: e.activation(out=DT.t[:], in_=DT.t[:], func=AF.Ln, bias=1.0, scale=1.0),
                     reads=[DT.r], writes=[DT.r])
                p.dma("act", S["dt"].t[i * 128:(i + 1) * 128, :], DT.t[:], reads=[DT.r], writes=[S["dt"].r])
            p.flush()
        with ExitStack() as st:
            w_x = cx.sb(st, [128, 8, 3072], BF16, "w_x")
            wv = w_in.rearrange("(kc p) n -> p kc n", p=128)
            for kc in range(8):
                p.dma("pool", w_x.t[:, kc, :], wv[:, kc, 2048:5120], writes=[w_x.r])
            cwr = cx.sb(st, [6, 3072], F32, "cwr")
            cw = cx.sb(st, [128, 24, 6], F32, "cw")
            p.dma("sp", cwr.t[0:5, :], Wd["conv_w"], writes=[cwr.r])
            p.dma("sp", cwr.t[5:6, :], Wd["conv_b"].rearrange("(o n) -> o n", o=1), writes=[cwr.r])
            pcw = cx.ps(st, [128, 512], F32, "pcw")
            for cc in range(24):
                p.op("pe", lambda e, cc=cc: e.transpose(pcw.t[:, cc * 6:(cc + 1) * 6], cwr.t[0:6, cc * 128:(cc + 1) * 128], idn.t[0:6, 0:6]),
                     reads=[cwr.r, idn.r], writes=[pcw.r])
            p.op("dve", lambda e: e.tensor_copy(out=cw.t[:].rearrange("p a b -> p (a b)"), in_=pcw.t[:, 0:144]), reads=[pcw.r], writes=[cw.r])
            U = cx.sb(st, [128, 4100], F32, "U")
            Uc = cx.sb(st, [128, 260], F32, "Uc")
            A = cx.sb(st, [128, 4096], F32, "Aconv")
            Ac = cx.sb(st, [128, 256], F32, "Acc")
            Vb = [cx.sb(st, [128, NK], BF16, "Vb") for _ in range(2)]
            xst = [cx.sb(st, [128, NCH, 128], BF16, "xst") for _ in range(2)]
            pp = [cx.ps(st, [128, 512], F32, "pp") for _ in range(2)]
            ptb = [cx.ps(st, [128, 1024], BF16, "ptb") for _ in range(2)]
            p.op("pool", lambda e: e.memset(U.t[:], 0.0), writes=[U.r])
            p.op("pool", lambda e: e.memset(Uc.t[:], 0.0), writes=[Uc.r])
            k = 0
            kt_ = 0
            for cc in range(24):
                V = Vb[cc % 2]
                groups = [(0, 256)] + [(256 + 512 * g, 512) for g in range(8)]
                for (t0, n) in groups:
                    P_ = pp[k % 2]
                    k += 1
                    for kc in range(8):
                        p.op("pe", lambda e, P_=P_, kc=kc, cc=cc, t0=t0, n=n: e.matmul(
                            P_.t[:, 0:n], lhsT=w_x.t[:, kc, cc * 128:(cc + 1) * 128], rhs=hT.t[:, kc, t0:t0 + n],
                            start=(kc == 0), stop=(kc == 7)), reads=[w_x.r, hT.r], writes=[P_.r])
                    if t0 == 0:
                        p.op("act", lambda e, P_=P_: e.activation(out=Uc.t[:, 2:258], in_=P_.t[:, 0:256], func=AF.Copy),
                             reads=[P_.r], writes=[Uc.r])
                    else:
                        p.op("act", lambda e, P_=P_, t0=t0: e.activation(out=U.t[:, 2 + t0 - 256:2 + t0 - 256 + 512], in_=P_.t[:, 0:512], func=AF.Copy),
                             reads=[P_.r], writes=[U.r])
                for (src, acc_, n, eng, off) in ((Uc, Ac, 256, "dve", 0), (U, A, 4096, "dve", 256)):
                    p.op(eng, lambda e, src=src, acc_=acc_, n=n, cc=cc: e.tensor_scalar(
                        out=acc_.t[:, 0:n], in0=src.t[:, 0:n], scalar1=cw.t[:, cc, 0:1], scalar2=None, op0=ALU.mult),
                        reads=[src.r, cw.r], writes=[acc_.r])
                    for kk in range(1, 5):
                        e2 = "dve"
                        p.op(e2, lambda e, src=src, acc_=acc_, n=n, cc=cc, kk=kk: e.scalar_tensor_tensor(
                            out=acc_.t[:, 0:n], in0=src.t[:, kk:kk + n], scalar=cw.t[:, cc, kk:kk + 1], in1=acc_.t[:, 0:n],
                            op0=ALU.mult, op1=ALU.add), reads=[src.r, cw.r, acc_.r], writes=[acc_.r])
                    p.op("act", lambda e, acc_=acc_, n=n, cc=cc, V=V, off=off: e.activation(
                        out=V.t[:, off:off + n], in_=acc_.t[:, 0:n], func=AF.Silu, bias=cw.t[:, cc, 5:6], scale=1.0),
                        reads=[acc_.r, cw.r], writes=[V.r])
                if cc >= 16:
                    p.dma("sp", S["BCT"].t[:, cc - 16, :], V.t[:], reads=[V.r], writes=[S["BCT"].r])
                if cc < 20:
                    XS = xst[cc % 2]
                    for i0 in range(0, NCH, 8):
                        PT = ptb[kt_ % 2]
                        kt_ += 1
                        i1 = min(NCH, i0 + 8)
                        for i in range(i0, i1):
                            p.op("pe", lambda e, PT=PT, V=V, i=i, i0=i0: e.transpose(PT.t[:, (i - i0) * 128:(i - i0 + 1) * 128],
                                                                                   V.t[:, i * 128:(i + 1) * 128], idb.t[:]),
                                 reads=[V.r, idb.r], writes=[PT.r])
                        p.op("dve", lambda e, PT=PT, XS=XS, i0=i0, i1=i1: e.tensor_copy(
                            out=XS.t[:, i0:i1, :].rearrange("p a b -> p (a b)"), in_=PT.t[:, 0:(i1 - i0) * 128]),
                            reads=[PT.r], writes=[XS.r])
                    p.dma("act", S["xB"].t.rearrange("(t p) f -> p t f", p=128)[:, :, cc * 128:(cc + 1) * 128], XS.t[:],
                          reads=[XS.r], writes=[S["xB"].r])
            p.flush()


def phase_ssd_scan(cx, Wd, S):
    p, nc = cx.p, cx.nc
    with ExitStack() as st:
        Tf = tri_const(cx, st, "p_le_j")
        Tb = tri_const(cx, st, "p_ge_j")
        Lf = tri_const(cx, st, "p_gt_j")
        Lb = tri_const(cx, st, "p_lt_j")
        ones = cx.sb(st, [128, 128], F32, "ones_s")
        p.op("pool", lambda e: e.memset(ones.t[:], 1.0), writes=[ones.r])
        abc = cx.sb(st, [128, 64], F32, "abc")
        dsk = cx.sb(st, [128, 64], F32, "dsk")
        load_bc(cx, "sp", (abc, abc.t[:]), Wd["a_log"], 64)
        load_bc(cx, "sp", (dsk, dsk.t[:]), Wd["d_skip"], 64)
        p.op("act", lambda e: e.activation(out=abc.t[:], in_=abc.t[:], func=AF.Exp), reads=[abc.r], writes=[abc.r])
        p.op("dve", lambda e: e.tensor_scalar(out=abc.t[:], in0=abc.t[:], scalar1=-1.0, scalar2=None, op0=ALU.mult),
             reads=[abc.r], writes=[abc.r])
        ST = cx.sb(st, [128, 32, 64], F32, "ST")
        STb = cx.sb(st, [128, 32, 64], BF16, "STb")
        xB = [cx.sb(st, [128, 2560], BF16, "xB") for _ in range(2)]
        BCT = [cx.sb(st, [128, 8, 128], BF16, "BCT") for _ in range(2)]
        dtt = [cx.sb(st, [128, 64], F32, "dtt") for _ in range(2)]
        sm = [cx.sb(st, [128, 6, 32], F32, "sm6") for _ in range(2)]
        xw = [cx.sb(st, [128, 32, 64], BF16, "xw") for _ in range(2)]
        CBm = [cx.sb(st, [128, 4, 128], F32, "CBm") for _ in range(2)]
        Lh = [cx.sb(st, [128, 128], F32, "Lh") for _ in range(3)]
        Eh = [cx.sb(st, [128, 128], F32, "Eh") for _ in range(3)]
        Mh = [cx.sb(st, [128, 128], BF16, "Mh") for _ in range(3)]
        yo = [cx.sb(st, [128, 2048], F32, "yo") for _ in range(2)]
        y2 = [cx.sb(st, [128, 2048], F32, "y2") for _ in range(2)]
        psm = cx.ps(st, [128, 512], F32, "psm6")
        pD = [cx.ps(st, [128, 512], F32, "pD") for _ in range(2)]
        pY = [cx.ps(st, [128, 512], F32, "pY") for _ in range(2)]
        pI = [cx.ps(st, [128, 512], F32, "pI") for _ in range(2)]
        pS = cx.ps(st, [128, 512], F32, "pS")
        xBv = S["xB"].t
        kq = 0
        kh = 0
        for d in range(2):
            Tc, Ls = (Tf, Lf) if d == 0 else (Tb, Lb)
            p.op("pool", lambda e: e.memset(ST.t[:], 0.0), writes=[ST.r])
            order = list(range(NCH)) if d == 0 else [1, 0] + list(range(NCH - 1, 1, -1))
            for c in order:
                b = kq % 2
                kq += 1
                XB, BC, DT, SM, XW, CB, YO, Y2 = xB[b], BCT[b], dtt[b], sm[b], xw[b], CBm[b], yo[b], y2[b]
                p.dma("sp", XB.t[:], xBv[c * 128:(c + 1) * 128, :], reads=[S["xB"].r], writes=[XB.r])
                p.dma("act", BC.t[:], S["BCT"].t[:, :, c * 128:(c + 1) * 128], reads=[S["BCT"].r], writes=[BC.r])
                p.dma("sp", DT.t[:], S["dt"].t[c * 128:(c + 1) * 128, :], reads=[S["dt"].r], writes=[DT.r])
                dtd = DT.t[:, d * 32:(d + 1) * 32]
                R_ = [SM.r]
                p.op("dve", lambda e, SM=SM, dtd=dtd, d=d: e.tensor_tensor(out=SM.t[:, 0, :], in0=dtd, in1=abc.t[:, d * 32:(d + 1) * 32], op=ALU.mult),
                     reads=[DT.r, abc.r], writes=R_)
                p.op("pe", lambda e, SM=SM, Tc=Tc: e.matmul(psm.t[:, 0:32], lhsT=Tc.t[:], rhs=SM.t[:, 0, :], start=True, stop=True),
                     reads=[Tc.r] + R_, writes=[psm.r])
                p.op("pe", lambda e, SM=SM: e.matmul(psm.t[:, 32:64], lhsT=ones.t[:], rhs=SM.t[:, 0, :], start=True, stop=True),
                     reads=[ones.r] + R_, writes=[psm.r])
                p.op("dve", lambda e, SM=SM: e.tensor_copy(out=SM.t[:, 2, :], in_=psm.t[:, 0:32]), reads=[psm.r], writes=R_)
                p.op("dve", lambda e, SM=SM: e.tensor_copy(out=SM.t[:, 5, :], in_=psm.t[:, 32:64]), reads=[psm.r], writes=R_)
                p.op("act", lambda e, SM=SM: e.activation(out=SM.t[:, 1, :], in_=SM.t[:, 2, :], func=AF.Exp), reads=R_, writes=R_)
                p.op("act", lambda e, SM=SM: e.activation(out=SM.t[:, 4, :], in_=SM.t[:, 5, :], func=AF.Exp), reads=R_, writes=R_)
                p.op("dve", lambda e, SM=SM: e.tensor_tensor(out=SM.t[:, 3, :], in0=SM.t[:, 5, :], in1=SM.t[:, 2, :], op=ALU.subtract),
                     reads=R_, writes=R_)
                p.op("act", lambda e, SM=SM: e.activation(out=SM.t[:, 3, :], in_=SM.t[:, 3, :], func=AF.Exp), reads=R_, writes=R_)
                p.op("dve", lambda e, SM=SM, dtd=dtd: e.tensor_tensor(out=SM.t[:, 3, :], in0=SM.t[:, 3, :], in1=dtd, op=ALU.mult),
                     reads=R_ + [DT.r], writes=R_)
                xs3 = XB.t[:, 0:2048].rearrange("p (h d) -> p h d", h=32)
                p.op("pool", lambda e, XW=XW, xs3=xs3, SM=SM: e.tensor_tensor(
                    out=XW.t[:], in0=xs3, in1=SM.t[:, 3, :].unsqueeze(2).to_broadcast([128, 32, 64]), op=ALU.mult),
                    reads=[XB.r] + R_, writes=[XW.r])
                if c >= 2:
                    for g in range(4):
                        p.op("pe", lambda e, BC=BC, g=g: e.matmul(pS.t[:, g * 128:(g + 1) * 128], lhsT=BC.t[:, g, :], rhs=BC.t[:, 4 + g, :],
                                                                  start=True, stop=True), reads=[BC.r], writes=[pS.r])
                    p.op("dve", lambda e, CB=CB, Tc=Tc: e.tensor_tensor(
                        out=CB.t[:], in0=pS.t[:].rearrange("p (g t) -> p g t", g=4),
                        in1=Tc.t[:].unsqueeze(1).to_broadcast([128, 4, 128]), op=ALU.mult), reads=[pS.r, Tc.r], writes=[CB.r])
                    p.op("dve", lambda e, STb=STb: e.tensor_copy(out=STb.t[:], in_=ST.t[:]), reads=[ST.r], writes=[STb.r])
                    for g in range(4):
                        PI = pI[g % 2]
                        PY = pY[g % 2]
                        p.op("pe", lambda e, PI=PI, BC=BC, g=g: e.matmul(
                            PI.t[:], lhsT=BC.t[:, 4 + g, :], rhs=STb.t[:, g * 8:(g + 1) * 8, :].rearrange("p a b -> p (a b)"),
                            start=True, stop=True), reads=[BC.r, STb.r], writes=[PI.r])
                        for hh in range(8):
                            h = g * 8 + hh
                            L_, E_, M_ = Lh[kh % 3], Eh[kh % 3], Mh[kh % 3]
                            PD = pD[kh % 2]
                            kh += 1
                            p.op("pool", lambda e, L_=L_, Ls=Ls, SM=SM, h=h: e.tensor_scalar(
                                out=L_.t[:], in0=Ls.t[:], scalar1=SM.t[:, 0, h:h + 1], scalar2=None, op0=ALU.mult),
                                reads=[Ls.r] + R_, writes=[L_.r])
                            p.op("pe", lambda e, PD=PD, L_=L_, Tc=Tc: e.matmul(PD.t[:, 0:128], lhsT=L_.t[:], rhs=Tc.t[:], start=True, stop=True),
                                 reads=[L_.r, Tc.r], writes=[PD.r])
                            p.op("act", lambda e, PD=PD, E_=E_: e.activation(out=E_.t[:], in_=PD.t[:, 0:128], func=AF.Exp),
                                 reads=[PD.r], writes=[E_.r])
                            p.op("dve", lambda e, E_=E_, M_=M_, CB=CB, g=g, h=h, dtd=dtd: e.scalar_tensor_tensor(
                                out=M_.t[:], in0=E_.t[:], scalar=dtd[:, h:h + 1], in1=CB.t[:, g, :], op0=ALU.mult, op1=ALU.mult),
                                reads=[E_.r, CB.r, DT.r], writes=[M_.r])
                            p.op("pe", lambda e, PY=PY, M_=M_, XB=XB, hh=hh, h=h: e.matmul(
                                PY.t[:, hh * 64:(hh + 1) * 64], lhsT=M_.t[:], rhs=XB.t[:, h * 64:(h + 1) * 64], start=True, stop=True),
                                reads=[M_.r, XB.r], writes=[PY.r])
                        p.op("dve", lambda e, YO=YO, PI=PI, SM=SM, g=g: e.tensor_tensor(
                            out=YO.t[:, g * 512:(g + 1) * 512].rearrange("p (a b) -> p a b", a=8),
                            in0=PI.t[:].rearrange("p (a b) -> p a b", a=8),
                            in1=SM.t[:, 1, g * 8:(g + 1) * 8].unsqueeze(2).to_broadcast([128, 8, 64]), op=ALU.mult),
                            reads=[PI.r] + R_, writes=[YO.r])
                        p.op("dve", lambda e, YO=YO, PY=PY, g=g: e.tensor_tensor(
                            out=YO.t[:, g * 512:(g + 1) * 512], in0=YO.t[:, g * 512:(g + 1) * 512], in1=PY.t[:], op=ALU.add),
                            reads=[PY.r, YO.r], writes=[YO.r])
                    p.op("pool", lambda e, Y2=Y2, xs3=xs3, d=d: e.tensor_tensor(
                        out=Y2.t[:].rearrange("p (h d) -> p h d", h=32), in0=xs3,
                        in1=dsk.t[:, d * 32:(d + 1) * 32].unsqueeze(2).to_broadcast([128, 32, 64]), op=ALU.mult),
                        reads=[XB.r, dsk.r], writes=[Y2.r])
                    p.op("pool", lambda e, Y2=Y2, YO=YO: e.tensor_tensor(out=Y2.t[:], in0=Y2.t[:], in1=YO.t[:], op=ALU.add),
                         reads=[YO.r, Y2.r], writes=[Y2.r])
                    yd = S["yf"] if d == 0 else S["yb"]
                    p.dma("act", yd.t[(c - 2) * 128:(c - 1) * 128, :], Y2.t[:], reads=[Y2.r], writes=[yd.r])
                for g in range(4):
                    PI = pI[g % 2]
                    p.op("pe", lambda e, PI=PI, XB=XB, XW=XW, g=g: e.matmul(
                        PI.t[:], lhsT=XB.t[:, 2048 + g * 128:2048 + (g + 1) * 128],
                        rhs=XW.t[:, g * 8:(g + 1) * 8, :].rearrange("p a b -> p (a b)"), start=True, stop=True),
                        reads=[XB.r, XW.r], writes=[PI.r])
                    stg = ST.t[:, g * 8:(g + 1) * 8, :]
                    p.op("dve", lambda e, stg=stg, SM=SM, g=g: e.tensor_tensor(
                        out=stg, in0=stg, in1=SM.t[:, 4, g * 8:(g + 1) * 8].unsqueeze(2).to_broadcast([128, 8, 64]), op=ALU.mult),
                        reads=[ST.r] + R_, writes=[ST.r])
                    p.op("dve", lambda e, stg=stg, PI=PI: e.tensor_tensor(
                        out=stg, in0=stg, in1=PI.t[:].rearrange("p (a b) -> p a b", a=8), op=ALU.add),
                        reads=[ST.r, PI.r], writes=[ST.r])
        p.flush()


def phase_ssd_out(cx, xown, sel, mod_d, g_norm, Wd, S, x2_d):
    p, nc = cx.p, cx.nc
    w_in = Wd["w_in"]
    with ExitStack() as st:
        idn = make_ident(cx, st, F32)
        idb = cx.sb(st, [128, 128], BF16, "idb7")
        p.op("dve", lambda e: e.tensor_copy(out=idb.t[:], in_=idn.t[:]), reads=[idn.r], writes=[idb.r])
        gbc = cx.sb(st, [128, 1024], F32, "gbc7")
        Abc = cx.sb(st, [128, 1024], F32, "Abc7")
        Bbc = cx.sb(st, [128, 1024], F32, "Bbc7")
        g1 = cx.sb(st, [128, 1024], F32, "g17")
        ngb = cx.sb(st, [128, 2048], F32, "ngb")
        selb = cx.sb(st, [128, 2], F32, "selb")
        load_bc(cx, "sp", (gbc, gbc.t[:]), g_norm, 1024)
        load_bc(cx, "sp", (Bbc, Bbc.t[:]), mod_d.t[0, 0:1024], 1024, src=mod_d)
        load_bc(cx, "act", (Abc, Abc.t[:]), mod_d.t[0, 1024:2048], 1024, src=mod_d)
        load_bc(cx, "act", (g1, g1.t[:]), mod_d.t[0, 2048:3072], 1024, src=mod_d)
        load_bc(cx, "sp", (ngb, ngb.t[:]), Wd["norm_g"], 2048)
        load_bc(cx, "sp", (selb, selb.t[:]), sel, 2)
        p.op("dve", lambda e: e.scalar_tensor_tensor(out=Abc.t[:], in0=Abc.t[:], scalar=1.0, in1=gbc.t[:], op0=ALU.add, op1=ALU.mult),
             reads=[Abc.r, gbc.r], writes=[Abc.r])
        w_z = cx.sb(st, [128, 8, 2048], BF16, "w_z")
        w_o = cx.sb(st, [128, 16, 1024], BF16, "w_o")
        wv = w_in.rearrange("(kc p) n -> p kc n", p=128)
        for kc in range(8):
            p.dma("pool", w_z.t[:, kc, :], wv[:, kc, 0:2048], writes=[w_z.r])
        wov = Wd["w_out"].rearrange("(kc p) n -> p kc n", p=128)
        for kc in range(0, 16, 4):
            p.dma("pool", w_o.t[:, kc:kc + 4, :], wov[:, kc:kc + 4, :], writes=[w_o.r])
        x_t = [cx.sb(st, [128, 1024], F32, "x_t7") for _ in range(2)]
        h_t = [cx.sb(st, [128, 1024], F32, "h_t7") for _ in range(2)]
        junk = cx.sb(st, [128, 1024], F32, "junk7")
        sm = [cx.sb(st, [128, 16], F32, "sm7") for _ in range(2)]
        hT = [cx.sb(st, [128, 8, 128], BF16, "hT7") for _ in range(2)]
        ya = [cx.sb(st, [128, 2048], F32, "ya") for _ in range(2)]
        yb_ = [cx.sb(st, [128, 2048], F32, "yb") for _ in range(2)]
        sz = [cx.sb(st, [128, 2048], F32, "sz") for _ in range(2)]
        un = [cx.sb(st, [128, 2048], BF16, "un") for _ in range(2)]
        uT = [cx.sb(st, [128, 16, 128], BF16, "uT") for _ in range(2)]
        ptA = cx.ps(st, [128, 1024], F32, "ptA7")
        pz = cx.ps(st, [128, 2048], F32, "pz")
        ptu = cx.ps(st, [128, 2048], BF16, "ptu")
        for i in range(16):
            b = i % 2
            X, H, SM, HT, YA, YB, SZ, UN, UT = x_t[b], h_t[b], sm[b], hT[b], ya[b], yb_[b], sz[b], un[b], uT[b]
            p.dma("sp", X.t[:], xown[i * 128:(i + 1) * 128, :], writes=[X.r])
            p.dma("act", YA.t[:], S["yf"].t[i * 128:(i + 1) * 128, :], reads=[S["yf"].r], writes=[YA.r])
            p.dma("sp", YB.t[:], S["yb"].t[i * 128:(i + 1) * 128, :], reads=[S["yb"].r], writes=[YB.r])
            p.op("pool", lambda e, YA=YA, YB=YB: e.tensor_tensor(out=YA.t[:], in0=YA.t[:], in1=YB.t[:], op=ALU.add),
                 reads=[YA.r, YB.r], writes=[YA.r])
            p.op("pool", lambda e, YA=YA: e.tensor_scalar(out=YA.t[:], in0=YA.t[:], scalar1=selb.t[:, 0:1], scalar2=None, op0=ALU.mult),
                 reads=[YA.r, selb.r], writes=[YA.r])
            p.dma("sp", YB.t[:], S["yf"].t[(16 + i) * 128:(17 + i) * 128, :], reads=[S["yf"].r], writes=[YB.r])
            p.op("dve", lambda e, YA=YA, YB=YB: e.scalar_tensor_tensor(out=YA.t[:], in0=YB.t[:], scalar=selb.t[:, 1:2], in1=YA.t[:],
                                                                        op0=ALU.mult, op1=ALU.add),
                 reads=[YA.r, YB.r, selb.r], writes=[YA.r])
            p.dma("sp", YB.t[:], S["yb"].t[(16 + i) * 128:(17 + i) * 128, :], reads=[S["yb"].r], writes=[YB.r])
            p.op("dve", lambda e, YA=YA, YB=YB: e.scalar_tensor_tensor(out=YA.t[:], in0=YB.t[:], scalar=selb.t[:, 1:2], in1=YA.t[:],
                                                                        op0=ALU.mult, op1=ALU.add),
                 reads=[YA.r, YB.r, selb.r], writes=[YA.r])
            p.op("act", lambda e, X=X, SM=SM: e.activation(out=junk.t[:], in_=X.t[:], func=AF.Square, accum_out=SM.t[:, 0:1]),
                 reads=[X.r], writes=[junk.r, SM.r])
            emit_rstd(cx, (SM, SM.t[:, 0:1]), (SM, SM.t[:, 1:2]), 1024.0)
            p.op("dve", lambda e, X=X, H=H, SM=SM: e.scalar_tensor_tensor(
                out=H.t[:], in0=X.t[:], scalar=SM.t[:, 1:2], in1=Abc.t[:], op0=ALU.mult, op1=ALU.mult),
                reads=[X.r, SM.r, Abc.r], writes=[H.r])
            p.op("dve", lambda e, H=H: e.tensor_tensor(out=H.t[:], in0=H.t[:], in1=Bbc.t[:], op=ALU.add),
                 reads=[H.r, Bbc.r], writes=[H.r])
            emit_transpose8(cx, H, HT, ptA, idn)
            for nb in range(4):
                for kc in range(8):
                    p.op("pe", lambda e, HT=HT, kc=kc, nb=nb: e.matmul(
                        pz.t[:, nb * 512:(nb + 1) * 512], lhsT=HT.t[:, kc, :], rhs=w_z.t[:, kc, nb * 512:(nb + 1) * 512],
                        start=(kc == 0), stop=(kc == 7)), reads=[HT.r, w_z.r], writes=[pz.r])
            p.op("act", lambda e, SZ=SZ: e.activation(out=SZ.t[:], in_=pz.t[:], func=AF.Silu), reads=[pz.r], writes=[SZ.r])
            p.op("dve", lambda e, SZ=SZ, YA=YA: e.tensor_tensor(out=SZ.t[:], in0=SZ.t[:], in1=YA.t[:], op=ALU.mult),
                 reads=[SZ.r, YA.r], writes=[SZ.r])
            for g in range(4):
                p.op("act", lambda e, SZ=SZ, SM=SM, g=g: e.activation(out=junk.t[:, 0:512], in_=SZ.t[:, g * 512:(g + 1) * 512], func=AF.Square,
                                                                     accum_out=SM.t[:, 4 + g:5 + g]), reads=[SZ.r], writes=[junk.r, SM.r])
            emit_rstd(cx, (SM, SM.t[:, 4:8]), (SM, SM.t[:, 8:12]), 512.0)
            p.op("dve", lambda e, SZ=SZ, SM=SM: e.tensor_tensor(
                out=SZ.t[:].rearrange("p (g c) -> p g c", g=4), in0=SZ.t[:].rearrange("p (g c) -> p g c", g=4),
                in1=SM.t[:, 8:12].unsqueeze(2).to_broadcast([128, 4, 512]), op=ALU.mult), reads=[SZ.r, SM.r], writes=[SZ.r])
            p.op("pool", lambda e, SZ=SZ, UN=UN: e.tensor_tensor(out=UN.t[:], in0=SZ.t[:], in1=ngb.t[:], op=ALU.mult),
                 reads=[SZ.r, ngb.r], writes=[UN.r])
            for kc in range(16):
                p.op("pe", lambda e, UN=UN, kc=kc: e.transpose(ptu.t[:, kc * 128:(kc + 1) * 128], UN.t[:, kc * 128:(kc + 1) * 128], idb.t[:]),
                     reads=[UN.r, idb.r], writes=[ptu.r])
            p.op("act", lambda e, UT=UT: e.activation(out=UT.t[:].rearrange("p a b -> p (a b)"), in_=ptu.t[:], func=AF.Copy),
                 reads=[ptu.r], writes=[UT.r])
            for hb in range(2):
                for kc in range(16):
                    p.op("pe", lambda e, UT=UT, hb=hb, kc=kc: e.matmul(
                        ptA.t[:, hb * 512:(hb + 1) * 512], lhsT=UT.t[:, kc, :], rhs=w_o.t[:, kc, hb * 512:(hb + 1) * 512],
                        start=(kc == 0), stop=(kc == 15)), reads=[UT.r, w_o.r], writes=[ptA.r])
            p.op("dve", lambda e, H=H: e.tensor_tensor(out=H.t[:], in0=ptA.t[:], in1=g1.t[:], op=ALU.mult),
                 reads=[ptA.r, g1.r], writes=[H.r])
            p.op("pool", lambda e, H=H, X=X: e.tensor_tensor(out=H.t[:], in0=H.t[:], in1=X.t[:], op=ALU.add),
                 reads=[H.r, X.r], writes=[H.r])
            p.dma("act", x2_d.t[i * 128:(i + 1) * 128, :], H.t[:], reads=[H.r], writes=[x2_d.r])
        p.flush()


def phase_final_norm(cx, x_d, final_g, out):
    p = cx.p
    with ExitStack() as st:
        gbc = cx.sb(st, [128, 1024], F32, "gbc9")
        load_bc(cx, "sp", (gbc, gbc.t[:]), final_g, 1024)
        x_t = [cx.sb(st, [128, 1024], F32, "x_t9") for _ in range(2)]
        junk = cx.sb(st, [128, 1024], F32, "junk9")
        sm = [cx.sb(st, [128, 4], F32, "sm9") for _ in range(2)]
        for i in range(16):
            X, SM = x_t[i % 2], sm[i % 2]
            p.dma("sp", X.t[:], x_d.t[i * 128:(i + 1) * 128, :], reads=[x_d.r], writes=[X.r])
            p.op("act", lambda e, X=X, SM=SM: e.activation(out=junk.t[:], in_=X.t[:], func=AF.Square, accum_out=SM.t[:, 0:1]),
                 reads=[X.r], writes=[junk.r, SM.r])
            emit_rstd(cx, (SM, SM.t[:, 0:1]), (SM, SM.t[:, 1:2]), 1024.0)
            p.op("dve", lambda e, X=X, SM=SM: e.scalar_tensor_tensor(out=X.t[:], in0=X.t[:], scalar=SM.t[:, 1:2], in1=gbc.t[:],
                                                                     op0=ALU.mult, op1=ALU.mult), reads=[X.r, SM.r, gbc.r], writes=[X.r])
            p.dma("act", out[i * 128:(i + 1) * 128, :], X.t[:], reads=[X.r])
        p.flush()


SSD_W = (("w_in", [1024, 5184]), ("conv_w", [5, 3072]), ("conv_b", [3072]), ("dt_bias", [64]), ("a_log", [64]),
         ("d_skip", [64]), ("norm_g", [2048]), ("w_out", [2048, 1024]))


def build_B(stop_after=None):
    nc = bass.Bass("TRN2", target_bir_lowering=False)
    inp = lambda name, shape: nc.dram_tensor(name, list(shape), F32, kind="ExternalInput").ap()
    xin = inp("xin", [NK, 1024])
    xown = inp("xown", [2048, 1024])
    sel = inp("sel", [2])
    cvec = inp("cvec", [2, 1024])
    w_mod = inp("w_mod", [1024, 6144])
    b_mod = inp("b_mod", [6144])
    norm_g = inp("norm_g", [2, 1024])
    final_g = inp("final_g", [1024])
    Wd = {k: inp("ssd_" + k, s) for k, s in SSD_W}
    M = {k: inp("moe_" + k, s) for k, s in MOE_W}
    out = nc.dram_tensor("out", [2048, 1024], F32, kind="ExternalOutput").ap()
    with ExitStack() as st:
        cx = Ctx(nc, st)
        mod_d = cx.dram([2, 6144], F32, "mod_d")
        S = {"dt": cx.dram([NK, 64], F32, "dt_d"), "BCT": cx.dram([128, 8, NK], BF16, "BCT_d"),
             "xB": cx.dram([NK, 2560], BF16, "xB_d"), "yf": cx.dram([4096, 2048], F32, "yf_d"),
             "yb": cx.dram([4096, 2048], F32, "yb_d")}
        phase_mod(cx, cvec, w_mod, b_mod, mod_d)
        phase_ssd_prep(cx, xin, mod_d, norm_g[0, :], Wd, S)
        phase_ssd_scan(cx, Wd, S)
        if stop_after == "ssd":
            phase_ssd_out(cx, xown, sel, mod_d, norm_g[0, :], Wd, S, TL(out))
            return nc
        x2_d = cx.dram([2048, 1024], F32, "x2_d")
        x3_d = cx.dram([2048, 1024], F32, "x3_d")
        phase_ssd_out(cx, xown, sel, mod_d, norm_g[0, :], Wd, S, x2_d)
        phase_moe(cx, x2_d, 16, mod_d, norm_g[1, :], M, x3_d, n_ctx_tiles=0)
        phase_final_norm(cx, x3_d, final_g, out)
    return nc


def inputs_B(core, x1, xc1, c, c_ctx, w_mod, b_mod, norm_g, final_g, ssd, moe):
    b, hf = core // 2, core % 2
    d = {"xin": np.ascontiguousarray(np.concatenate([xc1[b], x1[b]], axis=0)),
         "xown": np.ascontiguousarray(x1[b, hf * 2048:(hf + 1) * 2048]),
         "sel": np.array([1.0 - hf, float(hf)], np.float32),
         "cvec": np.ascontiguousarray(np.stack([c[b], c_ctx], axis=0)),
         "w_mod": w_mod[1], "b_mod": b_mod[1], "norm_g": norm_g[1], "final_g": final_g}
    for k, s in SSD_W:
        d["ssd_" + k] = np.ascontiguousarray(ssd[k][0].reshape(s))
    for k, s in MOE_W:
        d["moe_" + k] = np.ascontiguousarray(moe[k][1].reshape(s))
    return d


_NC_CACHE = {}


def kernel(x, c, ctx, c_ctx, w_mod, b_mod, norm_g, final_g,
           att_w_in, att_q_norm, att_w_uq, att_kv_norm, att_w_ukv,
           att_lq1, att_lk1, att_lq2, att_lk2, att_subln, att_w_out,
           ssd_w_in, ssd_conv_w, ssd_conv_b, ssd_dt_bias, ssd_a_log, ssd_d, ssd_norm_g, ssd_w_out,
           moe_w_rg, moe_b_rg, moe_w_rf, moe_b_rf, moe_w_gate, moe_w_up, moe_w_down):
    f = lambda a: np.asarray(a, dtype=np.float32)
    x, c, ctx, c_ctx, w_mod, b_mod, norm_g, final_g = map(f, (x, c, ctx, c_ctx, w_mod, b_mod, norm_g, final_g))
    att = {"w_in": f(att_w_in), "q_norm": f(att_q_norm), "w_uq": f(att_w_uq), "kv_norm": f(att_kv_norm), "w_ukv": f(att_w_ukv),
           "lq1": f(att_lq1), "lk1": f(att_lk1), "lq2": f(att_lq2), "lk2": f(att_lk2), "subln": f(att_subln), "w_out": f(att_w_out)}
    ssd = {"w_in": f(ssd_w_in), "conv_w": f(ssd_conv_w), "conv_b": f(ssd_conv_b), "dt_bias": f(ssd_dt_bias), "a_log": f(ssd_a_log),
           "d_skip": f(ssd_d), "norm_g": f(ssd_norm_g), "w_out": f(ssd_w_out)}
    moe = {"w_rg": f(moe_w_rg), "b_rg": f(moe_b_rg), "w_rf": f(moe_w_rf), "b_rf": f(moe_b_rf),
           "w_gate": f(moe_w_gate), "w_up": f(moe_w_up), "w_down": f(moe_w_down)}
    cores = list(range(8))
    ncA = build_A()
    resA = run_bass_kernel_spmd(ncA, [inputs_A(k, x, c, ctx, c_ctx, w_mod, b_mod, norm_g, att, moe) for k in cores], core_ids=cores)
    x1 = np.empty((4, 4096, 1024), np.float32)
    xc1 = np.empty((4, 256, 1024), np.float32)
    for k in cores:
        b, hf = k // 2, k % 2
        o = resA.results[k]["xout"]
        x1[b, hf * 2048:(hf + 1) * 2048] = o[256:]
        if hf == 0:
            xc1[b] = o[:256]
    ncB = build_B()
    resB = run_bass_kernel_spmd(ncB, [inputs_B(k, x1, xc1, c, c_ctx, w_mod, b_mod, norm_g, final_g, ssd, moe) for k in cores], core_ids=cores)
    out = np.empty((4, 4096, 1024), np.float32)
    for k in cores:
        b, hf = k // 2, k % 2
        out[b, hf * 2048:(hf + 1) * 2048] = resB.results[k]["out"]
    return out
```

```python
import math
from contextlib import ExitStack
import numpy as np
import concourse.bass as bass
import concourse.mybir as mybir
from concourse.bass_utils import run_bass_kernel_spmd

F32 = mybir.dt.float32
BF16 = mybir.dt.bfloat16
AF = mybir.ActivationFunctionType
ALU = mybir.AluOpType
AX = mybir.AxisListType

EPS = 1e-6
D = 1024
MLA_SCALE = 1.0 / math.sqrt(96.0)
DIFF_SCALE = 1.0 / math.sqrt(64.0)
LAMBDA_INIT0 = 0.8 - 0.6 * math.exp(-0.3 * 0)


class Res:
    __slots__ = ("w", "r")

    def __init__(self):
        self.w = None
        self.r = []


class Prog:
    ENGS = ("pe", "act", "dve", "pool", "sp")
    NDMASEM = 8

    def __init__(self, nc, stack):
        self.nc = nc
        self.ops = {e: [] for e in self.ENGS}
        self.cnt = {e: 0 for e in self.ENGS}
        self.stack = stack
        self.gen = 0
        self.semg = {(e, 0): stack.enter_context(nc.semaphore("s_%s0" % e)) for e in self.ENGS}
        self.seen = {e: {} for e in self.ENGS}
        self.dsem, self.dcnt, self.dnext = {}, {}, {}
        for q in ("sp", "act", "pool"):
            self.dsem[q] = [stack.enter_context(nc.semaphore("d_%s%d" % (q, i)))
                            for i in range(self.NDMASEM)]
            self.dcnt[q] = [0] * self.NDMASEM
            self.dnext[q] = 0

    def _need(self, e, tok, waits):
        if tok is None:
            return
        if tok[0] == "e":
            _, pe_, g_, v = tok
            if pe_ == e and e == "pe":
                return
            key = ("e", pe_, g_)
        else:
            _, q, i, v = tok
            key = ("d", q, i)
        if self.seen[e].get(key, 0) >= v:
            return
        if v > waits.get(key, 0):
            waits[key] = v

    def _deps(self, e, reads, writes):
        waits = {}
        for r in reads:
            self._need(e, r.w, waits)
        for w in writes:
            self._need(e, w.w, waits)
            for t in w.r:
                self._need(e, t, waits)
        out = []
        for key, v in waits.items():
            self.seen[e][key] = v
            if key[0] == "e":
                out.append((self.semg[(key[1], key[2])], v))
            else:
                out.append((self.dsem[key[1]][key[2]], v))
        return out

    def _mark(self, tok, reads, writes):
        for r in reads:
            r.r.append(tok)
            if len(r.r) > 64:
                r.r = r.r[-48:]
        for w in writes:
            w.w = tok
            w.r = []

    def op(self, e, fn, reads=(), writes=()):
        waits = self._deps(e, reads, writes)
        self.cnt[e] += 1
        v = self.cnt[e]
        sem = self.semg[(e, self.gen)]

        def run(eng, fn=fn, waits=waits, sem=sem):
            for s, val in waits:
                eng.wait_ge(s, val)
            fn(eng).then_inc(sem, 1)
        self.ops[e].append(run)
        tok = ("e", e, self.gen, v)
        self._mark(tok, reads, writes)
        return tok

    def dma(self, q, out, in_, reads=(), writes=(), **kw):
        i = self.dnext[q]
        self.dnext[q] = (i + 1) % self.NDMASEM
        waits = self._deps(q, reads, writes)
        prev = self.dcnt[q][i]
        key = ("d", q, i)
        if prev > 0 and self.seen[q].get(key, 0) < prev:
            waits.append((self.dsem[q][i], prev))
            self.seen[q][key] = prev
        self.dcnt[q][i] = prev + 16
        v = prev + 16
        sem = self.dsem[q][i]

        def run(eng, waits=waits, sem=sem, out=out, in_=in_, kw=kw):
            for s, val in waits:
                eng.wait_ge(s, val)
            eng.dma_start(out=out, in_=in_, **kw).then_inc(sem, 16)
        self.ops[q].append(run)
        tok = ("d", q, i, v)
        self._mark(tok, reads, writes)
        return tok

    def flush(self, final=False):
        for q in ("sp", "act", "pool"):
            fin = []
            for i in range(self.NDMASEM):
                if self.dcnt[q][i] > 0 and self.seen[q].get(("d", q, i), 0) < self.dcnt[q][i]:
                    fin.append((self.dsem[q][i], self.dcnt[q][i]))
                    self.seen[q][("d", q, i)] = self.dcnt[q][i]

            def run(eng, fin=fin):
                for s, val in fin:
                    eng.wait_ge(s, val)
            self.ops[q].append(run)
        ops = self.ops
        with self.nc.Block() as block:
            @block.tensor
            def _(eng):
                for f in ops["pe"]:
                    f(eng)

            @block.scalar
            def _(eng):
                for f in ops["act"]:
                    f(eng)

            @block.vector
            def _(eng):
                for f in ops["dve"]:
                    f(eng)

            @block.gpsimd
            def _(eng):
                for f in ops["pool"]:
                    f(eng)

            @block.sync
            def _(eng):
                for f in ops["sp"]:
                    f(eng)
        self.ops = {e: [] for e in self.ENGS}
        if max(self.cnt.values()) > 5000:
            self.gen += 1
            for e in self.ENGS:
                self.semg[(e, self.gen)] = self.stack.enter_context(self.nc.semaphore("s_%s%d" % (e, self.gen)))
                self.cnt[e] = 0

    def dma_like(self, q, fn, reads=(), writes=()):
        i = self.dnext[q]
        self.dnext[q] = (i + 1) % self.NDMASEM
        waits = self._deps(q, reads, writes)
        prev = self.dcnt[q][i]
        key = ("d", q, i)
        if prev > 0 and self.seen[q].get(key, 0) < prev:
            waits.append((self.dsem[q][i], prev))
            self.seen[q][key] = prev
        self.dcnt[q][i] = prev + 16
        v = prev + 16
        sem = self.dsem[q][i]

        def run(eng, waits=waits, sem=sem, fn=fn):
            for s, val in waits:
                eng.wait_ge(s, val)
            fn(eng).then_inc(sem, 16)
        self.ops[q].append(run)
        tok = ("d", q, i, v)
        self._mark(tok, reads, writes)
        return tok


class TL:
    __slots__ = ("t", "r")

    def __init__(self, t):
        self.t = t
        self.r = Res()


class Ctx:
    def __init__(self, nc, st):
        self.nc = nc
        self.p = Prog(nc, st)
        self.n = 0

    def sb(self, st, shape, dt, name=None):
        self.n += 1
        return TL(st.enter_context(self.nc.sbuf_tensor("%s_%d" % (name or "sb", self.n), list(shape), dt)))

    def ps(self, st, shape, dt, name=None):
        self.n += 1
        return TL(st.enter_context(self.nc.psum_tensor("%s_%d" % (name or "ps", self.n), list(shape), dt)))

    def dram(self, shape, dt, name):
        self.n += 1
        return TL(self.nc.dram_tensor("%s_%d" % (name, self.n), list(shape), dt).ap())


def make_ident(cx, st, dt):
    idn = cx.sb(st, [128, 128], dt, "ident")
    if dt == F32:
        cx.p.op("pool", lambda e: e.memset(idn.t[:], 0.0), writes=[idn.r])
        cx.p.op("pool", lambda e: e.affine_select(out=idn.t[:], in_=idn.t[:], pattern=[[-1, 128]],
                                                  compare_op=ALU.not_equal, fill=1.0, base=0,
                                                  channel_multiplier=1), reads=[idn.r], writes=[idn.r])
    return idn


def emit_rstd(cx, ssq, rstd, n, eps=EPS):
    p = cx.p
    (s_tl, s_ap), (r_tl, r_ap) = ssq, rstd
    p.op("dve", lambda e: e.tensor_scalar(out=r_ap, in0=s_ap, scalar1=1.0 / n, scalar2=eps,
                                          op0=ALU.mult, op1=ALU.add), reads=[s_tl.r], writes=[r_tl.r])
    p.op("act", lambda e: e.activation(out=r_ap, in_=r_ap, func=AF.Sqrt), reads=[r_tl.r], writes=[r_tl.r])
    p.op("dve", lambda e: e.reciprocal(out=r_ap, in_=r_ap), reads=[r_tl.r], writes=[r_tl.r])


def phase_mod(cx, cvec, w_mod, b_mod, mod_d):
    p, nc = cx.p, cx.nc
    with ExitStack() as st:
        crow = cx.sb(st, [1, 2048], F32, "crow")
        ones = cx.sb(st, [1, 128], F32, "ones")
        brow = cx.sb(st, [1, 6144], F32, "brow")
        cbc = cx.sb(st, [128, 2, 8, 128], F32, "cbc")
        wm = [cx.sb(st, [128, 8, 512], F32, "wm") for _ in range(2)]
        mrow = [cx.sb(st, [1, 512], F32, "mrow") for _ in range(2)]
        pst = [cx.ps(st, [128, 128], F32, "pst") for _ in range(2)]
        psm = [cx.ps(st, [128, 512], F32, "psm") for _ in range(2)]
        p.dma("sp", crow.t[0:1, :], cvec.rearrange("(o s) d -> o (s d)", o=1), writes=[crow.r])
        p.dma("sp", brow.t[0:1, :], b_mod.rearrange("(o n) -> o n", o=1), writes=[brow.r])
        p.op("dve", lambda e: e.memset(ones.t[:], 1.0), writes=[ones.r])
        k = 0
        for s in range(2):
            for kc in range(8):
                pt = pst[k % 2]
                k += 1
                p.op("pe", lambda e, pt=pt, s=s, kc=kc: e.matmul(
                    pt.t[:], lhsT=crow.t[0:1, s * 1024 + kc * 128: s * 1024 + (kc + 1) * 128],
                    rhs=ones.t[0:1, :], start=True, stop=True), reads=[crow.r, ones.r], writes=[pt.r])
                p.op("act", lambda e, pt=pt, s=s, kc=kc: e.activation(
                    out=cbc.t[:, s, kc, :], in_=pt.t[:], func=AF.Silu), reads=[pt.r], writes=[cbc.r])
        wv = w_mod.rearrange("(kc p) n -> p kc n", p=128)
        k = 0
        for nb in range(12):
            w = wm[nb % 2]
            p.dma("sp" if nb % 2 == 0 else "act", w.t[:], wv[:, :, nb * 512:(nb + 1) * 512], writes=[w.r])
            for s in range(2):
                pm = psm[k % 2]
                mr = mrow[k % 2]
                k += 1
                for kc in range(8):
                    p.op("pe", lambda e, pm=pm, w=w, s=s, kc=kc: e.matmul(
                        pm.t[:], lhsT=cbc.t[:, s, kc, :], rhs=w.t[:, kc, :], start=(kc == 0), stop=(kc == 7)),
                        reads=[cbc.r, w.r], writes=[pm.r])
                p.op("dve", lambda e, pm=pm, mr=mr, nb=nb: e.tensor_tensor(
                    out=mr.t[0:1, :], in0=pm.t[0:1, :], in1=brow.t[0:1, nb * 512:(nb + 1) * 512], op=ALU.add),
                    reads=[pm.r, brow.r], writes=[mr.r])
                p.dma("sp", mod_d.t[s:s + 1, nb * 512:(nb + 1) * 512], mr.t[0:1, :], reads=[mr.r], writes=[mod_d.r])
        p.flush()


def load_bc(cx, q, dst, src_ap, n, src=None):
    tl, ap = dst
    return cx.p.dma(q, ap, src_ap.partition_broadcast(128), reads=([src.r] if src is not None else []), writes=[tl.r])


def emit_norm_mod(cx, x_t, h_t, A, Bm, junk, ssq, rstd):
    p = cx.p
    p.op("act", lambda e: e.activation(out=junk.t[:], in_=x_t.t[:], func=AF.Square, accum_out=ssq.t[:, 0:1]),
         reads=[x_t.r], writes=[junk.r, ssq.r])
    emit_rstd(cx, (ssq, ssq.t[:, 0:1]), (rstd, rstd.t[:, 0:1]), 1024.0)
    p.op("dve", lambda e: e.scalar_tensor_tensor(out=h_t.t[:], in0=x_t.t[:], scalar=rstd.t[:, 0:1], in1=A[1],
                                                 op0=ALU.mult, op1=ALU.mult),
         reads=[x_t.r, rstd.r, A[0].r], writes=[h_t.r])
    p.op("pool", lambda e: e.tensor_tensor(out=h_t.t[:], in0=h_t.t[:], in1=Bm[1], op=ALU.add),
         reads=[h_t.r, Bm[0].r], writes=[h_t.r])


def emit_transpose8(cx, h_t, hT, pt, idn, dt_in=F32):
    p = cx.p
    for kc in range(8):
        p.op("pe", lambda e, kc=kc: e.transpose(pt.t[:, kc * 128:(kc + 1) * 128], h_t.t[:, kc * 128:(kc + 1) * 128],
                                                idn.t[:]), reads=[h_t.r, idn.r], writes=[pt.r])
    p.op("act", lambda e: e.activation(out=hT.t[:, 0:4, :].rearrange("p a b -> p (a b)"), in_=pt.t[:, 0:512], func=AF.Copy),
         reads=[pt.r], writes=[hT.r])
    p.op("dve", lambda e: e.tensor_copy(out=hT.t[:, 4:8, :].rearrange("p a b -> p (a b)"), in_=pt.t[:, 512:1024]),
         reads=[pt.r], writes=[hT.r])


def emit_rope(cx, eng, src, dst, tmp, cos, sin, H, half, reads, writes):
    p = cx.p
    cb = cos.unsqueeze(1).to_broadcast([128, H, half])
    sbc = sin.unsqueeze(1).to_broadcast([128, H, half])
    x1, x2 = src[:, :, 0:half], src[:, :, half:2 * half]
    o1, o2 = dst[:, :, 0:half], dst[:, :, half:2 * half]
    t1, t2 = tmp[:, 0, :, :], tmp[:, 1, :, :]
    rd, wr = list(reads), list(writes)
    p.op(eng, lambda e: e.tensor_tensor(out=t1, in0=x1, in1=cb, op=ALU.mult), reads=rd, writes=wr)
    p.op(eng, lambda e: e.tensor_tensor(out=t2, in0=x2, in1=sbc, op=ALU.mult), reads=rd, writes=wr)
    p.op(eng, lambda e: e.tensor_tensor(out=o1, in0=t1, in1=t2, op=ALU.subtract), reads=rd + wr, writes=wr)
    p.op(eng, lambda e: e.tensor_tensor(out=t1, in0=x2, in1=cb, op=ALU.mult), reads=rd, writes=wr)
    p.op(eng, lambda e: e.tensor_tensor(out=t2, in0=x1, in1=sbc, op=ALU.mult), reads=rd, writes=wr)
    p.op(eng, lambda e: e.tensor_tensor(out=o2, in0=t1, in1=t2, op=ALU.add), reads=rd + wr, writes=wr)


DBG_CUT = None
NT_ALL = 34
NQ_T = 18
NK = NT_ALL * 128
NQ = NQ_T * 128


def phase_attn_prep(cx, xin, rope, mod_d, norm_g0, W, S):
    p, nc = cx.p, cx.nc
    with ExitStack() as st:
        idn = make_ident(cx, st, F32)
        gbc = cx.sb(st, [128, 1024], F32, "gbc")
        Abc = [cx.sb(st, [128, 1024], F32, "Abc") for _ in range(2)]
        Bbc = [cx.sb(st, [128, 1024], F32, "Bbc") for _ in range(2)]
        load_bc(cx, "sp", (gbc, gbc.t[:]), norm_g0, 1024)
        for s in range(2):
            load_bc(cx, "sp", (Bbc[s], Bbc[s].t[:]), mod_d.t[s, 0:1024], 1024, src=mod_d)
            load_bc(cx, "act", (Abc[s], Abc[s].t[:]), mod_d.t[s, 1024:2048], 1024, src=mod_d)
            p.op("dve", lambda e, s=s: e.scalar_tensor_tensor(out=Abc[s].t[:], in0=Abc[s].t[:], scalar=1.0, in1=gbc.t[:],
                                                              op0=ALU.add, op1=ALU.mult),
                 reads=[Abc[s].r, gbc.r], writes=[Abc[s].r])
        gq = cx.sb(st, [128, 256], F32, "gq")
        gkv = cx.sb(st, [128, 128], F32, "gkv")
        load_bc(cx, "sp", (gq, gq.t[:]), W["q_norm"], 256)
        load_bc(cx, "sp", (gkv, gkv.t[:]), W["kv_norm"], 128)
        w_in = cx.sb(st, [128, 8, 1952], BF16, "w_in")
        w_uq = cx.sb(st, [128, 2, 768], BF16, "w_uq")
        w_ukv = cx.sb(st, [128, 1024], BF16, "w_ukv")
        wiv = W["w_in"].rearrange("(kc p) n -> p kc n", p=128)
        for kc in range(8):
            p.dma("pool", w_in.t[:, kc, :], wiv[:, kc, :], writes=[w_in.r])
        p.dma("pool", w_uq.t[:], W["w_uq"].rearrange("(kc p) n -> p kc n", p=128), writes=[w_uq.r])
        p.dma("pool", w_ukv.t[:], W["w_ukv"], writes=[w_ukv.r])

        NB = 2
        x_t = [cx.sb(st, [128, 1024], F32, "x_t") for _ in range(NB)]
        rp_t = [cx.sb(st, [128, 96], F32, "rp_t") for _ in range(NB)]
        h_t = [cx.sb(st, [128, 1024], F32, "h_t") for _ in range(NB)]
        junk = cx.sb(st, [128, 1024], F32, "junk")
        small = [cx.sb(st, [128, 8], F32, "small") for _ in range(NB)]
        hT = [cx.sb(st, [128, 8, 128], BF16, "hT") for _ in range(NB)]
        pj = [cx.sb(st, [128, 1952], F32, "pj") for _ in range(NB)]
        cn = [cx.sb(st, [128, 384], F32, "cn") for _ in range(NB)]
        cnT = [cx.sb(st, [128, 3, 128], BF16, "cnT") for _ in range(NB)]
        Kt = [cx.sb(st, [128, 8, 96], F32, "Kt") for _ in range(NB)]
        Qt = [cx.sb(st, [128, 8, 96], F32, "Qt") for _ in range(NB)]
        qraw = [cx.sb(st, [128, 8, 96], F32, "qraw") for _ in range(NB)]
        Vm = [cx.sb(st, [128, 8, 66], BF16, "Vm") for _ in range(NB)]
        Vd = [cx.sb(st, [128, 4, 130], BF16, "Vd") for _ in range(NB)]
        dkr = [cx.sb(st, [128, 8, 64], F32, "dkr") for _ in range(NB)]
        dqr = [cx.sb(st, [128, 8, 64], F32, "dqr") for _ in range(NB)]
        tmp = [cx.sb(st, [128, 2, 8, 32], F32, "tmp") for _ in range(NB)]
        kper = [cx.sb(st, [128, 1, 32], F32, "kper") for _ in range(NB)]
        KTm_s = [cx.sb(st, [96, 8, 128], BF16, "KTm_s") for _ in range(NB)]
        QTm_s = [cx.sb(st, [96, 8, 128], BF16, "QTm_s") for _ in range(NB)]
        KTd_s = [cx.sb(st, [128, 4, 128], BF16, "KTd_s") for _ in range(NB)]
        QTd_s = [cx.sb(st, [128, 4, 128], BF16, "QTd_s") for _ in range(NB)]
        ptA = cx.ps(st, [128, 1024], F32, "ptA")
        ppj = cx.ps(st, [128, 2048], F32, "ppj")
        pkv = cx.ps(st, [128, 1024], F32, "pkv")
        for b in range(NB):
            p.op("pool", lambda e, b=b: e.memset(Vm[b].t[:], 1.0), writes=[Vm[b].r])
            p.op("pool", lambda e, b=b: e.memset(Vd[b].t[:], 1.0), writes=[Vd[b].r])

        for i in range(NT_ALL):
            b = i % NB
            is_ctx = i < 2
            is_q = i < NQ_T
            s = 1 if is_ctx else 0
            X, RP, H, SM, HT, PJ, CN, CNT = x_t[b], rp_t[b], h_t[b], small[b], hT[b], pj[b], cn[b], cnT[b]
            p.dma("sp", X.t[:], xin[i * 128:(i + 1) * 128, :], writes=[X.r])
            p.dma("act", RP.t[:], rope[i * 128:(i + 1) * 128, :], writes=[RP.r])
            p.op("act", lambda e, X=X, SM=SM: e.activation(out=junk.t[:], in_=X.t[:], func=AF.Square, accum_out=SM.t[:, 0:1]),
                 reads=[X.r], writes=[junk.r, SM.r])
            emit_rstd(cx, (SM, SM.t[:, 0:1]), (SM, SM.t[:, 1:2]), 1024.0)
            p.op("dve", lambda e, X=X, H=H, SM=SM, s=s: e.scalar_tensor_tensor(
                out=H.t[:], in0=X.t[:], scalar=SM.t[:, 1:2], in1=Abc[s].t[:], op0=ALU.mult, op1=ALU.mult),
                reads=[X.r, SM.r, Abc[s].r], writes=[H.r])
            p.op("pool", lambda e, H=H, s=s: e.tensor_tensor(out=H.t[:], in0=H.t[:], in1=Bbc[s].t[:], op=ALU.add),
                 reads=[H.r, Bbc[s].r], writes=[H.r])
            if DBG_CUT == 1:
                p.flush()
                return
            emit_transpose8(cx, H, HT, ptA, idn)
            for nb in range(4):
                c0, c1 = nb * 512, min(1952, (nb + 1) * 512)
                for kc in range(8):
                    p.op("pe", lambda e, HT=HT, kc=kc, c0=c0, c1=c1: e.matmul(
                        ppj.t[:, c0:c1], lhsT=HT.t[:, kc, :], rhs=w_in.t[:, kc, c0:c1], start=(kc == 0), stop=(kc == 7)),
                        reads=[HT.r, w_in.r], writes=[ppj.r])
            p.op("act", lambda e, PJ=PJ: e.activation(out=PJ.t[:, 0:1024], in_=ppj.t[:, 0:1024], func=AF.Copy),
                 reads=[ppj.r], writes=[PJ.r])
            p.op("dve", lambda e, PJ=PJ: e.tensor_copy(out=PJ.t[:, 1024:1952], in_=ppj.t[:, 1024:1952]),
                 reads=[ppj.r], writes=[PJ.r])
            if DBG_CUT == 2:
                p.flush()
                return
            p.op("act", lambda e, PJ=PJ, SM=SM: e.activation(out=junk.t[:, 0:256], in_=PJ.t[:, 0:256], func=AF.Square,
                                                             accum_out=SM.t[:, 2:3]), reads=[PJ.r], writes=[junk.r, SM.r])
            p.op("act", lambda e, PJ=PJ, SM=SM: e.activation(out=junk.t[:, 0:128], in_=PJ.t[:, 256:384], func=AF.Square,
                                                             accum_out=SM.t[:, 3:4]), reads=[PJ.r], writes=[junk.r, SM.r])
            emit_rstd(cx, (SM, SM.t[:, 2:3]), (SM, SM.t[:, 4:5]), 256.0)
            emit_rstd(cx, (SM, SM.t[:, 3:4]), (SM, SM.t[:, 5:6]), 128.0)
            p.op("dve", lambda e, PJ=PJ, CN=CN, SM=SM: e.scalar_tensor_tensor(
                out=CN.t[:, 0:256], in0=PJ.t[:, 0:256], scalar=SM.t[:, 4:5], in1=gq.t[:], op0=ALU.mult, op1=ALU.mult),
                reads=[PJ.r, SM.r, gq.r], writes=[CN.r])
            p.op("dve", lambda e, PJ=PJ, CN=CN, SM=SM: e.scalar_tensor_tensor(
                out=CN.t[:, 256:384], in0=PJ.t[:, 256:384], scalar=SM.t[:, 5:6], in1=gkv.t[:], op0=ALU.mult, op1=ALU.mult),
                reads=[PJ.r, SM.r, gkv.r], writes=[CN.r])
            for j in range(3):
                p.op("pe", lambda e, CN=CN, j=j: e.transpose(ptA.t[:, j * 128:(j + 1) * 128], CN.t[:, j * 128:(j + 1) * 128],
                                                             idn.t[:]), reads=[CN.r, idn.r], writes=[ptA.r])
            p.op("act", lambda e, CNT=CNT: e.activation(out=CNT.t[:].rearrange("p a b -> p (a b)"), in_=ptA.t[:, 0:384],
                                                        func=AF.Copy), reads=[ptA.r], writes=[CNT.r])
            if DBG_CUT == 3:
                p.flush()
                return
            for hb in range(2):
                p.op("pe", lambda e, CNT=CNT, hb=hb: e.matmul(pkv.t[:, hb * 512:(hb + 1) * 512], lhsT=CNT.t[:, 2, :],
                                                              rhs=w_ukv.t[:, hb * 512:(hb + 1) * 512], start=True, stop=True),
                     reads=[CNT.r, w_ukv.r], writes=[pkv.r])
            if DBG_CUT == 31:
                p.flush()
                return
            KT_, VM, VD, KP, TMP, DKR = Kt[b], Vm[b], Vd[b], kper[b], tmp[b], dkr[b]
            kvv = pkv.t[:].rearrange("p (h d) -> p h d", h=8)
            p.op("act", lambda e, KT_=KT_, kvv=kvv: e.activation(out=KT_.t[:, :, 0:64], in_=kvv[:, :, 0:64], func=AF.Copy),
                 reads=[pkv.r], writes=[KT_.r])
            if DBG_CUT == 32:
                p.flush()
                return
            p.op("act", lambda e, VM=VM, kvv=kvv: e.activation(out=VM.t[:, :, 0:64], in_=kvv[:, :, 64:128], func=AF.Copy),
                 reads=[pkv.r], writes=[VM.r])
            if DBG_CUT == 4:
                p.flush()
                return
            emit_rope(cx, "pool", PJ.t[:, 384:416].rearrange("p (h d) -> p h d", h=1), KP.t[:], TMP.t[:, :, 0:1, 0:16],
                      RP.t[:, 0:16], RP.t[:, 16:32], 1, 16, [PJ.r, RP.r], [KP.r, TMP.r])
            p.op("pool", lambda e, KT_=KT_, KP=KP: e.tensor_copy(out=KT_.t[:, :, 64:96], in_=KP.t[:, 0:1, :].to_broadcast([128, 8, 32])),
                 reads=[KP.r], writes=[KT_.r])
            if DBG_CUT == 5:
                p.flush()
                return
            emit_rope(cx, "dve", PJ.t[:, 928:1440].rearrange("p (h d) -> p h d", h=8), DKR.t[:], TMP.t[:],
                      RP.t[:, 32:64], RP.t[:, 64:96], 8, 32, [PJ.r, RP.r], [DKR.r, TMP.r])
            if DBG_CUT == 6:
                p.flush()
                return
            p.op("pool", lambda e, VD=VD, PJ=PJ: e.tensor_copy(out=VD.t[:, :, 0:128],
                                                               in_=PJ.t[:, 1440:1952].rearrange("p (h d) -> p h d", h=4)),
                 reads=[PJ.r], writes=[VD.r])
            if DBG_CUT == 7:
                p.flush()
                return
            for h in range(8):
                p.op("pe", lambda e, KT_=KT_, h=h: e.transpose(pkv.t[0:96, h * 128:(h + 1) * 128], KT_.t[:, h, :], idn.t[:]),
                     reads=[KT_.r, idn.r], writes=[pkv.r])
            KS, KDS = KTm_s[b], KTd_s[b]
            p.op("act", lambda e, KS=KS: e.activation(out=KS.t[:].rearrange("p a b -> p (a b)"), in_=pkv.t[0:96, :], func=AF.Copy),
                 reads=[pkv.r], writes=[KS.r])
            p.dma("sp", S["KTm"].t[:, :, i * 128:(i + 1) * 128], KS.t[:], reads=[KS.r], writes=[S["KTm"].r])
            for h in range(4):
                p.op("pe", lambda e, DKR=DKR, h=h: e.transpose(ptA.t[:, h * 128:(h + 1) * 128],
                                                               DKR.t[:, 2 * h:2 * h + 2, :].rearrange("p a b -> p (a b)"), idn.t[:]),
                     reads=[DKR.r, idn.r], writes=[ptA.r])
            p.op("dve", lambda e, KDS=KDS: e.tensor_copy(out=KDS.t[:].rearrange("p a b -> p (a b)"), in_=ptA.t[:, 0:512]),
                 reads=[ptA.r], writes=[KDS.r])
            p.dma("act", S["KTd"].t[:, :, i * 128:(i + 1) * 128], KDS.t[:], reads=[KDS.r], writes=[S["KTd"].r])
            p.dma("sp", S["Vm"].t[i * 128:(i + 1) * 128, :], VM.t[:].rearrange("p a b -> p (a b)"), reads=[VM.r], writes=[S["Vm"].r])
            p.dma("act", S["Vd"].t[i * 128:(i + 1) * 128, :], VD.t[:].rearrange("p a b -> p (a b)"), reads=[VD.r], writes=[S["Vd"].r])
            if DBG_CUT == 8:
                p.flush()
                return
            if not is_q:
                continue
            QR, QT_, DQR, QS, QDS = qraw[b], Qt[b], dqr[b], QTm_s[b], QTd_s[b]
            for (c0, c1) in ((0, 512), (512, 768)):
                for kc in range(2):
                    p.op("pe", lambda e, CNT=CNT, kc=kc, c0=c0, c1=c1: e.matmul(
                        pkv.t[:, c0:c1], lhsT=CNT.t[:, kc, :], rhs=w_uq.t[:, kc, c0:c1], start=(kc == 0), stop=(kc == 1)),
                        reads=[CNT.r, w_uq.r], writes=[pkv.r])
            p.op("act", lambda e, QR=QR: e.activation(out=QR.t[:].rearrange("p a b -> p (a b)"), in_=pkv.t[:, 0:768], func=AF.Copy),
                 reads=[pkv.r], writes=[QR.r])
            p.op("pool", lambda e, QR=QR, QT_=QT_: e.tensor_copy(out=QT_.t[:, :, 0:64], in_=QR.t[:, :, 0:64]),
                 reads=[QR.r], writes=[QT_.r])
            emit_rope(cx, "dve", QR.t[:, :, 64:96], QT_.t[:, :, 64:96], TMP.t[:, :, :, 0:16],
                      RP.t[:, 0:16], RP.t[:, 16:32], 8, 16, [QR.r, RP.r], [QT_.r, TMP.r])
            emit_rope(cx, "pool", PJ.t[:, 416:928].rearrange("p (h d) -> p h d", h=8), DQR.t[:], TMP.t[:],
                      RP.t[:, 32:64], RP.t[:, 64:96], 8, 32, [PJ.r, RP.r], [DQR.r, TMP.r])
            for h in range(8):
                p.op("pe", lambda e, QT_=QT_, h=h: e.transpose(pkv.t[0:96, h * 128:(h + 1) * 128], QT_.t[:, h, :], idn.t[:]),
                     reads=[QT_.r, idn.r], writes=[pkv.r])
            p.op("act", lambda e, QS=QS: e.activation(out=QS.t[:].rearrange("p a b -> p (a b)"), in_=pkv.t[0:96, :], func=AF.Copy),
                 reads=[pkv.r], writes=[QS.r])
            p.dma("sp", S["QTm"].t[:, :, i * 128:(i + 1) * 128], QS.t[:], reads=[QS.r], writes=[S["QTm"].r])
            for h in range(4):
                p.op("pe", lambda e, DQR=DQR, h=h: e.transpose(ptA.t[:, h * 128:(h + 1) * 128],
                                                               DQR.t[:, 2 * h:2 * h + 2, :].rearrange("p a b -> p (a b)"), idn.t[:]),
                     reads=[DQR.r, idn.r], writes=[ptA.r])
            p.op("dve", lambda e, QDS=QDS: e.tensor_copy(out=QDS.t[:].rearrange("p a b -> p (a b)"), in_=ptA.t[:, 0:512]),
                 reads=[ptA.r], writes=[QDS.r])
            p.dma("act", S["QTd"].t[:, :, i * 128:(i + 1) * 128], QDS.t[:], reads=[QDS.r], writes=[S["QTd"].r])
        p.flush()


def phase_attn_core(cx, xin, mod_d, S, W, x1_d):
    p, nc = cx.p, cx.nc
    with ExitStack() as st0:
        o_sb = cx.sb(st0, [128, NQ_T, 1024], BF16, "o_sb")
        rec = [cx.sb(st0, [128, 8], F32, "rec") for _ in range(4)]
        with ExitStack() as st:
            KTm = cx.sb(st, [96, 8, NK], BF16, "KTm")
            Vm = cx.sb(st, [128, NT_ALL, 528], BF16, "Vm")
            QTm = cx.sb(st, [96, 8, NQ], BF16, "QTm")
            for h in range(8):
                p.dma("sp" if h % 2 == 0 else "act", KTm.t[:, h, :], S["KTm"].t[:, h, :], reads=[S["KTm"].r], writes=[KTm.r])
            p.dma("sp", QTm.t[:], S["QTm"].t[:], reads=[S["QTm"].r], writes=[QTm.r])
            vv = S["Vm"].t.rearrange("(t p) f -> p t f", p=128)
            for t4 in range(0, NT_ALL, 6):
                t5 = min(NT_ALL, t4 + 6)
                p.dma("act", Vm.t[:, t4:t5, :], vv[:, t4:t5, :], reads=[S["Vm"].r], writes=[Vm.r])
            sc = [cx.ps(st, [128, 512], F32, "sc") for _ in range(4)]
            ops_ = [cx.ps(st, [128, 512], F32, "ops") for _ in range(4)]
            pT = [cx.sb(st, [128, 512], BF16, "pT") for _ in range(6)]
            k = 0
            for h in range(8):
                for g in range(5):
                    if g == 0:
                        q0, nq, kts = 0, 256, range(0, 2)
                    else:
                        q0, nq, kts = (2 + 4 * (g - 1)) * 128, 512, range(0, NT_ALL)
                    nj = nq // 128
                    kl = list(kts)
                    for kt in kl:
                        s_, pt_ = sc[k % 4], pT[k % 6]
                        k += 1
                        p.op("pe", lambda e, s_=s_, h=h, kt=kt, q0=q0, nq=nq: e.matmul(
                            s_.t[:, 0:nq], lhsT=KTm.t[:, h, kt * 128:(kt + 1) * 128], rhs=QTm.t[:, h, q0:q0 + nq],
                            start=True, stop=True), reads=[KTm.r, QTm.r], writes=[s_.r])
                        p.op("act", lambda e, s_=s_, pt_=pt_, nq=nq: e.activation(
                            out=pt_.t[:, 0:nq], in_=s_.t[:, 0:nq], func=AF.Exp, scale=MLA_SCALE),
                            reads=[s_.r], writes=[pt_.r])
                        for j in range(nj):
                            p.op("pe", lambda e, pt_=pt_, j=j, kt=kt, h=h, first=(kt == kl[0]), last=(kt == kl[-1]): e.matmul(
                                ops_[j].t[:, 0:66], lhsT=pt_.t[:, j * 128:(j + 1) * 128], rhs=Vm.t[:, kt, h * 66:(h + 1) * 66],
                                start=first, stop=last), reads=[pt_.r, Vm.r], writes=[ops_[j].r])
                    for j in range(nj):
                        ti = q0 // 128 + j
                        rc = rec[j]
                        p.op("dve", lambda e, j=j, rc=rc: e.reciprocal(out=rc.t[:, 0:1], in_=ops_[j].t[:, 64:65]),
                             reads=[ops_[j].r], writes=[rc.r])
                        p.op("dve", lambda e, j=j, rc=rc, ti=ti, h=h: e.tensor_scalar(
                            out=o_sb.t[:, ti, h * 64:(h + 1) * 64], in0=ops_[j].t[:, 0:64], scalar1=rc.t[:, 0:1], scalar2=None,
                            op0=ALU.mult), reads=[ops_[j].r, rc.r], writes=[o_sb.r])
            p.flush()
        if DBG_CUT == 101:
            return
        with ExitStack() as st:
            KTd = cx.sb(st, [128, 4, NK], BF16, "KTd")
            Vd = cx.sb(st, [128, NT_ALL, 520], BF16, "Vd")
            QTd = cx.sb(st, [128, 4, NQ], BF16, "QTd")
            for h in range(4):
                p.dma("sp" if h % 2 == 0 else "act", KTd.t[:, h, :], S["KTd"].t[:, h, :], reads=[S["KTd"].r], writes=[KTd.r])
            p.dma("sp", QTd.t[:], S["QTd"].t[:], reads=[S["QTd"].r], writes=[QTd.r])
            vv = S["Vd"].t.rearrange("(t p) f -> p t f", p=128)
            for t4 in range(0, NT_ALL, 6):
                t5 = min(NT_ALL, t4 + 6)
                p.dma("act", Vd.t[:, t4:t5, :], vv[:, t4:t5, :], reads=[S["Vd"].r], writes=[Vd.r])
            lrow = cx.sb(st, [1, 4, 64], F32, "lrow")
            lsm = cx.sb(st, [1, 8], F32, "lsm")
            ones = cx.sb(st, [1, 128], F32, "ones")
            lamn = cx.sb(st, [128, 1], F32, "lamn")
            subbc = cx.sb(st, [128, 128], F32, "subbc")
            for a, nm in enumerate(("lq1", "lk1", "lq2", "lk2")):
                p.dma("sp", lrow.t[0:1, a, :], W[nm].rearrange("(o n) -> o n", o=1), writes=[lrow.r])
            load_bc(cx, "sp", (subbc, subbc.t[:]), W["subln"], 128)
            p.op("dve", lambda e: e.tensor_scalar(out=subbc.t[:], in0=subbc.t[:], scalar1=(1.0 - LAMBDA_INIT0), scalar2=None,
                                                  op0=ALU.mult), reads=[subbc.r], writes=[subbc.r])
            p.op("dve", lambda e: e.memset(ones.t[:], 1.0), writes=[ones.r])
            p.op("dve", lambda e: e.tensor_tensor(out=lrow.t[0:1, 0, :], in0=lrow.t[0:1, 0, :], in1=lrow.t[0:1, 1, :], op=ALU.mult),
                 reads=[lrow.r], writes=[lrow.r])
            p.op("dve", lambda e: e.tensor_tensor(out=lrow.t[0:1, 2, :], in0=lrow.t[0:1, 2, :], in1=lrow.t[0:1, 3, :], op=ALU.mult),
                 reads=[lrow.r], writes=[lrow.r])
            p.op("dve", lambda e: e.tensor_reduce(out=lsm.t[0:1, 0:1], in_=lrow.t[0:1, 0, :], axis=AX.X, op=ALU.add),
                 reads=[lrow.r], writes=[lsm.r])
            p.op("dve", lambda e: e.tensor_reduce(out=lsm.t[0:1, 1:2], in_=lrow.t[0:1, 2, :], axis=AX.X, op=ALU.add),
                 reads=[lrow.r], writes=[lsm.r])
            p.op("act", lambda e: e.activation(out=lsm.t[0:1, 2:4], in_=lsm.t[0:1, 0:2], func=AF.Exp), reads=[lsm.r], writes=[lsm.r])
            p.op("dve", lambda e: e.tensor_tensor(out=lsm.t[0:1, 4:5], in0=lsm.t[0:1, 3:4], in1=lsm.t[0:1, 2:3], op=ALU.subtract),
                 reads=[lsm.r], writes=[lsm.r])
            p.op("dve", lambda e: e.tensor_scalar(out=lsm.t[0:1, 4:5], in0=lsm.t[0:1, 4:5], scalar1=-LAMBDA_INIT0, scalar2=None,
                                                  op0=ALU.add), reads=[lsm.r], writes=[lsm.r])
            sc = [cx.ps(st, [128, 512], F32, "scd") for _ in range(4)]
            ops_ = [[cx.ps(st, [128, 512], F32, "opd") for _ in range(2)] for _ in range(2)]
            pT = [cx.sb(st, [128, 256], BF16, "pTd") for _ in range(8)]
            dd = [cx.sb(st, [128, 128], F32, "dd") for _ in range(2)]
            tt = [cx.sb(st, [128, 128], F32, "tt") for _ in range(2)]
            junk = cx.sb(st, [128, 128], F32, "junkd")
            p.op("pe", lambda e: e.matmul(sc[0].t[:, 0:1], lhsT=ones.t[0:1, :], rhs=lsm.t[0:1, 4:5], start=True, stop=True),
                 reads=[ones.r, lsm.r], writes=[sc[0].r])
            p.op("dve", lambda e: e.tensor_copy(out=lamn.t[:], in_=sc[0].t[:, 0:1]), reads=[sc[0].r], writes=[lamn.r])
            k = 0
            for h in range(4):
                for g in range(9):
                    if g == 0:
                        q0, kts = 0, range(0, 2)
                    else:
                        q0, kts = (2 + 2 * (g - 1)) * 128, range(0, NT_ALL)
                    kl = list(kts)
                    for kt in kl:
                        for m in range(2):
                            s_, pt_ = sc[k % 4], pT[k % 8]
                            k += 1
                            p.op("pe", lambda e, s_=s_, h=h, kt=kt, q0=q0, m=m: e.matmul(
                                s_.t[:, 0:256], lhsT=KTd.t[m * 64:(m + 1) * 64, h, kt * 128:(kt + 1) * 128],
                                rhs=QTd.t[m * 64:(m + 1) * 64, h, q0:q0 + 256], start=True, stop=True),
                                reads=[KTd.r, QTd.r], writes=[s_.r])
                            p.op("act", lambda e, s_=s_, pt_=pt_: e.activation(
                                out=pt_.t[:, :], in_=s_.t[:, 0:256], func=AF.Exp, scale=DIFF_SCALE),
                                reads=[s_.r], writes=[pt_.r])
                            for j in range(2):
                                p.op("pe", lambda e, pt_=pt_, j=j, kt=kt, h=h, m=m, first=(kt == kl[0]), last=(kt == kl[-1]): e.matmul(
                                    ops_[m][j].t[:, 0:130], lhsT=pt_.t[:, j * 128:(j + 1) * 128], rhs=Vd.t[:, kt, h * 130:(h + 1) * 130],
                                    start=first, stop=last), reads=[pt_.r, Vd.r], writes=[ops_[m][j].r])
                    for j in range(2):
                        ti = q0 // 128 + j
                        rc, d_, t_ = rec[j], dd[j], tt[j]
                        o1, o2 = ops_[0][j], ops_[1][j]
                        p.op("dve", lambda e, rc=rc, o1=o1: e.reciprocal(out=rc.t[:, 0:1], in_=o1.t[:, 128:129]),
                             reads=[o1.r], writes=[rc.r])
                        p.op("dve", lambda e, rc=rc, o2=o2: e.reciprocal(out=rc.t[:, 1:2], in_=o2.t[:, 128:129]),
                             reads=[o2.r], writes=[rc.r])
                        p.op("dve", lambda e, rc=rc: e.tensor_tensor(out=rc.t[:, 1:2], in0=rc.t[:, 1:2], in1=lamn.t[:, 0:1], op=ALU.mult),
                             reads=[rc.r, lamn.r], writes=[rc.r])
                        p.op("dve", lambda e, rc=rc, o2=o2, t_=t_: e.tensor_scalar(out=t_.t[:], in0=o2.t[:, 0:128], scalar1=rc.t[:, 1:2],
                                                                              scalar2=None, op0=ALU.mult),
                             reads=[o2.r, rc.r], writes=[t_.r])
                        p.op("dve", lambda e, rc=rc, o1=o1, t_=t_, d_=d_: e.scalar_tensor_tensor(
                            out=d_.t[:], in0=o1.t[:, 0:128], scalar=rc.t[:, 0:1], in1=t_.t[:], op0=ALU.mult, op1=ALU.add),
                            reads=[o1.r, rc.r, t_.r], writes=[d_.r])
                        p.op("act", lambda e, d_=d_, rc=rc: e.activation(out=junk.t[:], in_=d_.t[:], func=AF.Square, accum_out=rc.t[:, 2:3]),
                             reads=[d_.r], writes=[junk.r, rc.r])
                        emit_rstd(cx, (rc, rc.t[:, 2:3]), (rc, rc.t[:, 3:4]), 128.0)
                        p.op("dve", lambda e, d_=d_, rc=rc, ti=ti, h=h: e.scalar_tensor_tensor(
                            out=o_sb.t[:, ti, 512 + h * 128:512 + (h + 1) * 128], in0=d_.t[:], scalar=rc.t[:, 3:4], in1=subbc.t[:],
                            op0=ALU.mult, op1=ALU.mult), reads=[d_.r, rc.r, subbc.r], writes=[o_sb.r])
            p.flush()
        if DBG_CUT == 102:
            return
        with ExitStack() as st:
            idf = make_ident(cx, st, F32)
            idb = cx.sb(st, [128, 128], BF16, "idb")
            p.op("dve", lambda e: e.tensor_copy(out=idb.t[:], in_=idf.t[:]), reads=[idf.r], writes=[idb.r])
            w_out = cx.sb(st, [128, 8, 1024], BF16, "w_out")
            wov = W["w_out"].rearrange("(kc p) n -> p kc n", p=128)
            for kc in range(8):
                p.dma("pool", w_out.t[:, kc, :], wov[:, kc, :], writes=[w_out.r])
            g1 = [cx.sb(st, [128, 1024], F32, "g1") for _ in range(2)]
            for s in range(2):
                load_bc(cx, "sp", (g1[s], g1[s].t[:]), mod_d.t[s, 2048:3072], 1024, src=mod_d)
            ptr = [cx.ps(st, [128, 1024], BF16, "ptr") for _ in range(2)]
            py = [cx.ps(st, [128, 1024], F32, "py") for _ in range(2)]
            oT = [cx.sb(st, [128, 8, 128], BF16, "oT") for _ in range(2)]
            x_t = [cx.sb(st, [128, 1024], F32, "x_t2") for _ in range(2)]
            y_t = [cx.sb(st, [128, 1024], F32, "y_t") for _ in range(2)]
            for i in range(NQ_T):
                b = i % 2
                s = 1 if i < 2 else 0
                X, Y, OT, PT, PY = x_t[b], y_t[b], oT[b], ptr[b], py[b]
                p.dma("sp", X.t[:], xin[i * 128:(i + 1) * 128, :], writes=[X.r])
                for kc in range(8):
                    p.op("pe", lambda e, PT=PT, i=i, kc=kc: e.transpose(PT.t[:, kc * 128:(kc + 1) * 128],
                                                                      o_sb.t[:, i, kc * 128:(kc + 1) * 128], idb.t[:]),
                         reads=[o_sb.r, idb.r], writes=[PT.r])
                p.op("act", lambda e, PT=PT, OT=OT: e.activation(out=OT.t[:].rearrange("p a b -> p (a b)"), in_=PT.t[:], func=AF.Copy),
                     reads=[PT.r], writes=[OT.r])
                for hb in range(2):
                    for kc in range(8):
                        p.op("pe", lambda e, PY=PY, OT=OT, hb=hb, kc=kc: e.matmul(
                            PY.t[:, hb * 512:(hb + 1) * 512], lhsT=OT.t[:, kc, :], rhs=w_out.t[:, kc, hb * 512:(hb + 1) * 512],
                            start=(kc == 0), stop=(kc == 7)), reads=[OT.r, w_out.r], writes=[PY.r])
                p.op("dve", lambda e, PY=PY, Y=Y, s=s: e.tensor_tensor(out=Y.t[:], in0=PY.t[:], in1=g1[s].t[:], op=ALU.mult),
                     reads=[PY.r, g1[s].r], writes=[Y.r])
                p.op("pool", lambda e, X=X, Y=Y: e.tensor_tensor(out=Y.t[:], in0=Y.t[:], in1=X.t[:], op=ALU.add),
                     reads=[X.r, Y.r], writes=[Y.r])
                p.dma("act", x1_d.t[i * 128:(i + 1) * 128, :], Y.t[:], reads=[Y.r], writes=[x1_d.r])
            p.flush()


ATT_W = (("w_in", [1024, 1952]), ("q_norm", [256]), ("w_uq", [256, 768]), ("kv_norm", [128]), ("w_ukv", [128, 1024]),
         ("lq1", [64]), ("lk1", [64]), ("lq2", [64]), ("lk2", [64]), ("subln", [128]), ("w_out", [1024, 1024]))
MOE_W = (("w_rg", [1024, 4]), ("b_rg", [4]), ("w_rf", [1024, 32]), ("b_rf", [32]),
         ("w_gate", [32, 1024, 512]), ("w_up", [32, 1024, 512]), ("w_down", [32, 512, 1024]))


def build_A(stop_after=None):
    nc = bass.Bass("TRN2", target_bir_lowering=False)
    inp = lambda name, shape: nc.dram_tensor(name, list(shape), F32, kind="ExternalInput").ap()
    xin = inp("xin", [NK, 1024])
    cvec = inp("cvec", [2, 1024])
    rope = inp("rope", [NK, 96])
    w_mod = inp("w_mod", [1024, 6144])
    b_mod = inp("b_mod", [6144])
    norm_g = inp("norm_g", [2, 1024])
    W = {k: inp("att_" + k, s) for k, s in ATT_W}
    M = {k: inp("moe_" + k, s) for k, s in MOE_W}
    xout = nc.dram_tensor("xout", [NQ, 1024], F32, kind="ExternalOutput").ap()
    with ExitStack() as st:
        cx = Ctx(nc, st)
        mod_d = cx.dram([2, 6144], F32, "mod_d")
        S = {"KTm": cx.dram([96, 8, NK], BF16, "KTm_d"), "QTm": cx.dram([96, 8, NQ], BF16, "QTm_d"),
             "KTd": cx.dram([128, 4, NK], BF16, "KTd_d"), "QTd": cx.dram([128, 4, NQ], BF16, "QTd_d"),
             "Vm": cx.dram([NK, 528], BF16, "Vm_d"), "Vd": cx.dram([NK, 520], BF16, "Vd_d")}
        phase_mod(cx, cvec, w_mod, b_mod, mod_d)
        if stop_after == "mod":
            cx.p.dma("sp", xout[0:12, :].rearrange("(s a) d -> s (a d)", s=2), mod_d.t[:, :], reads=[mod_d.r])
            cx.p.flush()
            return nc
        phase_attn_prep(cx, xin, rope, mod_d, norm_g[0, :], W, S)
        if stop_after == "prep":
            cx.p.dma("sp", xout[0:12, :].rearrange("(s a) d -> s (a d)", s=2), mod_d.t[:, :], reads=[mod_d.r, S["Vd"].r, S["QTd"].r])
            cx.p.flush()
            return nc
        if stop_after == "attn":
            x1_d = TL(xout)
            phase_attn_core(cx, xin, mod_d, S, W, x1_d)
            return nc
        x1_d = cx.dram([NQ, 1024], F32, "x1_d")
        phase_attn_core(cx, xin, mod_d, S, W, x1_d)
        phase_moe(cx, x1_d, NQ_T, mod_d, norm_g[1, :], M, TL(xout), n_ctx_tiles=2)
    return nc


def rope_tables():
    rows = 64
    row = np.repeat(np.arange(rows, dtype=np.float32), 64)
    col = np.tile(np.arange(64, dtype=np.float32), rows)
    out = []
    for rot in (32, 64):
        nf = rot // 4
        inv = (np.float32(10000.0) ** (-np.arange(nf, dtype=np.float32) / np.float32(nf))).astype(np.float32)
        ang = np.concatenate([row[:, None] * inv, col[:, None] * inv], axis=-1).astype(np.float32)
        out += [np.cos(ang), np.sin(ang)]
    return np.concatenate(out, axis=-1).astype(np.float32)


def inputs_A(core, x, c, ctx, c_ctx, w_mod, b_mod, norm_g, att, moe):
    b, hf = core // 2, core % 2
    own = x[b, hf * 2048:(hf + 1) * 2048]
    oth = x[b, (1 - hf) * 2048:(2 - hf) * 2048]
    rt = rope_tables()
    rc = np.zeros((256, 96), np.float32)
    rc[:, 0:16] = 1.0
    rc[:, 32:64] = 1.0
    d = {"xin": np.ascontiguousarray(np.concatenate([ctx[b], own, oth], axis=0)),
         "cvec": np.ascontiguousarray(np.stack([c[b], c_ctx], axis=0)),
         "rope": np.ascontiguousarray(np.concatenate([rc, rt[hf * 2048:(hf + 1) * 2048], rt[(1 - hf) * 2048:(2 - hf) * 2048]], axis=0)),
         "w_mod": w_mod[0], "b_mod": b_mod[0], "norm_g": norm_g[0]}
    for k, s in ATT_W:
        d["att_" + k] = att[k][0]
    for k, s in MOE_W:
        d["moe_" + k] = np.ascontiguousarray(moe[k][0].reshape(s))
    return d


def emit_router_fine(cx, RT, R_, comb, i):
    p = cx.p
    lf = RT.t[:, 0, 4:36].rearrange("p (g e) -> p g e", g=4)
    mk1 = RT.t[:, 3, 0:32].rearrange("p (g e) -> p g e", g=4)
    lf2 = RT.t[:, 4, 0:32].rearrange("p (g e) -> p g e", g=4)
    mk2 = RT.t[:, 5, 0:32].rearrange("p (g e) -> p g e", g=4)
    fine = RT.t[:, 6, 0:32].rearrange("p (g e) -> p g e", g=4)
    m1, m2, w1, w2 = RT.t[:, 7, 0:4], RT.t[:, 7, 4:8], RT.t[:, 7, 8:12], RT.t[:, 7, 12:16]
    bc = lambda a: a.unsqueeze(2).to_broadcast([128, 4, 8])
    p.op("dve", lambda e: e.tensor_reduce(out=m1, in_=lf, axis=AX.X, op=ALU.max), reads=R_, writes=R_)
    p.op("dve", lambda e: e.tensor_tensor(out=mk1, in0=lf, in1=bc(m1), op=ALU.is_equal), reads=R_, writes=R_)
    p.op("dve", lambda e: e.scalar_tensor_tensor(out=lf2, in0=mk1, scalar=-1e30, in1=lf, op0=ALU.mult, op1=ALU.add),
         reads=R_, writes=R_)
    p.op("dve", lambda e: e.tensor_reduce(out=m2, in_=lf2, axis=AX.X, op=ALU.max), reads=R_, writes=R_)
    p.op("dve", lambda e: e.tensor_tensor(out=mk2, in0=lf2, in1=bc(m2), op=ALU.is_equal), reads=R_, writes=R_)
    p.op("dve", lambda e: e.tensor_tensor(out=w2, in0=m2, in1=m1, op=ALU.subtract), reads=R_, writes=R_)
    p.op("act", lambda e: e.activation(out=w2, in_=w2, func=AF.Exp), reads=R_, writes=R_)
    p.op("dve", lambda e: e.tensor_scalar(out=w1, in0=w2, scalar1=1.0, scalar2=None, op0=ALU.add), reads=R_, writes=R_)
    p.op("dve", lambda e: e.reciprocal(out=w1, in_=w1), reads=R_, writes=R_)
    p.op("dve", lambda e: e.tensor_tensor(out=w2, in0=w2, in1=w1, op=ALU.mult), reads=R_, writes=R_)
    p.op("dve", lambda e, RT=RT: e.tensor_tensor(out=w1, in0=w1, in1=RT.t[:, 2, 0:4], op=ALU.mult), reads=R_, writes=R_)
    p.op("dve", lambda e, RT=RT: e.tensor_tensor(out=w2, in0=w2, in1=RT.t[:, 2, 0:4], op=ALU.mult), reads=R_, writes=R_)
    p.op("dve", lambda e: e.tensor_tensor(out=mk1, in0=mk1, in1=bc(w1), op=ALU.mult), reads=R_, writes=R_)
    p.op("dve", lambda e: e.tensor_tensor(out=mk2, in0=mk2, in1=bc(w2), op=ALU.mult), reads=R_, writes=R_)
    p.op("dve", lambda e, i=i, RT=RT: e.tensor_tensor(out=comb.t[:, i, :], in0=RT.t[:, 3, 0:32], in1=RT.t[:, 5, 0:32], op=ALU.add),
         reads=R_, writes=[comb.r])


def phase_moe(cx, x_d, ntiles, mod_d, g_norm, M, out_d, n_ctx_tiles):
    p, nc = cx.p, cx.nc
    T = ntiles * 128
    with ExitStack() as st0:
        hT = cx.sb(st0, [128, 8, T], BF16, "hT_all")
        comb = cx.sb(st0, [128, ntiles, 32], F32, "comb")
        acc = cx.sb(st0, [128, ntiles, 1024], F32, "acc")
        with ExitStack() as st:
            idn = make_ident(cx, st, F32)
            gbc = cx.sb(st, [128, 1024], F32, "gbc2")
            nsrc = 2 if n_ctx_tiles > 0 else 1
            Abc = [cx.sb(st, [128, 1024], F32, "Abc2") for _ in range(nsrc)]
            Bbc = [cx.sb(st, [128, 1024], F32, "Bbc2") for _ in range(nsrc)]
            load_bc(cx, "sp", (gbc, gbc.t[:]), g_norm, 1024)
            for s in range(nsrc):
                load_bc(cx, "sp", (Bbc[s], Bbc[s].t[:]), mod_d.t[s, 3072:4096], 1024, src=mod_d)
                load_bc(cx, "act", (Abc[s], Abc[s].t[:]), mod_d.t[s, 4096:5120], 1024, src=mod_d)
                p.op("dve", lambda e, s=s: e.scalar_tensor_tensor(out=Abc[s].t[:], in0=Abc[s].t[:], scalar=1.0, in1=gbc.t[:],
                                                                  op0=ALU.add, op1=ALU.mult),
                     reads=[Abc[s].r, gbc.r], writes=[Abc[s].r])
            wr = cx.sb(st, [128, 8, 36], F32, "wr")
            brbc = cx.sb(st, [128, 36], F32, "brbc")
            p.dma("sp", wr.t[:, :, 0:4], M["w_rg"].rearrange("(kc p) n -> p kc n", p=128), writes=[wr.r])
            p.dma("sp", wr.t[:, :, 4:36], M["w_rf"].rearrange("(kc p) n -> p kc n", p=128), writes=[wr.r])
            load_bc(cx, "act", (brbc, brbc.t[:, 0:4]), M["b_rg"], 4)
            load_bc(cx, "act", (brbc, brbc.t[:, 4:36]), M["b_rf"], 32)
            x_t = [cx.sb(st, [128, 1024], F32, "x_t3") for _ in range(2)]
            h_t = [cx.sb(st, [128, 1024], F32, "h_t3") for _ in range(2)]
            junk = cx.sb(st, [128, 1024], F32, "junk3")
            hT32 = [cx.sb(st, [128, 8, 128], F32, "hT32") for _ in range(2)]
            sm = [cx.sb(st, [128, 8], F32, "sm3") for _ in range(2)]
            rt = [cx.sb(st, [128, 8, 36], F32, "rt3") for _ in range(2)]
            ptA = cx.ps(st, [128, 1024], F32, "ptA3")
            plg = cx.ps(st, [128, 512], F32, "plg")
            for i in range(ntiles):
                b = i % 2
                s = 1 if i < n_ctx_tiles else 0
                X, H, SM, H32, RT = x_t[b], h_t[b], sm[b], hT32[b], rt[b]
                if DBG_CUT == 210:
                    p.flush()
                    return
                p.dma("sp", X.t[:], x_d.t[i * 128:(i + 1) * 128, :], reads=[x_d.r], writes=[X.r])
                p.op("act", lambda e, X=X, SM=SM: e.activation(out=junk.t[:], in_=X.t[:], func=AF.Square, accum_out=SM.t[:, 0:1]),
                     reads=[X.r], writes=[junk.r, SM.r])
                emit_rstd(cx, (SM, SM.t[:, 0:1]), (SM, SM.t[:, 1:2]), 1024.0)
                p.op("dve", lambda e, X=X, H=H, SM=SM, s=s: e.scalar_tensor_tensor(
                    out=H.t[:], in0=X.t[:], scalar=SM.t[:, 1:2], in1=Abc[s].t[:], op0=ALU.mult, op1=ALU.mult),
                    reads=[X.r, SM.r, Abc[s].r], writes=[H.r])
                p.op("pool", lambda e, H=H, s=s: e.tensor_tensor(out=H.t[:], in0=H.t[:], in1=Bbc[s].t[:], op=ALU.add),
                     reads=[H.r, Bbc[s].r], writes=[H.r])
                for kc in range(8):
                    p.op("pe", lambda e, H=H, kc=kc: e.transpose(ptA.t[:, kc * 128:(kc + 1) * 128], H.t[:, kc * 128:(kc + 1) * 128],
                                                                 idn.t[:]), reads=[H.r, idn.r], writes=[ptA.r])
                p.op("act", lambda e, i=i: e.activation(out=hT.t[:, :, i * 128:(i + 1) * 128],
                                                        in_=ptA.t[:].rearrange("p (a b) -> p a b", a=8), func=AF.Copy),
                     reads=[ptA.r], writes=[hT.r])
                if DBG_CUT == 2105:
                    p.flush()
                    return
                p.op("dve", lambda e, H32=H32: e.tensor_copy(out=H32.t[:].rearrange("p a b -> p (a b)"), in_=ptA.t[:]),
                     reads=[ptA.r, hT.r], writes=[H32.r])
                if DBG_CUT == 211:
                    p.flush()
                    return
                for kc in range(8):
                    p.op("pe", lambda e, H32=H32, kc=kc: e.matmul(plg.t[:, 0:36], lhsT=H32.t[:, kc, :], rhs=wr.t[:, kc, :],
                                                                 start=(kc == 0), stop=(kc == 7)), reads=[H32.r, wr.r], writes=[plg.r])
                R_ = [RT.r]
                p.op("dve", lambda e, RT=RT: e.tensor_tensor(out=RT.t[:, 0, :], in0=plg.t[:, 0:36], in1=brbc.t[:], op=ALU.add),
                     reads=[plg.r, brbc.r], writes=R_)
                if DBG_CUT == 212:
                    p.flush()
                    return
                p.op("dve", lambda e, RT=RT, SM=SM: e.tensor_reduce(out=SM.t[:, 2:3], in_=RT.t[:, 0, 0:4], axis=AX.X, op=ALU.max),
                     reads=R_, writes=[SM.r])
                p.op("dve", lambda e, RT=RT, SM=SM: e.tensor_scalar(out=SM.t[:, 3:4], in0=SM.t[:, 2:3], scalar1=-1.0, scalar2=None,
                                                                    op0=ALU.mult), reads=[SM.r], writes=[SM.r])
                p.op("act", lambda e, RT=RT, SM=SM: e.activation(out=RT.t[:, 1, 0:4], in_=RT.t[:, 0, 0:4], func=AF.Exp,
                                                                 bias=SM.t[:, 3:4], scale=1.0, accum_out=SM.t[:, 4:5]),
                     reads=R_ + [SM.r], writes=R_ + [SM.r])
                p.op("dve", lambda e, SM=SM: e.reciprocal(out=SM.t[:, 5:6], in_=SM.t[:, 4:5]), reads=[SM.r], writes=[SM.r])
                p.op("dve", lambda e, RT=RT, SM=SM: e.tensor_scalar(out=RT.t[:, 2, 0:4], in0=RT.t[:, 0, 0:4], scalar1=SM.t[:, 2:3],
                                                                    scalar2=SM.t[:, 5:6], op0=ALU.is_equal, op1=ALU.mult),
                     reads=R_ + [SM.r], writes=R_)
                if DBG_CUT == 213:
                    p.flush()
                    return
                emit_router_fine(cx, RT, R_, comb, i)
            p.flush()
        if DBG_CUT == 201:
            return
        with ExitStack() as st:
            wg = [cx.sb(st, [128, 8, 512], BF16, "wg") for _ in range(2)]
            wu = [cx.sb(st, [128, 8, 512], BF16, "wu") for _ in range(2)]
            wd = [cx.sb(st, [128, 4, 1024], BF16, "wd") for _ in range(2)]
            actT = cx.sb(st, [128, 4, T], BF16, "actT")
            sg = [cx.sb(st, [128, 512], F32, "sg") for _ in range(2)]
            pg = [cx.ps(st, [128, 512], F32, "pg") for _ in range(2)]
            pu = [cx.ps(st, [128, 512], F32, "pu") for _ in range(2)]
            pd = [cx.ps(st, [128, 512], F32, "pd") for _ in range(2)]
            groups = [(t0, min(512, T - t0)) for t0 in range(0, T, 512)]
            k = 0
            kd = 0
            for ex in range(32):
                b = ex % 2
                WG, WU, WD = wg[b], wu[b], wd[b]
                p.dma("pool", WG.t[:], M["w_gate"][ex].rearrange("(kc p) n -> p kc n", p=128), writes=[WG.r])
                p.dma("pool", WU.t[:], M["w_up"][ex].rearrange("(kc p) n -> p kc n", p=128), writes=[WU.r])
                p.dma("pool", WD.t[:], M["w_down"][ex].rearrange("(fc p) n -> p fc n", p=128), writes=[WD.r])
                for fc in range(4):
                    for (t0, n) in groups:
                        PG, PU, SG = pg[k % 2], pu[k % 2], sg[k % 2]
                        k += 1
                        for kc in range(8):
                            p.op("pe", lambda e, PG=PG, WG=WG, kc=kc, fc=fc, t0=t0, n=n: e.matmul(
                                PG.t[:, 0:n], lhsT=WG.t[:, kc, fc * 128:(fc + 1) * 128], rhs=hT.t[:, kc, t0:t0 + n],
                                start=(kc == 0), stop=(kc == 7)), reads=[WG.r, hT.r], writes=[PG.r])
                        for kc in range(8):
                            p.op("pe", lambda e, PU=PU, WU=WU, kc=kc, fc=fc, t0=t0, n=n: e.matmul(
                                PU.t[:, 0:n], lhsT=WU.t[:, kc, fc * 128:(fc + 1) * 128], rhs=hT.t[:, kc, t0:t0 + n],
                                start=(kc == 0), stop=(kc == 7)), reads=[WU.r, hT.r], writes=[PU.r])
                        p.op("act", lambda e, PG=PG, SG=SG, n=n: e.activation(out=SG.t[:, 0:n], in_=PG.t[:, 0:n], func=AF.Silu),
                             reads=[PG.r], writes=[SG.r])
                        p.op("dve", lambda e, PU=PU, SG=SG, fc=fc, t0=t0, n=n: e.tensor_tensor(
                            out=actT.t[:, fc, t0:t0 + n], in0=SG.t[:, 0:n], in1=PU.t[:, 0:n], op=ALU.mult),
                            reads=[SG.r, PU.r], writes=[actT.r])
                for t in range(ntiles):
                    for hb in range(2):
                        PD = pd[kd % 2]
                        kd += 1
                        for fc in range(4):
                            p.op("pe", lambda e, PD=PD, WD=WD, fc=fc, t=t, hb=hb: e.matmul(
                                PD.t[:], lhsT=actT.t[:, fc, t * 128:(t + 1) * 128], rhs=WD.t[:, fc, hb * 512:(hb + 1) * 512],
                                start=(fc == 0), stop=(fc == 3)), reads=[actT.r, WD.r], writes=[PD.r])
                        if ex == 0:
                            p.op("dve", lambda e, PD=PD, t=t, hb=hb, ex=ex: e.tensor_scalar(
                                out=acc.t[:, t, hb * 512:(hb + 1) * 512], in0=PD.t[:], scalar1=comb.t[:, t, ex:ex + 1], scalar2=None,
                                op0=ALU.mult), reads=[PD.r, comb.r], writes=[acc.r])
                        else:
                            p.op("dve", lambda e, PD=PD, t=t, hb=hb, ex=ex: e.scalar_tensor_tensor(
                                out=acc.t[:, t, hb * 512:(hb + 1) * 512], in0=PD.t[:], scalar=comb.t[:, t, ex:ex + 1],
                                in1=acc.t[:, t, hb * 512:(hb + 1) * 512], op0=ALU.mult, op1=ALU.add),
                                reads=[PD.r, comb.r, acc.r], writes=[acc.r])
            p.flush()
        if DBG_CUT == 202:
            return
        with ExitStack() as st:
            nsrc = 2 if n_ctx_tiles > 0 else 1
            g2 = [cx.sb(st, [128, 1024], F32, "g2") for _ in range(nsrc)]
            for s in range(nsrc):
                load_bc(cx, "sp", (g2[s], g2[s].t[:]), mod_d.t[s, 5120:6144], 1024, src=mod_d)
            x_t = [cx.sb(st, [128, 1024], F32, "x_t4") for _ in range(2)]
            for i in range(ntiles):
                X = x_t[i % 2]
                s = 1 if i < n_ctx_tiles else 0
                p.dma("sp", X.t[:], x_d.t[i * 128:(i + 1) * 128, :], reads=[x_d.r], writes=[X.r])
                p.op("dve", lambda e, i=i, s=s: e.tensor_tensor(out=acc.t[:, i, :], in0=acc.t[:, i, :], in1=g2[s].t[:], op=ALU.mult),
                     reads=[acc.r, g2[s].r], writes=[acc.r])
                p.op("pool", lambda e, i=i, X=X: e.tensor_tensor(out=X.t[:], in0=X.t[:], in1=acc.t[:, i, :], op=ALU.add),
                     reads=[acc.r, X.r], writes=[X.r])
                p.dma("act", out_d.t[i * 128:(i + 1) * 128, :], X.t[:], reads=[X.r], writes=[out_d.r])
            p.flush()


NCH = 34


def tri_const(cx, st, kind):
    t = cx.sb(st, [128, 128], F32, "tri")
    pat, cm, op = {"p_le_j": ([[1, 128]], -1, ALU.is_ge), "p_ge_j": ([[-1, 128]], 1, ALU.is_ge),
                   "p_gt_j": ([[-1, 128]], 1, ALU.is_gt), "p_lt_j": ([[1, 128]], -1, ALU.is_gt)}[kind]
    cx.p.op("pool", lambda e: e.memset(t.t[:], 1.0), writes=[t.r])
    cx.p.op("pool", lambda e: e.affine_select(out=t.t[:], in_=t.t[:], pattern=pat, compare_op=op, fill=0.0, base=0,
                                              channel_multiplier=cm), reads=[t.r], writes=[t.r])
    return t


def phase_ssd_prep(cx, xin, mod_d, g_norm, Wd, S):
    p, nc = cx.p, cx.nc
    w_in = Wd["w_in"]
    with ExitStack() as st0:
        hT = cx.sb(st0, [128, 8, NK], BF16, "hT_ssd")
        idn = make_ident(cx, st0, F32)
        idb = cx.sb(st0, [128, 128], BF16, "idb")
        p.op("dve", lambda e: e.tensor_copy(out=idb.t[:], in_=idn.t[:]), reads=[idn.r], writes=[idb.r])
        with ExitStack() as st:
            gbc = cx.sb(st, [128, 1024], F32, "gbc5")
            Abc = [cx.sb(st, [128, 1024], F32, "Abc5") for _ in range(2)]
            Bbc = [cx.sb(st, [128, 1024], F32, "Bbc5") for _ in range(2)]
            load_bc(cx, "sp", (gbc, gbc.t[:]), g_norm, 1024)
            for s in range(2):
                load_bc(cx, "sp", (Bbc[s], Bbc[s].t[:]), mod_d.t[s, 0:1024], 1024, src=mod_d)
                load_bc(cx, "act", (Abc[s], Abc[s].t[:]), mod_d.t[s, 1024:2048], 1024, src=mod_d)
                p.op("dve", lambda e, s=s: e.scalar_tensor_tensor(out=Abc[s].t[:], in0=Abc[s].t[:], scalar=1.0, in1=gbc.t[:],
                                                                  op0=ALU.add, op1=ALU.mult),
                     reads=[Abc[s].r, gbc.r], writes=[Abc[s].r])
            w_dt = cx.sb(st, [128, 8, 64], BF16, "w_dt")
            p.dma("pool", w_dt.t[:], w_in.rearrange("(kc p) n -> p kc n", p=128)[:, :, 5120:5184], writes=[w_dt.r])
            dtb = cx.sb(st, [128, 64], F32, "dtb")
            load_bc(cx, "sp", (dtb, dtb.t[:]), Wd["dt_bias"], 64)
            x_t = [cx.sb(st, [128, 1024], F32, "x_t5") for _ in range(2)]
            h_t = [cx.sb(st, [128, 1024], F32, "h_t5") for _ in range(2)]
            junk = cx.sb(st, [128, 1024], F32, "junk5")
            sm = [cx.sb(st, [128, 8], F32, "sm5") for _ in range(2)]
            dts = [cx.sb(st, [128, 64], F32, "dts") for _ in range(2)]
            ptA = cx.ps(st, [128, 1024], F32, "ptA5")
            pdt = cx.ps(st, [128, 512], F32, "pdt")
            for i in range(NCH):
                b = i % 2
                s = 1 if i < 2 else 0
                X, H, SM, DT = x_t[b], h_t[b], sm[b], dts[b]
                xap, xrd = xin(i) if callable(xin) else (xin[i * 128:(i + 1) * 128, :], [])
                p.dma("sp", X.t[:], xap, reads=xrd, writes=[X.r])
                p.op("act", lambda e, X=X, SM=SM: e.activation(out=junk.t[:], in_=X.t[:], func=AF.Square, accum_out=SM.t[:, 0:1]),
                     reads=[X.r], writes=[junk.r, SM.r])
                emit_rstd(cx, (SM, SM.t[:, 0:1]), (SM, SM.t[:, 1:2]), 1024.0)
                p.op("dve", lambda e, X=X, H=H, SM=SM, s=s: e.scalar_tensor_tensor(
                    out=H.t[:], in0=X.t[:], scalar=SM.t[:, 1:2], in1=Abc[s].t[:], op0=ALU.mult, op1=ALU.mult),
                    reads=[X.r, SM.r, Abc[s].r], writes=[H.r])
                p.op("pool", lambda e, H=H, s=s: e.tensor_tensor(out=H.t[:], in0=H.t[:], in1=Bbc[s].t[:], op=ALU.add),
                     reads=[H.r, Bbc[s].r], writes=[H.r])
                for kc in range(8):
                    p.op("pe", lambda e, H=H, kc=kc: e.transpose(ptA.t[:, kc * 128:(kc + 1) * 128], H.t[:, kc * 128:(kc + 1) * 128],
                                                                 idn.t[:]), reads=[H.r, idn.r], writes=[ptA.r])
                p.op("act", lambda e, i=i: e.activation(out=hT.t[:, :, i * 128:(i + 1) * 128],
                                                        in_=ptA.t[:].rearrange("p (a b) -> p a b", a=8), func=AF.Copy),
                     reads=[ptA.r], writes=[hT.r])
                for kc in range(8):
                    p.op("pe", lambda e, i=i, kc=kc: e.matmul(pdt.t[:, 0:64], lhsT=hT.t[:, kc, i * 128:(i + 1) * 128], rhs=w_dt.t[:, kc, :],
                                                              start=(kc == 0), stop=(kc == 7)), reads=[hT.r, w_dt.r], writes=[pdt.r])
                p.op("dve", lambda e, DT=DT: e.tensor_tensor(out=DT.t[:], in0=pdt.t[:, 0:64], in1=dtb.t[:], op=ALU.add),
                     reads=[pdt.r, dtb.r], writes=[DT.r])
                p.op("act", lambda e, DT=DT: e.activation(out=DT.t[:], in_=DT.t[:], func=AF.Exp), reads=[DT.r], writes=[DT.r])
                p.op("act", lambda e, DT=DT: e.activation(out=DT.t[:], in_=DT.t[:], func=AF.Ln, bias=1.0, scale=1.0),
                     reads=[DT.r], writes=[DT.r])
                p.dma("act", S["dt"].t[i * 128:(i + 1) * 128, :], DT.t[:], reads=[DT.r], writes=[S["dt"].r])
            p.flush()
        with ExitStack() as st:
            w_x = cx.sb(st, [128, 8, 3072], BF16, "w_x")
            wv = w_in.rearrange("(kc p) n -> p kc n", p=128)
            for kc in range(8):
                p.dma("pool", w_x.t[:, kc, :], wv[:, kc, 2048:5120], writes=[w_x.r])
            cwr = cx.sb(st, [6, 3072], F32, "cwr")
            cw = cx.sb(st, [128, 24, 6], F32, "cw")
            p.dma("sp", cwr.t[0:5, :], Wd["conv_w"], writes=[cwr.r])
            p.dma("sp", cwr.t[5:6, :], Wd["conv_b"].rearrange("(o n) -> o n", o=1), writes=[cwr.r])
            pcw = cx.ps(st, [128, 512], F32, "pcw")
            for cc in range(24):
                p.op("pe", lambda e, cc=cc: e.transpose(pcw.t[:, cc * 6:(cc + 1) * 6], cwr.t[0:6, cc * 128:(cc + 1) * 128], idn.t[0:6, 0:6]),
                     reads=[cwr.r, idn.r], writes=[pcw.r])
            p.op("dve", lambda e: e.tensor_copy(out=cw.t[:].rearrange("p a b -> p (a b)"), in_=pcw.t[:, 0:144]), reads=[pcw.r], writes=[cw.r])
            U = cx.sb(st, [128, 4100], F32, "U")
            Uc = cx.sb(st, [128, 260], F32, "Uc")
            A = cx.sb(st, [128, 4096], F32, "Aconv")
            Ac = cx.sb(st, [128, 256], F32, "Acc")
            Vb = [cx.sb(st, [128, NK], BF16, "Vb") for _ in range(2)]
            xst = [cx.sb(st, [128, NCH, 128], BF16, "xst") for _ in range(2)]
            pp = [cx.ps(st, [128, 512], F32, "pp") for _ in range(2)]
            ptb = [cx.ps(st, [128, 1024], BF16, "ptb") for _ in range(2)]
            p.op("pool", lambda e: e.memset(U.t[:], 0.0), writes=[U.r])
            p.op("pool", lambda e: e.memset(Uc.t[:], 0.0), writes=[Uc.r])
            k = 0
            kt_ = 0
            for cc in range(24):
                V = Vb[cc % 2]
                groups = [(0, 256)] + [(256 + 512 * g, 512) for g in range(8)]
                for (t0, n) in groups:
                    P_ = pp[k % 2]
                    k += 1
                    for kc in range(8):
                        p.op("pe", lambda e, P_=P_, kc=kc, cc=cc, t0=t0, n=n: e.matmul(
                            P_.t[:, 0:n], lhsT=w_x.t[:, kc, cc * 128:(cc + 1) * 128], rhs=hT.t[:, kc, t0:t0 + n],
                            start=(kc == 0), stop=(kc == 7)), reads=[w_x.r, hT.r], writes=[P_.r])
                    if t0 == 0:
                        p.op("act", lambda e, P_=P_: e.activation(out=Uc.t[:, 2:258], in_=P_.t[:, 0:256], func=AF.Copy),
                             reads=[P_.r], writes=[Uc.r])
                    else:
                        p.op("act", lambda e, P_=P_, t0=t0: e.activation(out=U.t[:, 2 + t0 - 256:2 + t0 - 256 + 512], in_=P_.t[:, 0:512], func=AF.Copy),
                             reads=[P_.r], writes=[U.r])
                for (src, acc_, n, eng, off) in ((Uc, Ac, 256, "dve", 0), (U, A, 4096, "dve", 256)):
                    p.op(eng, lambda e, src=src, acc_=acc_, n=n, cc=cc: e.tensor_scalar(
                        out=acc_.t[:, 0:n], in0=src.t[:, 0:n], scalar1=cw.t[:, cc, 0:1], scalar2=None, op0=ALU.mult),
                        reads=[src.r, cw.r], writes=[acc_.r])
                    for kk in range(1, 5):
                        e2 = "dve"
                        p.op(e2, lambda e, src=src, acc_=acc_, n=n, cc=cc, kk=kk: e.scalar_tensor_tensor(
                            out=acc_.t[:, 0:n], in0=src.t[:, kk:kk + n], scalar=cw.t[:, cc, kk:kk + 1], in1=acc_.t[:, 0:n],
                            op0=ALU.mult, op1=ALU.add), reads=[src.r, cw.r, acc_.r], writes=[acc_.r])
                    p.op("act", lambda e, acc_=acc_, n=n, cc=cc, V=V, off=off: e.activation(
                        out=V.t[:, off:off + n], in_=acc_.t[:, 0:n], func=AF.Silu, bias=cw.t[:, cc, 5:6], scale=1.0),
                        reads=[acc_.r, cw.r], writes=[V.r])
                if cc >= 16:
                    p.dma("sp", S["BCT"].t[:, cc - 16, :], V.t[:], reads=[V.r], writes=[S["BCT"].r])
                if cc < 20:
                    XS = xst[cc % 2]
                    for i0 in range(0, NCH, 8):
                        PT = ptb[kt_ % 2]
                        kt_ += 1
                        i1 = min(NCH, i0 + 8)
                        for i in range(i0, i1):
                            p.op("pe", lambda e, PT=PT, V=V, i=i, i0=i0: e.transpose(PT.t[:, (i - i0) * 128:(i - i0 + 1) * 128],
                                                                                   V.t[:, i * 128:(i + 1) * 128], idb.t[:]),
                                 reads=[V.r, idb.r], writes=[PT.r])
                        p.op("dve", lambda e, PT=PT, XS=XS, i0=i0, i1=i1: e.tensor_copy(
                            out=XS.t[:, i0:i1, :].rearrange("p a b -> p (a b)"), in_=PT.t[:, 0:(i1 - i0) * 128]),
                            reads=[PT.r], writes=[XS.r])
                    p.dma("act", S["xB"].t.rearrange("(t p) f -> p t f", p=128)[:, :, cc * 128:(cc + 1) * 128], XS.t[:],
                          reads=[XS.r], writes=[S["xB"].r])
            p.flush()


def phase_ssd_scan(cx, Wd, S):
    p, nc = cx.p, cx.nc
    with ExitStack() as st:
        Tf = tri_const(cx, st, "p_le_j")
        Tb = tri_const(cx, st, "p_ge_j")
        Lf = tri_const(cx, st, "p_gt_j")
        Lb = tri_const(cx, st, "p_lt_j")
        ones = cx.sb(st, [128, 128], F32, "ones_s")
        p.op("pool", lambda e: e.memset(ones.t[:], 1.0), writes=[ones.r])
        abc = cx.sb(st, [128, 64], F32, "abc")
        dsk = cx.sb(st, [128, 64], F32, "dsk")
        load_bc(cx, "sp", (abc, abc.t[:]), Wd["a_log"], 64)
        load_bc(cx, "sp", (dsk, dsk.t[:]), Wd["d_skip"], 64)
        p.op("act", lambda e: e.activation(out=abc.t[:], in_=abc.t[:], func=AF.Exp), reads=[abc.r], writes=[abc.r])
        p.op("dve", lambda e: e.tensor_scalar(out=abc.t[:], in0=abc.t[:], scalar1=-1.0, scalar2=None, op0=ALU.mult),
             reads=[abc.r], writes=[abc.r])
        ST = cx.sb(st, [128, 32, 64], F32, "ST")
        STb = cx.sb(st, [128, 32, 64], BF16, "STb")
        xB = [cx.sb(st, [128, 2560], BF16, "xB") for _ in range(2)]
        BCT = [cx.sb(st, [128, 8, 128], BF16, "BCT") for _ in range(2)]
        dtt = [cx.sb(st, [128, 64], F32, "dtt") for _ in range(2)]
        sm = [cx.sb(st, [128, 6, 32], F32, "sm6") for _ in range(2)]
        xw = [cx.sb(st, [128, 32, 64], BF16, "xw") for _ in range(2)]
        xdt = [cx.sb(st, [128, 32, 64], BF16, "xdt") for _ in range(2)]
        CBm = [cx.sb(st, [128, 4, 128], F32, "CBm") for _ in range(2)]
        Lq = [cx.sb(st, [128, 8, 128], F32, "Lq") for _ in range(2)]
        Eq = [cx.sb(st, [128, 8, 128], F32, "Eq") for _ in range(2)]
        Mq = [cx.sb(st, [128, 8, 128], BF16, "Mq") for _ in range(2)]
        yo = [cx.sb(st, [128, 2048], F32, "yo") for _ in range(2)]
        y2 = [cx.sb(st, [128, 2048], F32, "y2") for _ in range(2)]
        psm = cx.ps(st, [128, 512], F32, "psm6")
        pD = [cx.ps(st, [128, 1024], F32, "pD") for _ in range(2)]
        pY = cx.ps(st, [128, 512], F32, "pY")
        pI = cx.ps(st, [128, 512], F32, "pI")
        pS = cx.ps(st, [128, 512], F32, "pS")
        xBv = S["xB"].t
        kq = 0
        kh = 0
        for d in range(2):
            Tc, Ls = (Tf, Lf) if d == 0 else (Tb, Lb)
            p.op("pool", lambda e: e.memset(ST.t[:], 0.0), writes=[ST.r])
            order = list(range(NCH)) if d == 0 else [1, 0] + list(range(NCH - 1, 1, -1))
            def pre(c, b, d=d, Tc=Tc, Ls=Ls):
                XB, BC, DT, SM, XW, XD, CB, YO, Y2 = xB[b], BCT[b], dtt[b], sm[b], xw[b], xdt[b], CBm[b], yo[b], y2[b]
                p.dma("sp", XB.t[:], xBv[c * 128:(c + 1) * 128, :], reads=[S["xB"].r], writes=[XB.r])
                p.dma("act", BC.t[:], S["BCT"].t[:, :, c * 128:(c + 1) * 128], reads=[S["BCT"].r], writes=[BC.r])
                p.dma("sp", DT.t[:], S["dt"].t[c * 128:(c + 1) * 128, :], reads=[S["dt"].r], writes=[DT.r])
                dtd = DT.t[:, d * 32:(d + 1) * 32]
                R_ = [SM.r]
                p.op("dve", lambda e, SM=SM, dtd=dtd, d=d: e.tensor_tensor(out=SM.t[:, 0, :], in0=dtd, in1=abc.t[:, d * 32:(d + 1) * 32], op=ALU.mult),
                     reads=[DT.r, abc.r], writes=R_)
                p.op("pe", lambda e, SM=SM, Tc=Tc: e.matmul(psm.t[:, 0:32], lhsT=Tc.t[:], rhs=SM.t[:, 0, :], start=True, stop=True),
                     reads=[Tc.r] + R_, writes=[psm.r])
                p.op("pe", lambda e, SM=SM: e.matmul(psm.t[:, 32:64], lhsT=ones.t[:], rhs=SM.t[:, 0, :], start=True, stop=True),
                     reads=[ones.r] + R_, writes=[psm.r])
                p.op("dve", lambda e, SM=SM: e.tensor_copy(out=SM.t[:, 2, :], in_=psm.t[:, 0:32]), reads=[psm.r], writes=R_)
                p.op("dve", lambda e, SM=SM: e.tensor_copy(out=SM.t[:, 5, :], in_=psm.t[:, 32:64]), reads=[psm.r], writes=R_)
                p.op("act", lambda e, SM=SM: e.activation(out=SM.t[:, 1, :], in_=SM.t[:, 2, :], func=AF.Exp), reads=R_, writes=R_)
                p.op("act", lambda e, SM=SM: e.activation(out=SM.t[:, 4, :], in_=SM.t[:, 5, :], func=AF.Exp), reads=R_, writes=R_)
                p.op("dve", lambda e, SM=SM: e.tensor_tensor(out=SM.t[:, 3, :], in0=SM.t[:, 5, :], in1=SM.t[:, 2, :], op=ALU.subtract),
                     reads=R_, writes=R_)
                p.op("act", lambda e, SM=SM: e.activation(out=SM.t[:, 3, :], in_=SM.t[:, 3, :], func=AF.Exp), reads=R_, writes=R_)
                p.op("dve", lambda e, SM=SM, dtd=dtd: e.tensor_tensor(out=SM.t[:, 3, :], in0=SM.t[:, 3, :], in1=dtd, op=ALU.mult),
                     reads=R_ + [DT.r], writes=R_)
                xs3 = XB.t[:, 0:2048].rearrange("p (h d) -> p h d", h=32)
                p.op("pool", lambda e, XW=XW, xs3=xs3, SM=SM: e.tensor_tensor(
                    out=XW.t[:], in0=xs3, in1=SM.t[:, 3, :].unsqueeze(2).to_broadcast([128, 32, 64]), op=ALU.mult),
                    reads=[XB.r] + R_, writes=[XW.r])
                return dict(XB=XB, BC=BC, DT=DT, SM=SM, XW=XW, XD=XD, CB=CB, YO=YO, Y2=Y2, dtd=dtd, R_=R_, xs3=xs3)

            def body(c, L, d=d, Tc=Tc, Ls=Ls):
                nonlocal kh
                XB, BC, DT, SM, XW, XD, CB, YO, Y2 = L['XB'], L['BC'], L['DT'], L['SM'], L['XW'], L['XD'], L['CB'], L['YO'], L['Y2']
                dtd, R_, xs3 = L['dtd'], L['R_'], L['xs3']
                if c >= 2:
                    p.op("pool", lambda e, XD=XD, xs3=xs3, dtd=dtd: e.tensor_tensor(
                        out=XD.t[:], in0=xs3, in1=dtd.unsqueeze(2).to_broadcast([128, 32, 64]), op=ALU.mult),
                        reads=[XB.r, DT.r], writes=[XD.r])
                    for g in range(4):
                        p.op("pe", lambda e, BC=BC, g=g: e.matmul(pS.t[:, g * 128:(g + 1) * 128], lhsT=BC.t[:, g, :], rhs=BC.t[:, 4 + g, :],
                                                                  start=True, stop=True), reads=[BC.r], writes=[pS.r])
                    p.op("dve", lambda e, CB=CB, Tc=Tc: e.tensor_tensor(
                        out=CB.t[:], in0=pS.t[:].rearrange("p (g t) -> p g t", g=4),
                        in1=Tc.t[:].unsqueeze(1).to_broadcast([128, 4, 128]), op=ALU.mult), reads=[pS.r, Tc.r], writes=[CB.r])
                    p.op("dve", lambda e, STb=STb: e.tensor_copy(out=STb.t[:], in_=ST.t[:]), reads=[ST.r], writes=[STb.r])
                    for g in range(4):
                        L_, E_, M_, PD = Lq[kh % 2], Eq[kh % 2], Mq[kh % 2], pD[kh % 2]
                        kh += 1
                        p.op("pe", lambda e, BC=BC, g=g: e.matmul(
                            pI.t[:], lhsT=BC.t[:, 4 + g, :], rhs=STb.t[:, g * 8:(g + 1) * 8, :].rearrange("p a b -> p (a b)"),
                            start=True, stop=True), reads=[BC.r, STb.r], writes=[pI.r])
                        p.op("pool", lambda e, L_=L_, Ls=Ls, SM=SM, g=g: e.tensor_tensor(
                            out=L_.t[:], in0=Ls.t[:].unsqueeze(1).to_broadcast([128, 8, 128]),
                            in1=SM.t[:, 0, g * 8:(g + 1) * 8].unsqueeze(2).to_broadcast([128, 8, 128]), op=ALU.mult),
                            reads=[Ls.r] + R_, writes=[L_.r])
                        for hh in range(8):
                            p.op("pe", lambda e, PD=PD, L_=L_, Tc=Tc, hh=hh: e.matmul(
                                PD.t[:, hh * 128:(hh + 1) * 128], lhsT=L_.t[:, hh, :], rhs=Tc.t[:], start=True, stop=True),
                                reads=[L_.r, Tc.r], writes=[PD.r])
                        p.op("act", lambda e, PD=PD, E_=E_: e.activation(out=E_.t[:].rearrange("p a b -> p (a b)"), in_=PD.t[:], func=AF.Exp),
                             reads=[PD.r], writes=[E_.r])
                        p.op("dve", lambda e, E_=E_, M_=M_, CB=CB, g=g: e.tensor_tensor(
                            out=M_.t[:], in0=E_.t[:], in1=CB.t[:, g, :].unsqueeze(1).to_broadcast([128, 8, 128]), op=ALU.mult),
                            reads=[E_.r, CB.r], writes=[M_.r])
                        for hh in range(8):
                            h = g * 8 + hh
                            p.op("pe", lambda e, M_=M_, XD=XD, hh=hh, h=h: e.matmul(
                                pY.t[:, hh * 64:(hh + 1) * 64], lhsT=M_.t[:, hh, :], rhs=XD.t[:, h, :], start=True, stop=True),
                                reads=[M_.r, XD.r], writes=[pY.r])
                        p.op("dve", lambda e, YO=YO, SM=SM, g=g: e.tensor_tensor(
                            out=YO.t[:, g * 512:(g + 1) * 512].rearrange("p (a b) -> p a b", a=8),
                            in0=pI.t[:].rearrange("p (a b) -> p a b", a=8),
                            in1=SM.t[:, 1, g * 8:(g + 1) * 8].unsqueeze(2).to_broadcast([128, 8, 64]), op=ALU.mult),
                            reads=[pI.r] + R_, writes=[YO.r])
                        p.op("dve", lambda e, YO=YO, g=g: e.tensor_tensor(
                            out=YO.t[:, g * 512:(g + 1) * 512], in0=YO.t[:, g * 512:(g + 1) * 512], in1=pY.t[:], op=ALU.add),
                            reads=[pY.r, YO.r], writes=[YO.r])
                    p.op("pool", lambda e, Y2=Y2, xs3=xs3, d=d: e.tensor_tensor(
                        out=Y2.t[:].rearrange("p (h d) -> p h d", h=32), in0=xs3,
                        in1=dsk.t[:, d * 32:(d + 1) * 32].unsqueeze(2).to_broadcast([128, 32, 64]), op=ALU.mult),
                        reads=[XB.r, dsk.r], writes=[Y2.r])
                    p.op("pool", lambda e, Y2=Y2, YO=YO: e.tensor_tensor(out=Y2.t[:], in0=Y2.t[:], in1=YO.t[:], op=ALU.add),
                         reads=[YO.r, Y2.r], writes=[Y2.r])
                    yd = S["yf"] if d == 0 else S["yb"]
                    p.dma("act", yd.t[(c - 2) * 128:(c - 1) * 128, :], Y2.t[:], reads=[Y2.r], writes=[yd.r])
                for g in range(4):
                    p.op("pe", lambda e, XB=XB, XW=XW, g=g: e.matmul(
                        pI.t[:], lhsT=XB.t[:, 2048 + g * 128:2048 + (g + 1) * 128],
                        rhs=XW.t[:, g * 8:(g + 1) * 8, :].rearrange("p a b -> p (a b)"), start=True, stop=True),
                        reads=[XB.r, XW.r], writes=[pI.r])
                    stg = ST.t[:, g * 8:(g + 1) * 8, :]
                    p.op("dve", lambda e, stg=stg, SM=SM, g=g: e.tensor_tensor(
                        out=stg, in0=stg, in1=SM.t[:, 4, g * 8:(g + 1) * 8].unsqueeze(2).to_broadcast([128, 8, 64]), op=ALU.mult),
                        reads=[ST.r] + R_, writes=[ST.r])
                    p.op("dve", lambda e, stg=stg: e.tensor_tensor(
                        out=stg, in0=stg, in1=pI.t[:].rearrange("p (a b) -> p a b", a=8), op=ALU.add),
                        reads=[ST.r, pI.r], writes=[ST.r])

            nxt = pre(order[0], kq % 2)
            kq += 1
            for ci, c in enumerate(order):
                cur = nxt
                if ci + 1 < len(order):
                    nxt = pre(order[ci + 1], kq % 2)
                    kq += 1
                body(c, cur)
        p.flush()


def phase_ssd_out(cx, xown, sel, mod_d, g_norm, Wd, S, x2_d):
    p, nc = cx.p, cx.nc
    w_in = Wd["w_in"]
    with ExitStack() as st:
        idn = make_ident(cx, st, F32)
        idb = cx.sb(st, [128, 128], BF16, "idb7")
        p.op("dve", lambda e: e.tensor_copy(out=idb.t[:], in_=idn.t[:]), reads=[idn.r], writes=[idb.r])
        gbc = cx.sb(st, [128, 1024], F32, "gbc7")
        Abc = cx.sb(st, [128, 1024], F32, "Abc7")
        Bbc = cx.sb(st, [128, 1024], F32, "Bbc7")
        g1 = cx.sb(st, [128, 1024], F32, "g17")
        ngb = cx.sb(st, [128, 2048], F32, "ngb")
        selb = cx.sb(st, [128, 2], F32, "selb")
        load_bc(cx, "sp", (gbc, gbc.t[:]), g_norm, 1024)
        load_bc(cx, "sp", (Bbc, Bbc.t[:]), mod_d.t[0, 0:1024], 1024, src=mod_d)
        load_bc(cx, "act", (Abc, Abc.t[:]), mod_d.t[0, 1024:2048], 1024, src=mod_d)
        load_bc(cx, "act", (g1, g1.t[:]), mod_d.t[0, 2048:3072], 1024, src=mod_d)
        load_bc(cx, "sp", (ngb, ngb.t[:]), Wd["norm_g"], 2048)
        load_bc(cx, "sp", (selb, selb.t[:]), sel, 2)
        p.op("dve", lambda e: e.scalar_tensor_tensor(out=Abc.t[:], in0=Abc.t[:], scalar=1.0, in1=gbc.t[:], op0=ALU.add, op1=ALU.mult),
             reads=[Abc.r, gbc.r], writes=[Abc.r])
        w_z = cx.sb(st, [128, 8, 2048], BF16, "w_z")
        w_o = cx.sb(st, [128, 16, 1024], BF16, "w_o")
        wv = w_in.rearrange("(kc p) n -> p kc n", p=128)
        for kc in range(8):
            p.dma("pool", w_z.t[:, kc, :], wv[:, kc, 0:2048], writes=[w_z.r])
        wov = Wd["w_out"].rearrange("(kc p) n -> p kc n", p=128)
        for kc in range(0, 16, 4):
            p.dma("pool", w_o.t[:, kc:kc + 4, :], wov[:, kc:kc + 4, :], writes=[w_o.r])
        x_t = [cx.sb(st, [128, 1024], F32, "x_t7") for _ in range(2)]
        h_t = [cx.sb(st, [128, 1024], F32, "h_t7") for _ in range(2)]
        junk = cx.sb(st, [128, 1024], F32, "junk7")
        sm = [cx.sb(st, [128, 16], F32, "sm7") for _ in range(2)]
        hT = [cx.sb(st, [128, 8, 128], BF16, "hT7") for _ in range(2)]
        ya = [cx.sb(st, [128, 2048], F32, "ya") for _ in range(2)]
        yb_ = [cx.sb(st, [128, 2048], F32, "yb") for _ in range(2)]
        sz = [cx.sb(st, [128, 2048], F32, "sz") for _ in range(2)]
        un = [cx.sb(st, [128, 2048], BF16, "un") for _ in range(2)]
        uT = [cx.sb(st, [128, 16, 128], BF16, "uT") for _ in range(2)]
        ptA = cx.ps(st, [128, 1024], F32, "ptA7")
        pz = cx.ps(st, [128, 2048], F32, "pz")
        ptu = cx.ps(st, [128, 2048], BF16, "ptu")
        for i in range(16):
            b = i % 2
            X, H, SM, HT, YA, YB, SZ, UN, UT = x_t[b], h_t[b], sm[b], hT[b], ya[b], yb_[b], sz[b], un[b], uT[b]
            xap, xrd = xown(i) if callable(xown) else (xown[i * 128:(i + 1) * 128, :], [])
            p.dma("sp", X.t[:], xap, reads=xrd, writes=[X.r])
            p.dma("act", YA.t[:], S["yf"].t[i * 128:(i + 1) * 128, :], reads=[S["yf"].r], writes=[YA.r])
            p.dma("sp", YB.t[:], S["yb"].t[i * 128:(i + 1) * 128, :], reads=[S["yb"].r], writes=[YB.r])
            p.op("pool", lambda e, YA=YA, YB=YB: e.tensor_tensor(out=YA.t[:], in0=YA.t[:], in1=YB.t[:], op=ALU.add),
                 reads=[YA.r, YB.r], writes=[YA.r])
            p.op("pool", lambda e, YA=YA: e.tensor_scalar(out=YA.t[:], in0=YA.t[:], scalar1=selb.t[:, 0:1], scalar2=None, op0=ALU.mult),
                 reads=[YA.r, selb.r], writes=[YA.r])
            p.dma("sp", YB.t[:], S["yf"].t[(16 + i) * 128:(17 + i) * 128, :], reads=[S["yf"].r], writes=[YB.r])
            p.op("dve", lambda e, YA=YA, YB=YB: e.scalar_tensor_tensor(out=YA.t[:], in0=YB.t[:], scalar=selb.t[:, 1:2], in1=YA.t[:],
                                                                        op0=ALU.mult, op1=ALU.add),
                 reads=[YA.r, YB.r, selb.r], writes=[YA.r])
            p.dma("sp", YB.t[:], S["yb"].t[(16 + i) * 128:(17 + i) * 128, :], reads=[S["yb"].r], writes=[YB.r])
            p.op("dve", lambda e, YA=YA, YB=YB: e.scalar_tensor_tensor(out=YA.t[:], in0=YB.t[:], scalar=selb.t[:, 1:2], in1=YA.t[:],
                                                                        op0=ALU.mult, op1=ALU.add),
                 reads=[YA.r, YB.r, selb.r], writes=[YA.r])
            p.op("act", lambda e, X=X, SM=SM: e.activation(out=junk.t[:], in_=X.t[:], func=AF.Square, accum_out=SM.t[:, 0:1]),
                 reads=[X.r], writes=[junk.r, SM.r])
            emit_rstd(cx, (SM, SM.t[:, 0:1]), (SM, SM.t[:, 1:2]), 1024.0)
            p.op("dve", lambda e, X=X, H=H, SM=SM: e.scalar_tensor_tensor(
                out=H.t[:], in0=X.t[:], scalar=SM.t[:, 1:2], in1=Abc.t[:], op0=ALU.mult, op1=ALU.mult),
                reads=[X.r, SM.r, Abc.r], writes=[H.r])
            p.op("dve", lambda e, H=H: e.tensor_tensor(out=H.t[:], in0=H.t[:], in1=Bbc.t[:], op=ALU.add),
                 reads=[H.r, Bbc.r], writes=[H.r])
            emit_transpose8(cx, H, HT, ptA, idn)
            for nb in range(4):
                for kc in range(8):
                    p.op("pe", lambda e, HT=HT, kc=kc, nb=nb: e.matmul(
                        pz.t[:, nb * 512:(nb + 1) * 512], lhsT=HT.t[:, kc, :], rhs=w_z.t[:, kc, nb * 512:(nb + 1) * 512],
                        start=(kc == 0), stop=(kc == 7)), reads=[HT.r, w_z.r], writes=[pz.r])
            p.op("act", lambda e, SZ=SZ: e.activation(out=SZ.t[:], in_=pz.t[:], func=AF.Silu), reads=[pz.r], writes=[SZ.r])
            p.op("dve", lambda e, SZ=SZ, YA=YA: e.tensor_tensor(out=SZ.t[:], in0=SZ.t[:], in1=YA.t[:], op=ALU.mult),
                 reads=[SZ.r, YA.r], writes=[SZ.r])
            for g in range(4):
                p.op("act", lambda e, SZ=SZ, SM=SM, g=g: e.activation(out=junk.t[:, 0:512], in_=SZ.t[:, g * 512:(g + 1) * 512], func=AF.Square,
                                                                     accum_out=SM.t[:, 4 + g:5 + g]), reads=[SZ.r], writes=[junk.r, SM.r])
            emit_rstd(cx, (SM, SM.t[:, 4:8]), (SM, SM.t[:, 8:12]), 512.0)
            p.op("dve", lambda e, SZ=SZ, SM=SM: e.tensor_tensor(
                out=SZ.t[:].rearrange("p (g c) -> p g c", g=4), in0=SZ.t[:].rearrange("p (g c) -> p g c", g=4),
                in1=SM.t[:, 8:12].unsqueeze(2).to_broadcast([128, 4, 512]), op=ALU.mult), reads=[SZ.r, SM.r], writes=[SZ.r])
            p.op("pool", lambda e, SZ=SZ, UN=UN: e.tensor_tensor(out=UN.t[:], in0=SZ.t[:], in1=ngb.t[:], op=ALU.mult),
                 reads=[SZ.r, ngb.r], writes=[UN.r])
            for kc in range(16):
                p.op("pe", lambda e, UN=UN, kc=kc: e.transpose(ptu.t[:, kc * 128:(kc + 1) * 128], UN.t[:, kc * 128:(kc + 1) * 128], idb.t[:]),
                     reads=[UN.r, idb.r], writes=[ptu.r])
            p.op("act", lambda e, UT=UT: e.activation(out=UT.t[:].rearrange("p a b -> p (a b)"), in_=ptu.t[:], func=AF.Copy),
                 reads=[ptu.r], writes=[UT.r])
            for hb in range(2):
                for kc in range(16):
                    p.op("pe", lambda e, UT=UT, hb=hb, kc=kc: e.matmul(
                        ptA.t[:, hb * 512:(hb + 1) * 512], lhsT=UT.t[:, kc, :], rhs=w_o.t[:, kc, hb * 512:(hb + 1) * 512],
                        start=(kc == 0), stop=(kc == 15)), reads=[UT.r, w_o.r], writes=[ptA.r])
            p.op("dve", lambda e, H=H: e.tensor_tensor(out=H.t[:], in0=ptA.t[:], in1=g1.t[:], op=ALU.mult),
                 reads=[ptA.r, g1.r], writes=[H.r])
            p.op("pool", lambda e, H=H, X=X: e.tensor_tensor(out=H.t[:], in0=H.t[:], in1=X.t[:], op=ALU.add),
                 reads=[H.r, X.r], writes=[H.r])
            p.dma("act", x2_d.t[i * 128:(i + 1) * 128, :], H.t[:], reads=[H.r], writes=[x2_d.r])
        p.flush()


def phase_final_norm(cx, x_d, final_g, out):
    p = cx.p
    with ExitStack() as st:
        gbc = cx.sb(st, [128, 1024], F32, "gbc9")
        load_bc(cx, "sp", (gbc, gbc.t[:]), final_g, 1024)
        x_t = [cx.sb(st, [128, 1024], F32, "x_t9") for _ in range(2)]
        junk = cx.sb(st, [128, 1024], F32, "junk9")
        sm = [cx.sb(st, [128, 4], F32, "sm9") for _ in range(2)]
        for i in range(16):
            X, SM = x_t[i % 2], sm[i % 2]
            p.dma("sp", X.t[:], x_d.t[i * 128:(i + 1) * 128, :], reads=[x_d.r], writes=[X.r])
            p.op("act", lambda e, X=X, SM=SM: e.activation(out=junk.t[:], in_=X.t[:], func=AF.Square, accum_out=SM.t[:, 0:1]),
                 reads=[X.r], writes=[junk.r, SM.r])
            emit_rstd(cx, (SM, SM.t[:, 0:1]), (SM, SM.t[:, 1:2]), 1024.0)
            p.op("dve", lambda e, X=X, SM=SM: e.scalar_tensor_tensor(out=X.t[:], in0=X.t[:], scalar=SM.t[:, 1:2], in1=gbc.t[:],
                                                                     op0=ALU.mult, op1=ALU.mult), reads=[X.r, SM.r, gbc.r], writes=[X.r])
            p.dma("act", out[i * 128:(i + 1) * 128, :], X.t[:], reads=[X.r])
        p.flush()


SSD_W = (("w_in", [1024, 5184]), ("conv_w", [5, 3072]), ("conv_b", [3072]), ("dt_bias", [64]), ("a_log", [64]),
         ("d_skip", [64]), ("norm_g", [2048]), ("w_out", [2048, 1024]))


def build_B(stop_after=None):
    nc = bass.Bass("TRN2", target_bir_lowering=False)
    inp = lambda name, shape: nc.dram_tensor(name, list(shape), F32, kind="ExternalInput").ap()
    xin = inp("xin", [NK, 1024])
    xown = inp("xown", [2048, 1024])
    sel = inp("sel", [2])
    cvec = inp("cvec", [2, 1024])
    w_mod = inp("w_mod", [1024, 6144])
    b_mod = inp("b_mod", [6144])
    norm_g = inp("norm_g", [2, 1024])
    final_g = inp("final_g", [1024])
    Wd = {k: inp("ssd_" + k, s) for k, s in SSD_W}
    M = {k: inp("moe_" + k, s) for k, s in MOE_W}
    out = nc.dram_tensor("out", [2048, 1024], F32, kind="ExternalOutput").ap()
    with ExitStack() as st:
        cx = Ctx(nc, st)
        mod_d = cx.dram([2, 6144], F32, "mod_d")
        S = {"dt": cx.dram([NK, 64], F32, "dt_d"), "BCT": cx.dram([128, 8, NK], BF16, "BCT_d"),
             "xB": cx.dram([NK, 2560], BF16, "xB_d"), "yf": cx.dram([4096, 2048], F32, "yf_d"),
             "yb": cx.dram([4096, 2048], F32, "yb_d")}
        phase_mod(cx, cvec, w_mod, b_mod, mod_d)
        phase_ssd_prep(cx, xin, mod_d, norm_g[0, :], Wd, S)
        if DBG_CUT == 301:
            return nc
        phase_ssd_scan(cx, Wd, S)
        if DBG_CUT == 302:
            return nc
        if stop_after == "ssd":
            phase_ssd_out(cx, xown, sel, mod_d, norm_g[0, :], Wd, S, TL(out))
            return nc
        x2_d = cx.dram([2048, 1024], F32, "x2_d")
        x3_d = cx.dram([2048, 1024], F32, "x3_d")
        phase_ssd_out(cx, xown, sel, mod_d, norm_g[0, :], Wd, S, x2_d)
        phase_moe(cx, x2_d, 16, mod_d, norm_g[1, :], M, x3_d, n_ctx_tiles=0)
        phase_final_norm(cx, x3_d, final_g, out)
    return nc


def inputs_B(core, x1, xc1, c, c_ctx, w_mod, b_mod, norm_g, final_g, ssd, moe):
    b, hf = core // 2, core % 2
    d = {"xin": np.ascontiguousarray(np.concatenate([xc1[b], x1[b]], axis=0)),
         "xown": np.ascontiguousarray(x1[b, hf * 2048:(hf + 1) * 2048]),
         "sel": np.array([1.0 - hf, float(hf)], np.float32),
         "cvec": np.ascontiguousarray(np.stack([c[b], c_ctx], axis=0)),
         "w_mod": w_mod[1], "b_mod": b_mod[1], "norm_g": norm_g[1], "final_g": final_g}
    for k, s in SSD_W:
        d["ssd_" + k] = np.ascontiguousarray(ssd[k][0].reshape(s))
    for k, s in MOE_W:
        d["moe_" + k] = np.ascontiguousarray(moe[k][1].reshape(s))
    return d


def build_fused():
    nc = bass.Bass("TRN2", target_bir_lowering=False)
    inp = lambda name, shape: nc.dram_tensor(name, list(shape), F32, kind="ExternalInput").ap()
    xin = inp("xin", [NK, 1024])
    cvec = inp("cvec", [2, 1024])
    rope = inp("rope", [NK, 96])
    sel = inp("sel", [2])
    w_mod = inp("w_mod", [2, 1024, 6144])
    b_mod = inp("b_mod", [2, 6144])
    norm_g = inp("norm_g", [2, 2, 1024])
    final_g = inp("final_g", [1024])
    W = {k: inp("att_" + k, s) for k, s in ATT_W}
    Wd = {k: inp("ssd_" + k, s) for k, s in SSD_W}
    M0 = {k: inp("moe0_" + k, s) for k, s in MOE_W}
    M1 = {k: inp("moe1_" + k, s) for k, s in MOE_W}
    out = nc.dram_tensor("out", [2048, 1024], F32, kind="ExternalOutput").ap()
    with ExitStack() as st:
        cx = Ctx(nc, st)
        mod0 = cx.dram([2, 6144], F32, "mod0_d")
        mod1 = cx.dram([2, 6144], F32, "mod1_d")
        S = {"KTm": cx.dram([96, 8, NK], BF16, "KTm_d"), "QTm": cx.dram([96, 8, NQ], BF16, "QTm_d"),
             "KTd": cx.dram([128, 4, NK], BF16, "KTd_d"), "QTd": cx.dram([128, 4, NQ], BF16, "QTd_d"),
             "Vm": cx.dram([NK, 528], BF16, "Vm_d"), "Vd": cx.dram([NK, 520], BF16, "Vd_d")}
        phase_mod(cx, cvec, w_mod[0], b_mod[0], mod0)
        phase_mod(cx, cvec, w_mod[1], b_mod[1], mod1)
        phase_attn_prep(cx, xin, rope, mod0, norm_g[0, 0, :], W, S)
        x1_d = cx.dram([NQ, 1024], F32, "x1_d")
        phase_attn_core(cx, xin, mod0, S, W, x1_d)
        xo_d = cx.dram([NQ, 1024], F32, "xo_d")
        phase_moe(cx, x1_d, NQ_T, mod0, norm_g[0, 1, :], M0, xo_d, n_ctx_tiles=2)
        xall = cx.dram([4096, 1024], F32, "xall_d")
        cx.p.dma_like("pool", lambda e: e.collective_compute(
            "AllGather", ALU.bypass, replica_groups=[[0, 1], [2, 3], [4, 5], [6, 7]],
            ins=[xo_d.t[256:NQ, :]], outs=[xall.t[:, :]]), reads=[xo_d.r], writes=[xall.r])
        cx.p.flush()
        S1 = {"dt": cx.dram([NK, 64], F32, "dt_d"), "BCT": cx.dram([128, 8, NK], BF16, "BCT_d"),
              "xB": cx.dram([NK, 2560], BF16, "xB_d"), "yf": cx.dram([4096, 2048], F32, "yf_d"),
              "yb": cx.dram([4096, 2048], F32, "yb_d")}

        def xtile(i):
            if i < 2:
                return xo_d.t[i * 128:(i + 1) * 128, :], [xo_d.r]
            return xall.t[(i - 2) * 128:(i - 1) * 128, :], [xall.r]

        def xown(i):
            return xo_d.t[256 + i * 128:256 + (i + 1) * 128, :], [xo_d.r]

        phase_ssd_prep(cx, xtile, mod1, norm_g[1, 0, :], Wd, S1)
        phase_ssd_scan(cx, Wd, S1)
        x2_d = cx.dram([2048, 1024], F32, "x2_d")
        x3_d = cx.dram([2048, 1024], F32, "x3_d")
        phase_ssd_out(cx, xown, sel, mod1, norm_g[1, 0, :], Wd, S1, x2_d)
        phase_moe(cx, x2_d, 16, mod1, norm_g[1, 1, :], M1, x3_d, n_ctx_tiles=0)
        phase_final_norm(cx, x3_d, final_g, out)
    return nc


def inputs_fused(core, x, c, ctx, c_ctx, w_mod, b_mod, norm_g, final_g, att, ssd, moe):
    b, hf = core // 2, core % 2
    d = inputs_A(core, x, c, ctx, c_ctx, w_mod, b_mod, norm_g, att, moe)
    for k, s in MOE_W:
        d["moe0_" + k] = d.pop("moe_" + k)
        d["moe1_" + k] = np.ascontiguousarray(moe[k][1].reshape(s))
    d["w_mod"], d["b_mod"], d["norm_g"], d["final_g"] = w_mod, b_mod, norm_g, final_g
    d["sel"] = np.array([1.0 - hf, float(hf)], np.float32)
    for k, s in SSD_W:
        d["ssd_" + k] = np.ascontiguousarray(ssd[k][0].reshape(s))
    return d


_NC_CACHE = {}


def kernel(x, c, ctx, c_ctx, w_mod, b_mod, norm_g, final_g,
           att_w_in, att_q_norm, att_w_uq, att_kv_norm, att_w_ukv,
           att_lq1, att_lk1, att_lq2, att_lk2, att_subln, att_w_out,
           ssd_w_in, ssd_conv_w, ssd_conv_b, ssd_dt_bias, ssd_a_log, ssd_d, ssd_norm_g, ssd_w_out,
           moe_w_rg, moe_b_rg, moe_w_rf, moe_b_rf, moe_w_gate, moe_w_up, moe_w_down):
    f = lambda a: np.asarray(a, dtype=np.float32)
    x, c, ctx, c_ctx, w_mod, b_mod, norm_g, final_g = map(f, (x, c, ctx, c_ctx, w_mod, b_mod, norm_g, final_g))
    att = {"w_in": f(att_w_in), "q_norm": f(att_q_norm), "w_uq": f(att_w_uq), "kv_norm": f(att_kv_norm), "w_ukv": f(att_w_ukv),
           "lq1": f(att_lq1), "lk1": f(att_lk1), "lq2": f(att_lq2), "lk2": f(att_lk2), "subln": f(att_subln), "w_out": f(att_w_out)}
    ssd = {"w_in": f(ssd_w_in), "conv_w": f(ssd_conv_w), "conv_b": f(ssd_conv_b), "dt_bias": f(ssd_dt_bias), "a_log": f(ssd_a_log),
           "d_skip": f(ssd_d), "norm_g": f(ssd_norm_g), "w_out": f(ssd_w_out)}
    moe = {"w_rg": f(moe_w_rg), "b_rg": f(moe_b_rg), "w_rf": f(moe_w_rf), "b_rf": f(moe_b_rf),
           "w_gate": f(moe_w_gate), "w_up": f(moe_w_up), "w_down": f(moe_w_down)}
    cores = list(range(8))
    ncA = build_A()
    resA = run_bass_kernel_spmd(ncA, [inputs_A(k, x, c, ctx, c_ctx, w_mod, b_mod, norm_g, att, moe) for k in cores], core_ids=cores)
    x1 = np.empty((4, 4096, 1024), np.float32)
    xc1 = np.empty((4, 256, 1024), np.float32)
    for k in cores:
        b, hf = k // 2, k % 2
        o = resA.results[k]["xout"]
        x1[b, hf * 2048:(hf + 1) * 2048] = o[256:]
        if hf == 0:
            xc1[b] = o[:256]
    ncB = build_B()
    resB = run_bass_kernel_spmd(ncB, [inputs_B(k, x1, xc1, c, c_ctx, w_mod, b_mod, norm_g, final_g, ssd, moe) for k in cores], core_ids=cores)
    out = np.empty((4, 4096, 1024), np.float32)
    for k in cores:
        b, hf = k // 2, k % 2
        out[b, hf * 2048:(hf + 1) * 2048] = resB.results[k]["out"]
    return out
```
